# Optimizing a Trainium2 kernel written in Bass

```python
import jax, jax.numpy as jnp
from jax import lax
import numpy as np

D_MODEL = 1024
BATCH = 2
SEQ = 8192
DEPTH = 1

HEAD_DIM = 64
DSA_GROUPS = ((128, 1), (512, 4), (2048, 16))
DSA_HEADS_PER_GROUP = 4
DSA_HEADS = DSA_HEADS_PER_GROUP * len(DSA_GROUPS)
MOBA_HEADS = 8
MOBA_BLOCK = 256
MOBA_TOPK = 3
MOBA_Q_CHUNK = 64
D_FF = ((8 * D_MODEL // 3 + 127) // 128) * 128
DSA_WIDTH = DSA_HEADS * HEAD_DIM
MOBA_WIDTH = MOBA_HEADS * HEAD_DIM
DSA_OUT = DSA_HEADS_PER_GROUP * HEAD_DIM
IN_WIDTH = 3 * DSA_WIDTH + 3 * MOBA_WIDTH + 2 * D_MODEL
EPS = 1e-6
NEG = -1e30

kernel_name = "hybrid_dilated_moba_macaron"

f32 = jnp.float32


def rmsnorm(x, g):
    xf = x.astype(f32)
    y = xf * lax.rsqrt(jnp.mean(xf * xf, axis=-1, keepdims=True) + EPS)
    return (y * g.astype(f32)).astype(x.dtype)


def swiglu(x, w_gate, w_up, w_down):
    return (jax.nn.silu(x @ w_gate) * (x @ w_up)) @ w_down


def alibi_slopes(n):
    return 2.0 ** (-8.0 * jnp.arange(1, n + 1, dtype=f32) / n)


def to_heads(a, n):
    b, s, _ = a.shape
    return a.reshape(b, s, n, HEAD_DIM).transpose(0, 2, 1, 3)


def dilated_window_attention(q, k, v, slopes, window, dilation):
    b, h, s, hd = q.shape
    blk = window // dilation
    L = s // dilation
    Lp = -(-L // blk) * blk
    nb = Lp // blk

    def to_residue(a):
        a = a.reshape(b, h, L, dilation, hd).transpose(0, 1, 3, 2, 4)
        return jnp.pad(a, ((0, 0), (0, 0), (0, 0), (0, Lp - L), (0, 0)))

    qr, kr, vr = to_residue(q), to_residue(k), to_residue(v)
    qb = qr.reshape(b, h, dilation, nb, blk, hd)

    def band(a):
        ap = jnp.pad(a, ((0, 0), (0, 0), (0, 0), (blk, 0), (0, 0)))
        prev = ap[:, :, :, :Lp].reshape(b, h, dilation, nb, blk, hd)
        cur = a.reshape(b, h, dilation, nb, blk, hd)
        return jnp.concatenate([prev, cur], axis=4)

    kb, vb = band(kr), band(vr)
    scores = jnp.einsum('bhrnqd,bhrnkd->bhrnqk', qb, kb).astype(f32)
    qi = jnp.arange(blk)[:, None]
    kj = jnp.arange(2 * blk)[None, :]
    delta = qi + blk - kj
    kpos = jnp.arange(nb)[:, None, None] * blk - blk + kj[None]
    valid = (delta >= 0)[None] & (delta <= blk)[None] & (kpos >= 0)
    bias = -slopes[:, None, None] * (delta * dilation).astype(f32)[None]
    scores = jnp.where(valid[None, None, None], scores + bias[None, :, None, None], NEG)
    lse = jax.nn.logsumexp(scores, axis=-1)
    p = jnp.exp(scores - lse[..., None]).astype(v.dtype)
    out = jnp.einsum('bhrnqk,bhrnkd->bhrnqd', p, vb)
    out = out.reshape(b, h, dilation, Lp, hd)[:, :, :, :L]
    out = out.transpose(0, 1, 3, 2, 4).reshape(b, h, s, hd)
    lse = lse.reshape(b, h, dilation, Lp)[..., :L].transpose(0, 1, 3, 2).reshape(b, h, s)
    return out, lse


def moba_attention(q, k, v, slopes):
    b, h, s, hd = q.shape
    nblk = -(-s // MOBA_BLOCK)
    sp = nblk * MOBA_BLOCK
    n_sel = min(MOBA_TOPK, nblk)
    padw = ((0, 0), (0, 0), (0, sp - s), (0, 0))
    q, k, v = jnp.pad(q, padw), jnp.pad(k, padw), jnp.pad(v, padw)
    bh = b * h
    kblk = k.reshape(bh, nblk, MOBA_BLOCK, hd)
    vblk = v.reshape(bh, nblk, MOBA_BLOCK, hd)
    qf = q.reshape(bh, sp, hd)
    kmean = jnp.mean(kblk.astype(f32), axis=2)
    gate = jnp.einsum('xtd,xnd->xtn', qf.astype(f32), kmean)
    own = jnp.arange(sp) // MOBA_BLOCK
    past = jnp.arange(nblk)[None, :] < own[:, None]
    gate = jnp.where(past[None], gate, NEG)
    _, sel = lax.top_k(gate, n_sel)
    slope_x = jnp.tile(slopes, b)
    nq = sp // MOBA_Q_CHUNK
    q_chunks = qf.reshape(bh, nq, MOBA_Q_CHUNK, hd).transpose(1, 0, 2, 3)
    sel_chunks = sel.reshape(bh, nq, MOBA_Q_CHUNK, n_sel).transpose(1, 0, 2, 3)
    take = jax.vmap(lambda blocks, ix: blocks[ix])
    blk_off = jnp.arange(MOBA_BLOCK)

    def chunk(args):
        qc, ix, c = args
        t = c * MOBA_Q_CHUNK + jnp.arange(MOBA_Q_CHUNK)
        own_blk = (c * MOBA_Q_CHUNK) // MOBA_BLOCK
        k_own = lax.dynamic_index_in_dim(kblk, own_blk, axis=1, keepdims=False)
        v_own = lax.dynamic_index_in_dim(vblk, own_blk, axis=1, keepdims=False)
        d_own = t[:, None] - (own_blk * MOBA_BLOCK + blk_off)[None, :]
        s_own = jnp.einsum('xqd,xkd->xqk', qc, k_own).astype(f32)
        s_own = jnp.where(d_own[None] >= 0,
                          s_own - slope_x[:, None, None] * d_own.astype(f32)[None], NEG)
        k_sel = take(kblk, ix)
        v_sel = take(vblk, ix)
        s_sel = jnp.einsum('xqd,xqjkd->xqjk', qc, k_sel).astype(f32)
        d_sel = t[None, :, None, None] - (ix[..., None] * MOBA_BLOCK + blk_off)
        ok = (ix < own_blk)[..., None]
        s_sel = jnp.where(ok, s_sel - slope_x[:, None, None, None] * d_sel.astype(f32), NEG)
        n_k = n_sel * MOBA_BLOCK
        scores = jnp.concatenate([s_sel.reshape(bh, MOBA_Q_CHUNK, n_k), s_own], axis=-1)
        p = jax.nn.softmax(scores, axis=-1).astype(v.dtype)
        p_sel = p[..., :n_k].reshape(bh, MOBA_Q_CHUNK, n_sel, MOBA_BLOCK)
        p_own = p[..., n_k:]
        return (jnp.einsum('xqjk,xqjkd->xqd', p_sel, v_sel)
                + jnp.einsum('xqk,xkd->xqd', p_own, v_own))

    out = lax.map(chunk, (q_chunks, sel_chunks, jnp.arange(nq)))
    return out.transpose(1, 0, 2, 3).reshape(b, h, sp, hd)[:, :, :s]


def hybrid_layer(x, norm_ffn1, ffn1_gate, ffn1_up, ffn1_down, norm_mix, w_in,
                 w_up_a, w_up_b, w_out, norm_ffn2, ffn2_gate, ffn2_up, ffn2_down):
    b, s, _ = x.shape
    x = x + 0.5 * swiglu(rmsnorm(x, norm_ffn1), ffn1_gate, ffn1_up, ffn1_down)
    h = rmsnorm(x, norm_mix)
    proj = h @ w_in
    o = 0
    parts = []
    for width in (DSA_WIDTH, DSA_WIDTH, DSA_WIDTH, MOBA_WIDTH, MOBA_WIDTH, MOBA_WIDTH,
                  D_MODEL, D_MODEL):
        parts.append(proj[..., o:o + width])
        o += width
    qa, ka, va, qb, kb, vb, g_a, g_b = parts
    scale = HEAD_DIM ** -0.5
    qa, ka, va = to_heads(qa, DSA_HEADS) * scale, to_heads(ka, DSA_HEADS), to_heads(va, DSA_HEADS)
    qb, kb, vb = to_heads(qb, MOBA_HEADS) * scale, to_heads(kb, MOBA_HEADS), to_heads(vb, MOBA_HEADS)

    slopes_a = alibi_slopes(DSA_HEADS)
    outs, lses = [], []
    for gi, (window, dilation) in enumerate(DSA_GROUPS):
        hs = slice(gi * DSA_HEADS_PER_GROUP, (gi + 1) * DSA_HEADS_PER_GROUP)
        og, lg = dilated_window_attention(qa[:, hs], ka[:, hs], va[:, hs], slopes_a[hs],
                                          window, dilation)
        outs.append(og)
        lses.append(lg)
    w_grp = jax.nn.softmax(jnp.stack(lses, 0), axis=0)
    y_a = jnp.sum(w_grp[..., None] * jnp.stack(outs, 0).astype(f32), axis=0).astype(x.dtype)
    y_a = y_a.transpose(0, 2, 1, 3).reshape(b, s, DSA_OUT)

    y_b = moba_attention(qb, kb, vb, alibi_slopes(MOBA_HEADS))
    y_b = y_b.transpose(0, 2, 1, 3).reshape(b, s, MOBA_WIDTH)

    merged = jax.nn.sigmoid(g_a) * (y_a @ w_up_a) + jax.nn.sigmoid(g_b) * (y_b @ w_up_b)
    x = x + merged @ w_out
    x = x + 0.5 * swiglu(rmsnorm(x, norm_ffn2), ffn2_gate, ffn2_up, ffn2_down)
    return x


def setup_inputs(seed: int = 0) -> dict:
    key = jax.random.key(seed)
    ks = jax.random.split(key, 16)

    def dense(k, fan_in, fan_out):
        return jax.random.normal(k, (DEPTH, fan_in, fan_out), f32) * fan_in ** -0.5

    def gain(k):
        return 1.0 + 0.02 * jax.random.normal(k, (DEPTH, D_MODEL), f32)

    return {
        "x": jax.random.normal(ks[0], (BATCH, SEQ, D_MODEL), f32),
        "norm_ffn1": gain(ks[1]),
        "ffn1_gate": dense(ks[2], D_MODEL, D_FF),
        "ffn1_up": dense(ks[3], D_MODEL, D_FF),
        "ffn1_down": dense(ks[4], D_FF, D_MODEL),
        "norm_mix": gain(ks[5]),
        "w_in": dense(ks[6], D_MODEL, IN_WIDTH),
        "w_up_a": dense(ks[7], DSA_OUT, D_MODEL),
        "w_up_b": dense(ks[8], MOBA_WIDTH, D_MODEL),
        "w_out": dense(ks[9], D_MODEL, D_MODEL),
        "norm_ffn2": gain(ks[10]),
        "ffn2_gate": dense(ks[11], D_MODEL, D_FF),
        "ffn2_up": dense(ks[12], D_MODEL, D_FF),
        "ffn2_down": dense(ks[13], D_FF, D_MODEL),
        "norm_final": 1.0 + 0.02 * jax.random.normal(ks[14], (D_MODEL,), f32),
    }


def reference(x, norm_ffn1, ffn1_gate, ffn1_up, ffn1_down, norm_mix, w_in, w_up_a,
              w_up_b, w_out, norm_ffn2, ffn2_gate, ffn2_up, ffn2_down, norm_final):
    for layer in range(DEPTH):
        x = hybrid_layer(x, norm_ffn1[layer], ffn1_gate[layer], ffn1_up[layer],
                         ffn1_down[layer], norm_mix[layer], w_in[layer], w_up_a[layer],
                         w_up_b[layer], w_out[layer], norm_ffn2[layer], ffn2_gate[layer],
                         ffn2_up[layer], ffn2_down[layer])
    return rmsnorm(x, norm_final)
```

```python
from contextlib import ExitStack
import numpy as np
import ml_dtypes
import concourse.bass as bass
import concourse.mybir as mybir
from concourse.bass_utils import run_bass_kernel_spmd

F32 = mybir.dt.float32
BF16 = mybir.dt.bfloat16
ALU = mybir.AluOpType
AF = mybir.ActivationFunctionType
AX = mybir.AxisListType

DM = 1024
SEQ = 8192
DFF = 2816
NFF = 22
WIN = 8192
OWN0 = 6144
NOWN = 2048
ST = 1024
EPS = 1e-6
BIG = 30000.0
NEGG = -1.0e30
DIL = (1, 4, 16)
CB = 4096

ENGS = ("pe", "act", "dve", "pool", "sp")
BUDGET = [None]


class Prog:
    def __init__(self, nc, stack, n_dma_slots=8):
        self.nc = nc
        self.sem = {e: stack.enter_context(nc.semaphore("sem_" + e)) for e in ("pe", "act", "dve", "pool")}
        self.base = {e: 0 for e in self.sem}
        self.dsem = {}
        self.dcnt = {}
        for q in ("sp", "pool", "act"):
            self.dsem[q] = [stack.enter_context(nc.semaphore(f"dma_{q}_{i}")) for i in range(n_dma_slots if q == "sp" else 4)]
            self.dcnt[q] = [0] * len(self.dsem[q])
        self.drr = {q: 0 for q in self.dsem}
        self._reset()

    def _reset(self):
        self.idx = {e: 0 for e in self.sem}
        self.ops = {e: [] for e in ENGS}
        self.last_w = {}
        self.readers = {}
        self.waited = {e: {} for e in ENGS}
        self.targets = {e: set() for e in self.sem}

    def _need(self, eng, tok, waits):
        if tok is None:
            return
        key, val = tok[0], tok[-1]
        if key == eng and eng == "pe":
            return
        if self.waited[eng].get(key, -1) >= val:
            return
        self.waited[eng][key] = val
        waits.append(tok)
        if key in self.targets:
            self.targets[key].add(val)

    def _deps(self, eng, reads, writes):
        waits = []
        for r in reads:
            self._need(eng, self.last_w.get(r), waits)
            if r.startswith("ps"):
                for t in self.readers.get(r, ()):
                    if t[0] != eng:
                        self._need(eng, t, waits)
        for w in writes:
            self._need(eng, self.last_w.get(w), waits)
            for t in self.readers.get(w, ()):
                self._need(eng, t, waits)
        return waits

    def _commit(self, tok, reads, writes):
        for r in reads:
            self.readers.setdefault(r, []).append(tok)
        for w in writes:
            self.last_w[w] = tok
            self.readers[w] = []

    def op(self, eng, fn, reads=(), writes=()):
        if BUDGET[0] is not None:
            BUDGET[0] -= 1
            if BUDGET[0] < 0:
                return
        waits = self._deps(eng, reads, writes)
        self.idx[eng] += 1
        tok = (eng, self.idx[eng])
        self.ops[eng].append((waits, fn, tok))
        self._commit(tok, reads, writes)

    def dma(self, fn, reads=(), writes=(), q="sp"):
        if BUDGET[0] is not None:
            BUDGET[0] -= 1
            if BUDGET[0] < 0:
                return
        waits = self._deps(q, reads, writes)
        i = self.drr[q]
        self.drr[q] = (i + 1) % len(self.dsem[q])
        key = f"d{q}{i}"
        prev = self.dcnt[q][i]
        if prev > 0 and self.waited[q].get(key, -1) < prev:
            self.waited[q][key] = prev
            waits.append((key, q, i, prev))
        self.dcnt[q][i] = prev + 16
        tok = (key, q, i, prev + 16)
        self.ops[q].append((waits, fn, tok))
        self._commit(tok, reads, writes)

    def flush(self):
        nc = self.nc
        for q in self.dsem:
            waits = []
            for i in range(len(self.dsem[q])):
                v = self.dcnt[q][i]
                key = f"d{q}{i}"
                if v > 0 and self.waited[q].get(key, -1) < v:
                    self.waited[q][key] = v
                    waits.append((key, q, i, v))
            if waits:
                self.ops[q].append((waits, None, None))
        ops = self.ops
        rank = {}
        for e in self.sem:
            for r_, ix in enumerate(sorted(self.targets[e])):
                rank[(e, ix)] = self.base[e] + r_ + 1
        sem, dsem = self.sem, self.dsem

        def resolve(tok):
            if len(tok) == 2:
                return sem[tok[0]], rank[tok]
            return dsem[tok[1]][tok[2]], tok[3]

        def replay(h, lst):
            for waits, fn, tok in lst:
                for w in waits:
                    s_, v_ = resolve(w)
                    h.wait_ge(s_, v_)
                if fn is not None:
                    ins = fn(h)
                    if len(tok) == 4:
                        ins.then_inc(dsem[tok[1]][tok[2]], 16)
                    elif tok in rank:
                        ins.then_inc(sem[tok[0]], 1)

        with nc.Block() as block:
            if ops["sp"]:
                @block.sync
                def _(e):
                    replay(e, ops["sp"])
            if ops["pe"]:
                @block.tensor
                def _(e):
                    replay(e, ops["pe"])
            if ops["act"]:
                @block.scalar
                def _(e):
                    replay(e, ops["act"])
            if ops["dve"]:
                @block.vector
                def _(e):
                    replay(e, ops["dve"])
            if ops["pool"]:
                @block.gpsimd
                def _(e):
                    replay(e, ops["pool"])
        for e in self.sem:
            self.base[e] += len(self.targets[e])
        self._reset()


def ss(start, n, step):
    return slice(start, start + (n - 1) * step + 1, step)


def _weight_layout():
    lay = {}
    off = 0

    def add(name, w):
        nonlocal off
        lay[name] = (off, w)
        off += w

    for t in ("f1", "f2"):
        for f in range(NFF):
            add(f"{t}gu{f}", 2 * 8 * 128)
        for o in range(8):
            add(f"{t}d{o}", NFF * 128)
    for g in range(3):
        for pr in range(2):
            add(f"dq{g}{pr}", 1024)
            add(f"dk{g}{pr}", 1024)
        add(f"dv{g}", 2048)
    for i in range(4):
        add(f"mq{i}", 1024)
        add(f"mk{i}", 1024)
        add(f"mv{i}", 1024)
    for o in range(8):
        add(f"ga{o}", 1024)
        add(f"gb{o}", 1024)
    for o in range(8):
        add(f"ua{o}", 512)
        add(f"ub{o}", 1024)
    for o in range(8):
        add(f"wo{o}", 1024)
    return lay, off


WLAY, TOTF = _weight_layout()

C_G1, C_GM, C_G2, C_GF = 0, 8, 16, 24
C_ID = 32
C_GMASK = C_ID + 128
C_OWNHOT = C_GMASK + 512
C_VALM = C_OWNHOT + 512
C_VALD = C_VALM + 64
C_DEC = C_VALD + 69
CF = C_DEC + 12 * 256
VALD_OFF = (0, 17, 37)


def _kchunks(w, c0, width):
    return np.ascontiguousarray(w[:, c0:c0 + width].reshape(8, 128, width).transpose(1, 0, 2)).reshape(128, 8 * width)


def _pack_weights(inp):
    wall = np.zeros((128, TOTF), np.float32)

    def put(name, arr):
        o, w = WLAY[name]
        assert arr.shape == (128, w), (name, arr.shape, w)
        wall[:, o:o + w] = arr

    for t, gk, uk, dk in (("f1", "ffn1_gate", "ffn1_up", "ffn1_down"), ("f2", "ffn2_gate", "ffn2_up", "ffn2_down")):
        wg, wu, wd = inp[gk][0], inp[uk][0], inp[dk][0]
        for f in range(NFF):
            a = np.stack([wg[:, f * 128:(f + 1) * 128].reshape(8, 128, 128), wu[:, f * 128:(f + 1) * 128].reshape(8, 128, 128)], 0)
            put(f"{t}gu{f}", a.transpose(2, 0, 1, 3).reshape(128, 2048))
        for o in range(8):
            put(f"{t}d{o}", wd[:, o * 128:(o + 1) * 128].reshape(NFF, 128, 128).transpose(1, 0, 2).reshape(128, NFF * 128))
    win = inp["w_in"][0]
    QA, KA, VA, QB, KB, VB, GA, GB = 0, 768, 1536, 2304, 2816, 3328, 3840, 4864
    for g in range(3):
        for pr in range(2):
            h0 = 4 * g + 2 * pr
            put(f"dq{g}{pr}", _kchunks(win, QA + h0 * 64, 128))
            put(f"dk{g}{pr}", _kchunks(win, KA + h0 * 64, 128))
        put(f"dv{g}", _kchunks(win, VA + 4 * g * 64, 256))
    for i in range(4):
        put(f"mq{i}", _kchunks(win, QB + i * 128, 128))
        put(f"mk{i}", _kchunks(win, KB + i * 128, 128))
        put(f"mv{i}", _kchunks(win, VB + i * 128, 128))
    for o in range(8):
        put(f"ga{o}", _kchunks(win, GA + o * 128, 128))
        put(f"gb{o}", _kchunks(win, GB + o * 128, 128))
    wua, wub, wo = inp["w_up_a"][0], inp["w_up_b"][0], inp["w_out"][0]
    for o in range(8):
        a = np.zeros((128, 4, 128), np.float32)
        a[:64] = wua[:, o * 128:(o + 1) * 128].reshape(4, 64, 128).transpose(1, 0, 2)
        put(f"ua{o}", a.reshape(128, 512))
        b = np.zeros((128, 8, 128), np.float32)
        b[:64] = wub[:, o * 128:(o + 1) * 128].reshape(8, 64, 128).transpose(1, 0, 2)
        put(f"ub{o}", b.reshape(128, 1024))
        put(f"wo{o}", _kchunks(wo, o * 128, 128))
    return wall


def _const_tables(inp, j):
    cst = np.zeros((128, CF), np.float32)
    for col, key in ((C_G1, "norm_ffn1"), (C_GM, "norm_mix"), (C_G2, "norm_ffn2"), (C_GF, "norm_final")):
        g = np.asarray(inp[key], np.float32).reshape(-1)
        cst[:, col:col + 8] = g.reshape(8, 128).T
    cst[:, C_ID:C_ID + 128] = np.eye(128, dtype=np.float32)
    first_valid_tok = 2048 * (3 - j)
    first_valid_blk = first_valid_tok // 256
    p = np.arange(128)
    blk = np.arange(32)
    for qt in range(16):
        q = qt * 128 + p
        ob = (OWN0 + q) // 256
        ok = (blk[None, :] < ob[:, None]) & (blk[None, :] >= first_valid_blk)
        cst[:, C_GMASK + qt * 32:C_GMASK + (qt + 1) * 32] = np.where(ok, 0.0, NEGG)
        cst[:, C_OWNHOT + qt * 32:C_OWNHOT + (qt + 1) * 32] = (blk[None, :] == ob[:, None]).astype(np.float32)
    t = np.arange(64)[None, :] * 128 + p[:, None]
    cst[:, C_VALM:C_VALM + 64] = (t >= first_valid_tok).astype(np.float32)
    for g, D in enumerate(DIL):
        nq = 16 // D
        for r in range(D):
            for jt in range(nq + 1):
                i = OWN0 // D - 128 + 128 * jt + p
                pos = r + D * i
                cst[:, C_VALD + VALD_OFF[g] + r * (nq + 1) + jt] = (pos >= first_valid_tok).astype(np.float32)
    slopes_a = 2.0 ** (-8.0 * np.arange(1, 13, dtype=np.float64) / 12)
    b = p[:, None].astype(np.float64)
    a = np.arange(128)[None, :].astype(np.float64)
    for h in range(12):
        D = DIL[h // 4]
        s = slopes_a[h]
        cur = np.where(a >= b, np.exp(-s * D * (a - b)), 0.0)
        prev = np.where(a <= b, np.exp(-s * D * (a - b + 128)), 0.0)
        cst[:, C_DEC + h * 256:C_DEC + h * 256 + 128] = cur
        cst[:, C_DEC + h * 256 + 128:C_DEC + (h + 1) * 256] = prev
    bf = ml_dtypes.bfloat16
    tk = np.arange(WIN)
    etab = (tk[None, :] // 256 == np.arange(32)[:, None]).astype(np.float32).astype(bf)
    slopes_b = 2.0 ** (-8.0 * np.arange(1, 9, dtype=np.float64) / 8)
    akt = np.zeros((8, 4, WIN), np.float32)
    aqt = np.zeros((8, 4, NOWN), np.float32)
    tq = OWN0 + np.arange(NOWN)
    for h in range(8):
        s = slopes_b[h]
        akt[h, 0] = s * 128 * (tk // 128)
        akt[h, 1] = s * (tk % 128)
        akt[h, 2] = 1.0
        akt[h, 3] = 1.0
        aqt[h, 0] = 1.0
        aqt[h, 1] = 1.0
        aqt[h, 2] = -s * 128 * (tq // 128)
        aqt[h, 3] = -s * (tq % 128)
    trib = (p[:, None] <= np.arange(128)[None, :]).astype(np.float32).astype(bf)
    return cst, etab, akt.astype(bf), aqt.astype(bf), trib


STOP = [99]
RUN = [set(range(6))]


def _phase(n):
    if n in RUN[0]:
        with ExitStack() as ph:
            yield ph
DBG = [False]
LAST = {}
SUB = [""]


class _Stop(Exception):
    pass


def _chk(n):
    if STOP[0] == n:
        raise _Stop()


def build():
    nc = bass.Bass("TRN2", target_bir_lowering=False)
    xw = nc.dram_tensor("xw", [8, 128, WIN], F32, kind="ExternalInput").ap()
    wall = nc.dram_tensor("wall", [128, TOTF], F32, kind="ExternalInput").ap()
    cstd = nc.dram_tensor("cst", [128, CF], F32, kind="ExternalInput").ap()
    etabd = nc.dram_tensor("etab", [32, WIN], BF16, kind="ExternalInput").ap()
    aktd = nc.dram_tensor("akt", [8, 4, WIN], BF16, kind="ExternalInput").ap()
    aqtd = nc.dram_tensor("aqt", [8, 4, NOWN], BF16, kind="ExternalInput").ap()
    tribd = nc.dram_tensor("trib", [128, 128], BF16, kind="ExternalInput").ap()
    outT = nc.dram_tensor("outT", [8, 128, NOWN], F32, kind="ExternalOutput").ap()
    WB = nc.dram_tensor("wb_s", [128, TOTF], BF16, kind=("Internal" if 0 in RUN[0] else "ExternalInput")).ap()
    sk = "ExternalOutput" if DBG[0] else "Internal"
    R_ = RUN[0]
    kin = lambda prod: sk if prod in R_ else "ExternalInput"
    HS = nc.dram_tensor("h_s", [8, 128, WIN], BF16, kind=kin(1)).ap()
    X1S = nc.dram_tensor("x1_s", [8, 128, NOWN], F32, kind=kin(1)).ap()
    MS = nc.dram_tensor("m_s", [8, 128, NOWN], BF16, kind=kin(4)).ap()
    YAD = nc.dram_tensor("ya_d", [64, 4, NOWN], BF16, kind=kin(2)).ap()
    YBD = nc.dram_tensor("yb_d", [64, 8, NOWN], BF16, kind=kin(3)).ap()
    HSr = HS.rearrange("k p t -> p k t")
    X1r = X1S.rearrange("k p t -> p k t")
    MSr = MS.rearrange("k p t -> p k t")
    xwr = xw.rearrange("k p t -> p k t")
    outr = outT.rearrange("k p t -> p k t")

    try:
        _build_body(nc, xwr, wall, cstd, etabd, aktd, aqtd, tribd, outr, WB, HSr, X1r, MSr, YAD, YBD)
    except _Stop:
        pass
    return nc


def _build_body(nc, xwr, wall, cstd, etabd, aktd, aqtd, tribd, outr, WB, HSr, X1r, MSr, YAD, YBD):
    with ExitStack() as top:
        P = Prog(nc, top)

        def SB(stk, name, shape, dt):
            return stk.enter_context(nc.sbuf_tensor("s_" + name, shape, dt))

        bank = [top.enter_context(nc.psum_tensor(f"bank{i}", [128, 512], F32)) for i in range(8)]
        cst = SB(top, "cst", [128, CF], F32)
        ones = SB(top, "ones", [128, 128], BF16)
        onesf = SB(top, "onesf", [128, 64], F32)
        trib = SB(top, "trib", [128, 128], BF16)

        def DMA(out, in_, reads, writes, q="sp"):
            P.dma(lambda e: e.dma_start(out=out, in_=in_), reads, writes, q=q)

        def MMS(groups, reads, writes):
            def fn(e):
                ins = None
                for out, pairs in groups:
                    n = len(pairs)
                    for i, (l, r) in enumerate(pairs):
                        ins = e.matmul(out, lhsT=l, rhs=r, start=(i == 0), stop=(i == n - 1))
                return ins
            P.op("pe", fn, reads, writes)

        def ACT(out, in_, func, reads, writes, scale=None):
            if scale is None:
                P.op("act", lambda e: e.activation(out=out, in_=in_, func=func), reads, writes)
            else:
                P.op("act", lambda e: e.activation(out=out, in_=in_, func=func, scale=scale), reads, writes)

        def ACP(out, in_, reads, writes, mul=None):
            if mul is None:
                P.op("act", lambda e: e.copy(out=out, in_=in_), reads, writes)
            else:
                P.op("act", lambda e: e.mul(out=out, in_=in_, mul=mul), reads, writes)

        def TT(out, in0, in1, op, reads, writes, eng="dve"):
            P.op(eng, lambda e: e.tensor_tensor(out=out, in0=in0, in1=in1, op=op), reads, writes)

        def TS(out, in0, s1, s2, op0, op1, reads, writes, eng="dve"):
            if op1 is None:
                P.op(eng, lambda e: e.tensor_scalar(out=out, in0=in0, scalar1=s1, scalar2=None, op0=op0), reads, writes)
            else:
                P.op(eng, lambda e: e.tensor_scalar(out=out, in0=in0, scalar1=s1, scalar2=s2, op0=op0, op1=op1), reads, writes)

        def STT(out, in0, scalar, in1, op0, op1, reads, writes):
            P.op("dve", lambda e: e.scalar_tensor_tensor(out=out, in0=in0, scalar=scalar, in1=in1, op0=op0, op1=op1), reads, writes)

        def CP(out, in_, reads, writes, eng="dve"):
            P.op(eng, lambda e: e.tensor_copy(out=out, in_=in_), reads, writes)

        def RCP(out, in_, reads, writes):
            P.op("dve", lambda e: e.reciprocal(out=out, in_=in_), reads, writes)

        def MSET(ap, val, writes, eng="pool"):
            P.op(eng, lambda e: e.memset(ap, val), (), writes)

        def wres(name):
            o, w = WLAY[name]
            return [f"WB{b}" for b in range(o // CB, (o + w - 1) // CB + 1)]

        def WLOAD(dst, name, writes, q="sp"):
            o, w = WLAY[name]
            DMA(dst, WB[:, o:o + w], wres(name), writes, q=q)

        DMA(cst[:], cstd, (), ["cst"])
        DMA(trib[:], tribd, (), ["trib"])
        MSET(ones[:], 1.0, ["ones"])
        MSET(onesf[:], 1.0, ["onesf"])
        for ph in _phase(0):
            stg = [SB(ph, f"stg{i}", [128, CB], F32) for i in range(3)]
            cvt = [SB(ph, f"cvt{i}", [128, CB], BF16) for i in range(3)]
            nblk = (TOTF + CB - 1) // CB
            engs = ("dve", "pool", "act")
            for b in range(nblk):
                s = b % 3
                c0 = b * CB
                w = min(CB, TOTF - c0)
                DMA(stg[s][:, 0:w], wall[:, c0:c0 + w], (), [f"stg{s}"])
                en = engs[b % 3]
                if en == "act":
                    ACP(cvt[s][:, 0:w], stg[s][:, 0:w], [f"stg{s}"], [f"cvt{s}"])
                else:
                    CP(cvt[s][:, 0:w], stg[s][:, 0:w], [f"stg{s}"], [f"cvt{s}"], eng=en)
                DMA(WB[:, c0:c0 + w], cvt[s][:, 0:w], [f"cvt{s}"], [f"WB{b}"], q="pool" if b % 2 else "sp")
            P.flush()
            _chk(0)

        def rmsnorm(tag, X, xr, gcol, out, outr, bufs, n=ST):
            sq, t1, t2 = bufs["sq"], bufs["t1"], bufs["t2"]
            nh = n // 512
            for c in range(8):
                s = c % 2
                ACT(sq[s][:, 0:n], X[:, c, :], AF.Square, [xr(c)], [f"{tag}sq{s}"])
                for hf in range(nh):
                    P.op("pe", (lambda hf=hf, s=s, c=c: (lambda e: e.matmul(bank[hf][:, :], lhsT=ones[:], rhs=sq[s][:, hf * 512:(hf + 1) * 512],
                                                                        start=(c == 0), stop=(c == 7))))(),
                         ["ones", f"{tag}sq{s}"], [f"ps{hf}"])
            for hf in range(nh):
                TS(t1[:, hf * 512:(hf + 1) * 512], bank[hf][:, :], 1.0 / DM, EPS, ALU.mult, ALU.add, [f"ps{hf}"], [f"{tag}t1"])
            ACT(t2[:, 0:n], t1[:, 0:n], AF.Sqrt, [f"{tag}t1"], [f"{tag}t2"])
            RCP(t1[:, 0:n], t2[:, 0:n], [f"{tag}t2"], [f"{tag}t1"])
            for c in range(8):
                STT(out[:, c, :], X[:, c, :], cst[:, gcol + c:gcol + c + 1], t1[:, 0:n], ALU.mult, ALU.mult,
                    [xr(c), "cst", f"{tag}t1"], [outr(c)])

        def ffn(tag, X, xr, gcol, wt, bufs):
            h0, act, sg, wgu, wd = bufs["h0"], bufs["act"], bufs["sg"], bufs["wgu"], bufs["wd"]
            h0r = lambda c: f"{tag}h0_{c}"
            rmsnorm(tag, X, xr, gcol, h0, h0r, bufs)
            h0all = [h0r(c) for c in range(8)]
            WLOAD(wgu[0][:], f"{wt}gu0", [f"{tag}wgu0"])
            it = 0
            for f in range(NFF):
                s = f % 2
                if f + 1 < NFF:
                    WLOAD(wgu[1 - s][:], f"{wt}gu{f + 1}", [f"{tag}wgu{1 - s}"])
                elif True:
                    WLOAD(wd[0][:], f"{wt}d0", [f"{tag}wd0"])
                for hf in range(2):
                    pb = 2 + 2 * (it % 2)
                    it += 1
                    cols = slice(hf * 512, (hf + 1) * 512)
                    w4 = wgu[s][:].rearrange("p (g k c) -> p g k c", g=2, k=8)
                    MMS([(bank[pb][:, :], [(w4[:, 0, k, :], h0[:, k, cols]) for k in range(8)]),
                         (bank[pb + 1][:, :], [(w4[:, 1, k, :], h0[:, k, cols]) for k in range(8)])],
                        [f"{tag}wgu{s}"] + h0all, [f"ps{pb}", f"ps{pb + 1}"])
                    ss_ = it % 2
                    ACT(sg[ss_][:, :], bank[pb][:, :], AF.Silu, [f"ps{pb}"], [f"{tag}sg{ss_}"])
                    TT(act[:, f, cols], sg[ss_][:, :], bank[pb + 1][:, :], ALU.mult, [f"{tag}sg{ss_}", f"ps{pb + 1}"], [f"{tag}act{f}_{hf}"])
            actall = [f"{tag}act{f}_{hf}" for f in range(NFF) for hf in range(2)]
            it = 0
            for o in range(8):
                s = o % 2
                if o + 1 < 8:
                    WLOAD(wd[1 - s][:], f"{wt}d{o + 1}", [f"{tag}wd{1 - s}"])
                w3 = wd[s][:].rearrange("p (f c) -> p f c", f=NFF)
                for hf in range(2):
                    pb = 6 + (it % 2)
                    it += 1
                    cols = slice(hf * 512, (hf + 1) * 512)
                    MMS([(bank[pb][:, :], [(w3[:, f, :], act[:, f, cols]) for f in range(NFF)])],
                        [f"{tag}wd{s}"] + actall, [f"ps{pb}"])
                    STT(X[:, o, cols], bank[pb][:, :], 0.5, X[:, o, cols], ALU.mult, ALU.add, [f"ps{pb}", xr(o)], [xr(o)])

        def ffn_bufs(stk, tag):
            return dict(
                h0=SB(stk, tag + "h0", [128, 8, ST], BF16),
                act=SB(stk, tag + "act", [128, NFF, ST], BF16),
                sq=[SB(stk, f"{tag}sq{i}", [128, ST], BF16) for i in range(2)],
                t1=SB(stk, tag + "t1", [128, ST], F32),
                t2=SB(stk, tag + "t2", [128, ST], F32),
                sg=[SB(stk, f"{tag}sg{i}", [128, 512], F32) for i in range(2)],
                wgu=[SB(stk, f"{tag}wgu{i}", [128, 2048], BF16) for i in range(2)],
                wd=[SB(stk, f"{tag}wd{i}", [128, NFF * 128], BF16) for i in range(2)],
            )

        for ph in _phase(1):
            bufs = ffn_bufs(ph, "a")
            Xb = [SB(ph, f"aX{i}", [128, 8, ST], F32) for i in range(2)]
            nst = WIN // ST
            DMA(Xb[0][:], xwr[:, :, 0:ST], (), [f"aX0_{c}" for c in range(8)])
            for st in range(nst):
                s = st % 2
                X = Xb[s]
                xr = (lambda s: (lambda c: f"aX{s}_{c}"))(s)
                if st + 1 < nst:
                    DMA(Xb[1 - s][:], xwr[:, :, (st + 1) * ST:(st + 2) * ST], (), [f"aX{1 - s}_{c}" for c in range(8)])
                ffn("a", X, xr, C_G1, "f1", bufs)
                if st * ST >= OWN0:
                    DMA(X1r[:, :, st * ST - OWN0:(st + 1) * ST - OWN0], X[:], [xr(c) for c in range(8)], [f"X1S{st}"])
                h0r = lambda c: f"ah0_{c}"
                rmsnorm("a", X, xr, C_GM, bufs["h0"], h0r, bufs)
                DMA(HSr[:, :, st * ST:(st + 1) * ST], bufs["h0"][:], [h0r(c) for c in range(8)], [f"HS{st}"])
            P.flush()
            _chk(1)

        with ExitStack() as mid:
            YA = SB(mid, "YA", [64, 4, NOWN], BF16)
            for ph in _phase(2):
                hb = SB(ph, "d_hb", [128, 8, 2048], BF16)
                ACC = SB(ph, "d_acc", [128, 4, NOWN], F32)
                QT = [SB(ph, f"d_qt{i}", [128, 2048], BF16) for i in range(2)]
                KT = [SB(ph, f"d_kt{i}", [128, 2048 + 128 * 16], BF16) for i in range(2)]
                VA = SB(ph, "d_va", [128, 32, 4, 65], BF16)
                wq = [SB(ph, f"d_wq{i}", [128, 8, 128], BF16) for i in range(2)]
                wk = [SB(ph, f"d_wk{i}", [128, 8, 128], BF16) for i in range(2)]
                wv = SB(ph, "d_wv", [128, 8, 256], BF16)
                et = [SB(ph, f"d_et{i}", [128, 2, 2, 128], F32) for i in range(2)]
                PT = [SB(ph, f"d_pt{i}", [128, 2, 2, 128], BF16) for i in range(4)]
                rd = SB(ph, "d_rd", [128, 512], F32)
                dec = cst[:, C_DEC:C_DEC + 3072].rearrange("p (h a b) -> p h a b", h=12, a=2)
                pcnt = [0]

                def nbank():
                    pcnt[0] += 1
                    return pcnt[0] % 4

                for g, D in enumerate(DIL):
                    npr = NOWN // D
                    nq = 16 // D
                    KW = npr + 128
                    ntile = D * (nq + 1)
                    for pr in range(2):
                        WLOAD(wq[pr][:].rearrange("p k c -> p (k c)"), f"dq{g}{pr}", [f"d_wq{pr}"])
                        WLOAD(wk[pr][:].rearrange("p k c -> p (k c)"), f"dk{g}{pr}", [f"d_wk{pr}"])
                    WLOAD(wv[:].rearrange("p k c -> p (k c)"), f"dv{g}", ["d_wv"])
                    for hd in range(4):
                        CP(VA[:, 0:ntile, hd, 64], cst[:, C_VALD + VALD_OFF[g]:C_VALD + VALD_OFF[g] + ntile], ["cst"], ["d_va"], eng="pool")
                    for half in range(2):
                        hs_res = ["HS4", "HS5"] if half == 0 else ["HS6", "HS7"]
                        DMA(hb[:], HSr[:, :, 4096 + 2048 * half:4096 + 2048 * (half + 1)], hs_res, ["d_hb"])
                        if half == 0:
                            col0 = lambda r: 2048 - 128 * D + r
                            ncol = 128
                            kdst0 = 0
                        else:
                            col0 = lambda r: r
                            ncol = npr
                            kdst0 = 128
                        for pr in range(2):
                            rpb = max(1, 512 // ncol)
                            for r0 in range(0, D, rpb):
                                rs = list(range(r0, min(D, r0 + rpb)))
                                for c0 in range(0, ncol, 512):
                                    cn = min(512, ncol - c0)
                                    targets = [("k", wk[pr], KT[pr])] + ([("q", wq[pr], QT[pr])] if half == 1 else [])
                                    for kind, wt_, dst in targets:
                                        b = nbank()
                                        groups = []
                                        for ri, r in enumerate(rs):
                                            groups.append((bank[b][:, ri * cn:(ri + 1) * cn],
                                                           [(wt_[:, k, :], hb[:, k, ss(col0(r) + D * c0, cn, D)]) for k in range(8)]))
                                        MMS(groups, ["d_hb", f"d_w{kind}{pr}"], [f"ps{b}"])
                                        for ri, r in enumerate(rs):
                                            src = bank[b][:, ri * cn:(ri + 1) * cn]
                                            if kind == "k":
                                                CP(KT[pr][:, r * KW + kdst0 + c0:r * KW + kdst0 + c0 + cn], src, [f"ps{b}"], [f"d_kt{pr}"])
                                            else:
                                                ACP(QT[pr][:, r * npr + c0:r * npr + c0 + cn], src, [f"ps{b}"], [f"d_qt{pr}"], mul=0.125)
                        jts = [0] if half == 0 else list(range(1, nq + 1))
                        for r in range(D):
                            for jt in jts:
                                c_first = (2048 - 128 * D + r) if half == 0 else (r + D * 128 * (jt - 1))
                                b = nbank()
                                MMS([(bank[b][:, 0:256], [(hb[:, k, ss(c_first, 128, D)], wv[:, k, :]) for k in range(8)])],
                                    ["d_hb", "d_wv"], [f"ps{b}"])
                                t_ = r * (nq + 1) + jt
                                src = bank[b][:, 0:256].rearrange("p (h d) -> p h d", h=4)
                                if (r + jt) % 2:
                                    ACP(VA[:, t_, :, 0:64], src, [f"ps{b}"], ["d_va"])
                                else:
                                    CP(VA[:, t_, :, 0:64], src, [f"ps{b}"], ["d_va"])
                    gcount = 0
                    for pr in range(2):
                        ob = [4 + 2 * pr, 5 + 2 * pr]
                        qslot = 0
                        ptc = 0
                        prev_pt = None
                        for r in range(D):
                            for jt in range(nq + 1):
                                halves = [1] if jt == 0 else ([0] if jt == nq else [0, 1])
                                h0_, h1_ = halves[0], halves[-1] + 1
                                qc0 = r * npr + 128 * (jt - 1 + h0_)
                                nqc = 128 * len(halves)
                                sb0 = 2 * (gcount % 2)
                                sv = [bank[sb0 + e_][:, 0:256].rearrange("p (a b) -> p a b", a=2) for e_ in range(2)]
                                kc = r * KW + 128 * jt
                                MMS([(sv[e_][:, h0_:h1_, :], [(KT[pr][64 * e_:64 * e_ + 64, kc:kc + 128], QT[pr][64 * e_:64 * e_ + 64, qc0:qc0 + nqc])])
                                     for e_ in range(2)], [f"d_kt{pr}", f"d_qt{pr}"], [f"ps{sb0}", f"ps{sb0 + 1}"])
                                es = gcount % 2
                                ps_ = ptc % 4
                                ptc += 1
                                gcount += 1
                                for e_ in range(2):
                                    ACT(et[es][:, e_, h0_:h1_, :], sv[e_][:, h0_:h1_, :], AF.Exp, [f"ps{sb0 + e_}"], [f"d_et{es}_{e_}"])
                                hh0 = 4 * g + 2 * pr
                                TT(PT[ps_][:, :, h0_:h1_, :], et[es][:, :, h0_:h1_, :], dec[:, hh0:hh0 + 2, h0_:h1_, :], ALU.mult,
                                   [f"d_et{es}_0", f"d_et{es}_1", "cst"], [f"d_pt{ps_}"])
                                if jt >= 1:
                                    qt_ = jt - 1
                                    t_prev = r * (nq + 1) + jt - 1
                                    t_cur = r * (nq + 1) + jt
                                    pp = prev_pt
                                    groups = []
                                    for e_ in range(2):
                                        hd = 2 * pr + e_
                                        groups.append((bank[ob[e_]][0:65, qslot * 128:(qslot + 1) * 128],
                                                       [(VA[:, t_prev, hd, :], PT[pp][:, e_, 1, :]), (VA[:, t_cur, hd, :], PT[ps_][:, e_, 0, :])]))
                                    MMS(groups, ["d_va", f"d_pt{pp}", f"d_pt{ps_}"], [f"ps{ob[0]}", f"ps{ob[1]}"])
                                    qslot += 1
                                    if qslot == 4:
                                        qslot = 0
                                        for e_ in range(2):
                                            hd = 2 * pr + e_
                                            if D == 1:
                                                m = qt_ // 4
                                                dst = ACC[0:65, hd, m * 512:(m + 1) * 512]
                                                src = bank[ob[e_]][0:65, :]
                                            elif D == 4:
                                                dst = ACC[0:65, hd, ss(r, 512, 4)]
                                                src = bank[ob[e_]][0:65, :]
                                            else:
                                                r0 = r - 3
                                                dst = ACC[0:65, hd, :].rearrange("p (i r) -> p r i", r=16)[:, r0:r0 + 4, :]
                                                src = bank[ob[e_]][0:65, :].rearrange("p (r i) -> p r i", r=4)
                                            if g == 0:
                                                CP(dst, src, [f"ps{ob[e_]}"], [f"d_acc{hd}"])
                                            else:
                                                TT(dst, src, dst, ALU.add, [f"ps{ob[e_]}", f"d_acc{hd}"], [f"d_acc{hd}"])
                                prev_pt = ps_
                for hd in range(4):
                    for m in range(4):
                        cols = slice(m * 512, (m + 1) * 512)
                        RCP(rd[64:65, :], ACC[64:65, hd, cols], [f"d_acc{hd}"], ["d_rd"])
                        b = nbank()
                        MMS([(bank[b][0:64, :], [(onesf[64:65, 0:64], rd[64:65, :])])], ["onesf", "d_rd"], [f"ps{b}"])
                        TT(YA[0:64, hd, cols], ACC[0:64, hd, cols], bank[b][0:64, :], ALU.mult, [f"d_acc{hd}", f"ps{b}"], [f"YA{hd}"])
                if DBG[0]:
                    DMA(YAD, YA[:], [f"YA{hd}" for hd in range(4)], ["YAD"])
                P.flush()
                _chk(2)

            YB = SB(mid, "YB", [64, 8, NOWN], BF16)
            for ph in _phase(3):
                KAB = [SB(ph, f"m_k{i}", [128, WIN], BF16) for i in range(2)]
                QAB = [SB(ph, f"m_q{i}", [128, NOWN], BF16) for i in range(2)]
                VAB = SB(ph, "m_v", [128, 64, 2, 65], BF16)
                hbuf = [SB(ph, f"m_hb{i}", [128, 8, 512], BF16) for i in range(2)]
                wq = SB(ph, "m_wq", [128, 8, 128], BF16)
                wk = SB(ph, "m_wk", [128, 8, 128], BF16)
                wv = SB(ph, "m_wv", [128, 8, 128], BF16)
                QF = SB(ph, "m_qf", [128, 512], F32)
                KM = SB(ph, "m_km", [128, 32], F32)
                g2 = SB(ph, "m_g2", [128, 2, 32], F32)
                mx = SB(ph, "m_mx", [128, 2, 8], F32)
                thr = SB(ph, "m_thr", [128, 2, 1], F32)
                sel = SB(ph, "m_sel", [128, 2, 32], F32)
                ZA = SB(ph, "m_za", [128, 96], F32)
                ZB = SB(ph, "m_zb", [128, 32], F32)
                PTm = [SB(ph, f"m_pt{i}", [128, 512], BF16) for i in range(4)]
                osb = SB(ph, "m_osb", [128, 512], F32)
                rdm = SB(ph, "m_rd", [128, 512], F32)
                erow = (slice(64, 96), slice(0, 32))
                arow = (slice(96, 100), slice(32, 36))
                drow = (slice(0, 64), slice(64, 128))
                krows = (slice(0, 100), slice(0, 128))
                if "S1" not in SUB[0]:
                    MSET(KAB[1][32:64, :], 0.0, ["m_k1x"])
                    MSET(QAB[1][32:64, :], 0.0, ["m_q1x"])
                MSET(ZA[:], 0.0, ["m_za"])
                MSET(KM[:], 0.0, ["m_km"])
                if "S2" not in SUB[0]:
                    DMA(KAB[0][64:96, :], etabd, (), ["m_k0e"])
                    DMA(KAB[1][0:32, :], etabd, (), ["m_k1e"])
                for e_ in (range(2) if "S3" not in SUB[0] else ()):
                    CP(VAB[:, :, e_, 64], cst[:, C_VALM:C_VALM + 64], ["cst"], ["m_vval"], eng="pool")
                sring = [0]
                for i in (range(4) if "P1" not in SUB[0] else range(1)):
                    WLOAD(wq[:].rearrange("p k c -> p (k c)"), f"mq{i}", ["m_wq"])
                    WLOAD(wk[:].rearrange("p k c -> p (k c)"), f"mk{i}", ["m_wk"])
                    WLOAD(wv[:].rearrange("p k c -> p (k c)"), f"mv{i}", ["m_wv"])
                    for e_ in (range(2) if "L" not in SUB[0] else ()):
                        DMA(KAB[e_][arow[e_], :], aktd[2 * i + e_], ["m_k1x"], [f"m_k{e_}a"])
                        DMA(QAB[e_][arow[e_], :], aqtd[2 * i + e_], ["m_q1x"], [f"m_q{e_}a"])
                    DMA(hbuf[0][:], HSr[:, :, 0:512], ["HS0"], ["m_hb0"])
                    for s2 in range(16):
                        hs = s2 % 2
                        H_ = hbuf[hs]
                        if s2 + 1 < 16:
                            DMA(hbuf[1 - hs][:], HSr[:, :, (s2 + 1) * 512:(s2 + 2) * 512], [f"HS{(s2 + 1) // 2}"], [f"m_hb{1 - hs}"])
                        cols = slice(s2 * 512, (s2 + 1) * 512)
                        if "K" not in SUB[0]:
                          MMS([(bank[6][:, :], [(wk[:, k, :], H_[:, k, :]) for k in range(8)])], [f"m_hb{hs}", "m_wk"], ["ps6"])
                        ACP(KAB[0][0:64, cols], bank[6][0:64, :], ["ps6"], ["m_k0d"])
                        CP(KAB[1][64:128, cols], bank[6][64:128, :], ["ps6"], ["m_k1d"])
                        P.op("dve", (lambda s2=s2: (lambda e: e.tensor_reduce(out=KM[:, 2 * s2:2 * s2 + 2],
                                                                              in_=bank[6][:, :].rearrange("p (a b) -> p a b", a=2),
                                                                              axis=AX.X, op=ALU.add)))(), ["ps6"], ["m_km"])
                        MMS([(bank[7][:, tt * 128:(tt + 1) * 128], [(H_[:, k, tt * 128:(tt + 1) * 128], wv[:, k, :]) for k in range(8)])
                             for tt in range(4)], [f"m_hb{hs}", "m_wv"], ["ps7"])
                        v4 = bank[7][:, :].rearrange("p (t e d) -> p t e d", t=4, e=2)
                        ACP(VAB[:, 4 * s2:4 * s2 + 4, 0, 0:64], v4[:, :, 0, :], ["ps7"], ["m_v0"])
                        if "T0" not in SUB[0]:
                            ACP(VAB[:, 4 * s2:4 * s2 + 4, 1, 0:64], v4[:, :, 1, :], ["ps7"], ["m_v1"])
                        elif "T2" in SUB[0]:
                            TS(VAB[:, 4 * s2:4 * s2 + 4, 1, 0:64], v4[:, :, 1, :], 1.0, None, ALU.mult, None, ["ps7"], ["m_v1"])
                        elif "T3" in SUB[0]:
                            for tt_ in range(4):
                                CP(VAB[:, 4 * s2 + tt_, 1, 0:64], v4[:, tt_, 1, :], ["ps7"], ["m_v1"])
                        else:
                            CP(VAB[:, 4 * s2:4 * s2 + 4, 1, 0:64], v4[:, :, 1, :], ["ps7"], ["m_v1"])
                        if s2 >= 12:
                            qi = s2 - 12
                            qcols = slice(qi * 512, (qi + 1) * 512)
                            MMS([(bank[5][:, :], [(wq[:, k, :], H_[:, k, :]) for k in range(8)])], [f"m_hb{hs}", "m_wq"], ["ps5"])
                            ACP(QAB[0][0:64, qcols], bank[5][0:64, :], ["ps5"], ["m_q0d"], mul=0.125)
                            TS(QAB[1][64:128, qcols], bank[5][64:128, :], 0.125, None, ALU.mult, None, ["ps5"], ["m_q1d"])
                            ACP(QF[:, :], bank[5][:, :], ["ps5"], ["m_qf"], mul=0.125)
                            for sub in (range(4) if "G" not in SUB[0] else ()):
                                qt16 = 4 * qi + sub
                                sc = slice(sub * 128, (sub + 1) * 128)
                                MMS([(bank[4 + e_][:, 0:32], [(QF[drow[e_], sc], KM[drow[e_], :])]) for e_ in range(2)],
                                    ["m_qf", "m_km"], ["ps4", "ps5"])
                                for e_ in range(2):
                                    TT(g2[:, e_, :], bank[4 + e_][:, 0:32], cst[:, C_GMASK + 32 * qt16:C_GMASK + 32 * qt16 + 32],
                                       ALU.add, [f"ps{4 + e_}", "cst"], ["m_g2"])
                                for e_ in range(2):
                                    P.op("dve", (lambda e_=e_: (lambda e: e.max(out=mx[:, e_, :], in_=g2[:, e_, :])))(), ["m_g2"], ["m_mx"])
                                TS(thr[:, :, :], mx[:, :, 2:3], -1.0e29, None, ALU.max, None, ["m_mx"], ["m_thr"])
                                for e_ in range(2):
                                    TS(sel[:, e_, :], g2[:, e_, :], thr[:, e_, :], None, ALU.is_ge, None, ["m_g2", "m_thr"], ["m_sel"])
                                    TT(sel[:, e_, :], sel[:, e_, :], cst[:, C_OWNHOT + 32 * qt16:C_OWNHOT + 32 * qt16 + 32], ALU.add,
                                       ["m_sel", "cst"], ["m_sel"])
                                TS(ZA[:, 64:96], sel[:, 0, :], BIG, -BIG, ALU.mult, ALU.add, ["m_sel"], ["m_za"])
                                TS(ZB[:, :], sel[:, 1, :], BIG, -BIG, ALU.mult, ALU.add, ["m_sel"], ["m_zb"])
                                P.op("pe", (lambda: (lambda e: e.transpose(bank[3][0:96, 0:128], ZA[:], cst[:, C_ID:C_ID + 128])))(),
                                     ["m_za", "cst"], ["ps3"])
                                P.op("pe", (lambda: (lambda e: e.transpose(bank[3][0:32, 128:256], ZB[:], cst[:, C_ID:C_ID + 128])))(),
                                     ["m_zb", "cst"], ["ps3"])
                                qc = slice(qi * 512 + sub * 128, qi * 512 + (sub + 1) * 128)
                                ACP(QAB[0][64:96, qc], bank[3][64:96, 0:128], ["ps3"], ["m_q0n"])
                                ACP(QAB[1][0:32, qc], bank[3][0:32, 128:256], ["ps3"], ["m_q1n"])
                    for e_ in (range(2) if "A" not in SUB[0] else ()):
                        h = 2 * i + e_
                        kres = [f"m_k{e_}d", f"m_k{e_}a", f"m_k{e_}e", "m_k1x"]
                        qres = [f"m_q{e_}d", f"m_q{e_}a", f"m_q{e_}n", "m_q1x"]
                        rows = krows[e_]
                        for qi in range(4):
                            ob_ = 3 + 0
                            ob_ = 4 if (qi % 2 == 0) else 5
                            nfull = 48 + 4 * qi
                            nk = nfull + 4
                            for kt in range(nk):
                                di = kt - nfull
                                c0 = 128 * di if di >= 0 else 0
                                sb_ = sring[0] % 3
                                pt_ = sring[0] % 4
                                sring[0] += 1
                                MMS([(bank[sb_][:, c0:512], [(KAB[e_][rows, kt * 128:(kt + 1) * 128], QAB[e_][rows, qi * 512 + c0:(qi + 1) * 512])])],
                                    kres + qres, [f"ps{sb_}"])
                                ACT(PTm[pt_][:, c0:512], bank[sb_][:, c0:512], AF.Exp, [f"ps{sb_}"], [f"m_pt{pt_}"])
                                if di >= 0:
                                    TT(PTm[pt_][:, c0:c0 + 128], PTm[pt_][:, c0:c0 + 128], trib[:, :], ALU.mult, [f"m_pt{pt_}", "trib"], [f"m_pt{pt_}"],
                                       eng="pool")
                                P.op("pe", (lambda kt=kt, c0=c0, pt_=pt_, ob_=ob_, nk=nk, e_=e_:
                                            (lambda e: e.matmul(bank[ob_][0:65, c0:512], lhsT=VAB[:, kt, e_, :], rhs=PTm[pt_][:, c0:512],
                                                                start=(kt == 0), stop=(kt == nk - 1))))(),
                                     [f"m_v{e_}", "m_vval", f"m_pt{pt_}"], [f"ps{ob_}"])
                            ACP(osb[0:65, :], bank[ob_][0:65, :], [f"ps{ob_}"], ["m_osb"])
                            RCP(rdm[64:65, :], osb[64:65, :], ["m_osb"], ["m_rd"])
                            MMS([(bank[7][0:64, :], [(onesf[64:65, 0:64], rdm[64:65, :])])], ["onesf", "m_rd"], ["ps7"])
                            TT(YB[0:64, h, qi * 512:(qi + 1) * 512], osb[0:64, :], bank[7][0:64, :], ALU.mult, ["m_osb", "ps7"], [f"YB{h}"])
                if DBG[0]:
                    DMA(YBD, YB[:], [f"YB{h}" for h in range(8)], ["YBD"])
                P.flush()
                _chk(3)

            for ph in _phase(4):
                hown = [SB(ph, f"g_h{i}", [128, 8, ST], BF16) for i in range(2)]
                mg = SB(ph, "g_mg", [128, 8, ST], BF16)
                wga = [SB(ph, f"g_wga{i}", [128, 8, 128], BF16) for i in range(2)]
                wgb = [SB(ph, f"g_wgb{i}", [128, 8, 128], BF16) for i in range(2)]
                wua = [SB(ph, f"g_wua{i}", [128, 4, 128], BF16) for i in range(2)]
                wub = [SB(ph, f"g_wub{i}", [128, 8, 128], BF16) for i in range(2)]
                sga = [SB(ph, f"g_sa{i}", [128, 512], F32) for i in range(2)]
                sgb = [SB(ph, f"g_sb{i}", [128, 512], F32) for i in range(2)]
                yall = [f"YA{hd}" for hd in range(4)] + [f"YB{h}" for h in range(8)]
                if 2 not in RUN[0]:
                    DMA(YA[:], YAD, (), [f"YA{hd}" for hd in range(4)])
                if 3 not in RUN[0]:
                    DMA(YB[:], YBD, (), [f"YB{h}" for h in range(8)])
                it = 0
                for stl in range(2):
                    H_ = hown[stl]
                    DMA(H_[:], HSr[:, :, OWN0 + stl * ST:OWN0 + (stl + 1) * ST], [f"HS{6 + stl}"], [f"g_h{stl}"])
                    for o in range(8):
                        s = o % 2
                        WLOAD(wga[s][:].rearrange("p k c -> p (k c)"), f"ga{o}", [f"g_wga{s}"])
                        WLOAD(wgb[s][:].rearrange("p k c -> p (k c)"), f"gb{o}", [f"g_wgb{s}"])
                        WLOAD(wua[s][:].rearrange("p k c -> p (k c)"), f"ua{o}", [f"g_wua{s}"])
                        WLOAD(wub[s][:].rearrange("p k c -> p (k c)"), f"ub{o}", [f"g_wub{s}"])
                        for hf in range(2):
                            b0 = 4 * (it % 2)
                            ts_ = it % 2
                            it += 1
                            lc = slice(hf * 512, (hf + 1) * 512)
                            gc = slice(stl * ST + hf * 512, stl * ST + (hf + 1) * 512)
                            MMS([(bank[b0][:, :], [(wua[s][0:64, hd, :], YA[0:64, hd, gc]) for hd in range(4)]),
                                 (bank[b0 + 1][:, :], [(wub[s][0:64, h, :], YB[0:64, h, gc]) for h in range(8)]),
                                 (bank[b0 + 2][:, :], [(wga[s][:, k, :], H_[:, k, lc]) for k in range(8)]),
                                 (bank[b0 + 3][:, :], [(wgb[s][:, k, :], H_[:, k, lc]) for k in range(8)])],
                                yall + [f"g_h{stl}", f"g_wga{s}", f"g_wgb{s}", f"g_wua{s}", f"g_wub{s}"],
                                [f"ps{b0 + q_}" for q_ in range(4)])
                            ACT(sga[ts_][:, :], bank[b0 + 2][:, :], AF.Sigmoid, [f"ps{b0 + 2}"], [f"g_sa{ts_}"])
                            ACT(sgb[ts_][:, :], bank[b0 + 3][:, :], AF.Sigmoid, [f"ps{b0 + 3}"], [f"g_sb{ts_}"])
                            TT(sga[ts_][:, :], sga[ts_][:, :], bank[b0][:, :], ALU.mult, [f"g_sa{ts_}", f"ps{b0}"], [f"g_sa{ts_}"])
                            TT(sgb[ts_][:, :], sgb[ts_][:, :], bank[b0 + 1][:, :], ALU.mult, [f"g_sb{ts_}", f"ps{b0 + 1}"], [f"g_sb{ts_}"])
                            TT(mg[:, o, lc], sga[ts_][:, :], sgb[ts_][:, :], ALU.add, [f"g_sa{ts_}", f"g_sb{ts_}"], [f"g_mg{o}"], eng="pool")
                    DMA(MSr[:, :, stl * ST:(stl + 1) * ST], mg[:], [f"g_mg{o}" for o in range(8)], [f"MS{stl}"])
                P.flush()
                _chk(4)

        for ph in _phase(5):
            bufs = ffn_bufs(ph, "b")
            X = SB(ph, "bX", [128, 8, ST], F32)
            mgt = SB(ph, "b_mg", [128, 8, ST], BF16)
            wo = [SB(ph, f"b_wo{i}", [128, 8, 128], BF16) for i in range(2)]
            xr = lambda c: f"bX_{c}"
            for stl in range(2):
                DMA(X[:], X1r[:, :, stl * ST:(stl + 1) * ST], [f"X1S{6 + stl}"], [xr(c) for c in range(8)])
                DMA(mgt[:], MSr[:, :, stl * ST:(stl + 1) * ST], [f"MS{stl}"], ["b_mg"])
                it = 0
                for o2 in range(8):
                    s = o2 % 2
                    WLOAD(wo[s][:].rearrange("p k c -> p (k c)"), f"wo{o2}", [f"b_wo{s}"])
                    for hf in range(2):
                        pb = 6 + (it % 2)
                        it += 1
                        cols = slice(hf * 512, (hf + 1) * 512)
                        MMS([(bank[pb][:, :], [(wo[s][:, o, :], mgt[:, o, cols]) for o in range(8)])], ["b_mg", f"b_wo{s}"], [f"ps{pb}"])
                        TT(X[:, o2, cols], bank[pb][:, :], X[:, o2, cols], ALU.add, [f"ps{pb}", xr(o2)], [xr(o2)])
                ffn("b", X, xr, C_G2, "f2", bufs)
                rmsnorm("b", X, xr, C_GF, X, xr, bufs)
                DMA(outr[:, :, stl * ST:(stl + 1) * ST], X[:], [xr(c) for c in range(8)], [f"OUT{stl}"])
            P.flush()
            _chk(5)
    return nc


_NC_CACHE = {}


def kernel(x, norm_ffn1, ffn1_gate, ffn1_up, ffn1_down, norm_mix, w_in, w_up_a, w_up_b, w_out,
           norm_ffn2, ffn2_gate, ffn2_up, ffn2_down, norm_final):
    inp = dict(x=x, norm_ffn1=norm_ffn1, ffn1_gate=ffn1_gate, ffn1_up=ffn1_up, ffn1_down=ffn1_down,
               norm_mix=norm_mix, w_in=w_in, w_up_a=w_up_a, w_up_b=w_up_b, w_out=w_out, norm_ffn2=norm_ffn2,
               ffn2_gate=ffn2_gate, ffn2_up=ffn2_up, ffn2_down=ffn2_down, norm_final=norm_final)
    inp = {k: np.asarray(v, dtype=np.float32) for k, v in inp.items()}
    wall = _pack_weights(inp)
    in_maps = []
    for c in range(8):
        b, j = c // 4, c % 4
        end = 2048 * (j + 1)
        xwin = np.zeros((WIN, DM), np.float32)
        xwin[WIN - end:] = inp["x"][b, 0:end]
        xw = np.ascontiguousarray(xwin.T).reshape(8, 128, WIN)
        cst, etab, akt, aqt, trib = _const_tables(inp, j)
        in_maps.append(dict(xw=xw, wall=wall, cst=cst, etab=etab, akt=akt, aqt=aqt, trib=trib))
    if "nc" not in _NC_CACHE:
        _NC_CACHE["nc"] = build()
    res = run_bass_kernel_spmd(_NC_CACHE["nc"], in_maps, core_ids=list(range(8)))
    if DBG[0]:
        LAST["res"] = res.results
    out = np.zeros((2, SEQ, DM), np.float32)
    for c in range(8):
        b, j = c // 4, c % 4
        o = np.asarray(res.results[c]["outT"], np.float32).reshape(DM, NOWN)
        out[b, 2048 * j:2048 * (j + 1), :] = o.T
    return out
```

```python
from contextlib import ExitStack
import numpy as np
import ml_dtypes
import concourse.bass as bass
import concourse.mybir as mybir
from concourse.bass_utils import run_bass_kernel_spmd

F32 = mybir.dt.float32
BF16 = mybir.dt.bfloat16
ALU = mybir.AluOpType
AF = mybir.ActivationFunctionType
AX = mybir.AxisListType

DM = 1024
SEQ = 8192
DFF = 2816
NFF = 22
WIN = 8192
OWN0 = 6144
NOWN = 2048
ST = 1024
EPS = 1e-6
BIG = 30000.0
NEGG = -1.0e30
DIL = (1, 4, 16)
CB = 4096

ENGS = ("pe", "act", "dve", "pool", "sp")
BUDGET = [None]


class Prog:
    def __init__(self, nc, stack, n_dma_slots=8):
        self.nc = nc
        self.sem = {e: stack.enter_context(nc.semaphore("sem_" + e)) for e in ("pe", "act", "dve", "pool")}
        self.base = {e: 0 for e in self.sem}
        self.dsem = {}
        self.dcnt = {}
        for q in ("sp", "pool", "act"):
            self.dsem[q] = [stack.enter_context(nc.semaphore(f"dma_{q}_{i}")) for i in range(n_dma_slots if q == "sp" else 4)]
            self.dcnt[q] = [0] * len(self.dsem[q])
        self.drr = {q: 0 for q in self.dsem}
        self._reset()

    def _reset(self):
        self.idx = {e: 0 for e in self.sem}
        self.ops = {e: [] for e in ENGS}
        self.last_w = {}
        self.readers = {}
        self.waited = {e: {} for e in ENGS}
        self.targets = {e: set() for e in self.sem}

    def _need(self, eng, tok, waits):
        if tok is None:
            return
        key, val = tok[0], tok[-1]
        if key == eng and eng == "pe":
            return
        if self.waited[eng].get(key, -1) >= val:
            return
        self.waited[eng][key] = val
        waits.append(tok)
        if key in self.targets:
            self.targets[key].add(val)

    def _deps(self, eng, reads, writes):
        waits = []
        for r in reads:
            self._need(eng, self.last_w.get(r), waits)
            if r.startswith("ps"):
                for t in self.readers.get(r, ()):
                    if t[0] != eng:
                        self._need(eng, t, waits)
        for w in writes:
            self._need(eng, self.last_w.get(w), waits)
            for t in self.readers.get(w, ()):
                self._need(eng, t, waits)
        return waits

    def _commit(self, tok, reads, writes):
        for r in reads:
            self.readers.setdefault(r, []).append(tok)
        for w in writes:
            self.last_w[w] = tok
            self.readers[w] = []

    def op(self, eng, fn, reads=(), writes=()):
        if BUDGET[0] is not None:
            BUDGET[0] -= 1
            if BUDGET[0] < 0:
                return
        waits = self._deps(eng, reads, writes)
        self.idx[eng] += 1
        tok = (eng, self.idx[eng])
        self.ops[eng].append((waits, fn, tok))
        self._commit(tok, reads, writes)

    def dma(self, fn, reads=(), writes=(), q="sp"):
        if BUDGET[0] is not None:
            BUDGET[0] -= 1
            if BUDGET[0] < 0:
                return
        waits = self._deps(q, reads, writes)
        i = self.drr[q]
        self.drr[q] = (i + 1) % len(self.dsem[q])
        key = f"d{q}{i}"
        prev = self.dcnt[q][i]
        if prev > 0 and self.waited[q].get(key, -1) < prev:
            self.waited[q][key] = prev
            waits.append((key, q, i, prev))
        self.dcnt[q][i] = prev + 16
        tok = (key, q, i, prev + 16)
        self.ops[q].append((waits, fn, tok))
        self._commit(tok, reads, writes)

    def flush(self):
        nc = self.nc
        for q in self.dsem:
            waits = []
            for i in range(len(self.dsem[q])):
                v = self.dcnt[q][i]
                key = f"d{q}{i}"
                if v > 0 and self.waited[q].get(key, -1) < v:
                    self.waited[q][key] = v
                    waits.append((key, q, i, v))
            if waits:
                self.ops[q].append((waits, None, None))
        ops = self.ops
        rank = {}
        for e in self.sem:
            for r_, ix in enumerate(sorted(self.targets[e])):
                rank[(e, ix)] = self.base[e] + r_ + 1
        sem, dsem = self.sem, self.dsem

        def resolve(tok):
            if len(tok) == 2:
                return sem[tok[0]], rank[tok]
            return dsem[tok[1]][tok[2]], tok[3]

        def replay(h, lst):
            for waits, fn, tok in lst:
                for w in waits:
                    s_, v_ = resolve(w)
                    h.wait_ge(s_, v_)
                if fn is not None:
                    ins = fn(h)
                    if len(tok) == 4:
                        ins.then_inc(dsem[tok[1]][tok[2]], 16)
                    elif tok in rank:
                        ins.then_inc(sem[tok[0]], 1)

        with nc.Block() as block:
            if ops["sp"]:
                @block.sync
                def _(e):
                    replay(e, ops["sp"])
            if ops["pe"]:
                @block.tensor
                def _(e):
                    replay(e, ops["pe"])
            if ops["act"]:
                @block.scalar
                def _(e):
                    replay(e, ops["act"])
            if ops["dve"]:
                @block.vector
                def _(e):
                    replay(e, ops["dve"])
            if ops["pool"]:
                @block.gpsimd
                def _(e):
                    replay(e, ops["pool"])
        for e in self.sem:
            self.base[e] += len(self.targets[e])
        self._reset()


def ss(start, n, step):
    return slice(start, start + (n - 1) * step + 1, step)


def _weight_layout():
    lay = {}
    off = 0

    def add(name, w):
        nonlocal off
        lay[name] = (off, w)
        off += w

    for t in ("f1", "f2"):
        for f in range(NFF):
            add(f"{t}gu{f}", 2 * 8 * 128)
        for o in range(8):
            add(f"{t}d{o}", NFF * 128)
    for g in range(3):
        for pr in range(2):
            add(f"dq{g}{pr}", 1024)
            add(f"dk{g}{pr}", 1024)
        add(f"dv{g}", 2048)
    for i in range(4):
        add(f"mq{i}", 1024)
        add(f"mk{i}", 1024)
        add(f"mv{i}", 1024)
    for o in range(8):
        add(f"ga{o}", 1024)
        add(f"gb{o}", 1024)
    for o in range(8):
        add(f"ua{o}", 512)
        add(f"ub{o}", 1024)
    for o in range(8):
        add(f"wo{o}", 1024)
    return lay, off


WLAY, TOTF = _weight_layout()

C_G1, C_GM, C_G2, C_GF = 0, 8, 16, 24
C_ID = 32
C_GMASK = C_ID + 128
C_OWNHOT = C_GMASK + 512
C_VALM = C_OWNHOT + 512
C_VALD = C_VALM + 64
C_DEC = C_VALD + 69
CF = C_DEC + 12 * 256
VALD_OFF = (0, 17, 37)


def _kchunks(w, c0, width):
    return np.ascontiguousarray(w[:, c0:c0 + width].reshape(8, 128, width).transpose(1, 0, 2)).reshape(128, 8 * width)


def _pack_weights(inp):
    wall = np.zeros((128, TOTF), np.float32)

    def put(name, arr):
        o, w = WLAY[name]
        assert arr.shape == (128, w), (name, arr.shape, w)
        wall[:, o:o + w] = arr

    for t, gk, uk, dk in (("f1", "ffn1_gate", "ffn1_up", "ffn1_down"), ("f2", "ffn2_gate", "ffn2_up", "ffn2_down")):
        wg, wu, wd = inp[gk][0], inp[uk][0], inp[dk][0]
        for f in range(NFF):
            a = np.stack([wg[:, f * 128:(f + 1) * 128].reshape(8, 128, 128), wu[:, f * 128:(f + 1) * 128].reshape(8, 128, 128)], 0)
            put(f"{t}gu{f}", a.transpose(2, 0, 1, 3).reshape(128, 2048))
        for o in range(8):
            put(f"{t}d{o}", wd[:, o * 128:(o + 1) * 128].reshape(NFF, 128, 128).transpose(1, 0, 2).reshape(128, NFF * 128))
    win = inp["w_in"][0]
    QA, KA, VA, QB, KB, VB, GA, GB = 0, 768, 1536, 2304, 2816, 3328, 3840, 4864
    for g in range(3):
        for pr in range(2):
            h0 = 4 * g + 2 * pr
            put(f"dq{g}{pr}", _kchunks(win, QA + h0 * 64, 128))
            put(f"dk{g}{pr}", _kchunks(win, KA + h0 * 64, 128))
        put(f"dv{g}", _kchunks(win, VA + 4 * g * 64, 256))
    for i in range(4):
        put(f"mq{i}", _kchunks(win, QB + i * 128, 128))
        put(f"mk{i}", _kchunks(win, KB + i * 128, 128))
        put(f"mv{i}", _kchunks(win, VB + i * 128, 128))
    for o in range(8):
        put(f"ga{o}", _kchunks(win, GA + o * 128, 128))
        put(f"gb{o}", _kchunks(win, GB + o * 128, 128))
    wua, wub, wo = inp["w_up_a"][0], inp["w_up_b"][0], inp["w_out"][0]
    for o in range(8):
        a = np.zeros((128, 4, 128), np.float32)
        a[:64] = wua[:, o * 128:(o + 1) * 128].reshape(4, 64, 128).transpose(1, 0, 2)
        put(f"ua{o}", a.reshape(128, 512))
        b = np.zeros((128, 8, 128), np.float32)
        b[:64] = wub[:, o * 128:(o + 1) * 128].reshape(8, 64, 128).transpose(1, 0, 2)
        put(f"ub{o}", b.reshape(128, 1024))
        put(f"wo{o}", _kchunks(wo, o * 128, 128))
    return wall


def _const_tables(inp, j):
    cst = np.zeros((128, CF), np.float32)
    for col, key in ((C_G1, "norm_ffn1"), (C_GM, "norm_mix"), (C_G2, "norm_ffn2"), (C_GF, "norm_final")):
        g = np.asarray(inp[key], np.float32).reshape(-1)
        cst[:, col:col + 8] = g.reshape(8, 128).T
    cst[:, C_ID:C_ID + 128] = np.eye(128, dtype=np.float32)
    first_valid_tok = 2048 * (3 - j)
    first_valid_blk = first_valid_tok // 256
    p = np.arange(128)
    blk = np.arange(32)
    for qt in range(16):
        q = qt * 128 + p
        ob = (OWN0 + q) // 256
        ok = (blk[None, :] < ob[:, None]) & (blk[None, :] >= first_valid_blk)
        cst[:, C_GMASK + qt * 32:C_GMASK + (qt + 1) * 32] = np.where(ok, 0.0, NEGG)
        cst[:, C_OWNHOT + qt * 32:C_OWNHOT + (qt + 1) * 32] = (blk[None, :] == ob[:, None]).astype(np.float32)
    t = np.arange(64)[None, :] * 128 + p[:, None]
    cst[:, C_VALM:C_VALM + 64] = (t >= first_valid_tok).astype(np.float32)
    for g, D in enumerate(DIL):
        nq = 16 // D
        for r in range(D):
            for jt in range(nq + 1):
                i = OWN0 // D - 128 + 128 * jt + p
                pos = r + D * i
                cst[:, C_VALD + VALD_OFF[g] + r * (nq + 1) + jt] = (pos >= first_valid_tok).astype(np.float32)
    slopes_a = 2.0 ** (-8.0 * np.arange(1, 13, dtype=np.float64) / 12)
    b = p[:, None].astype(np.float64)
    a = np.arange(128)[None, :].astype(np.float64)
    for h in range(12):
        D = DIL[h // 4]
        s = slopes_a[h]
        cur = np.where(a >= b, np.exp(-s * D * (a - b)), 0.0)
        prev = np.where(a <= b, np.exp(-s * D * (a - b + 128)), 0.0)
        cst[:, C_DEC + h * 256:C_DEC + h * 256 + 128] = cur
        cst[:, C_DEC + h * 256 + 128:C_DEC + (h + 1) * 256] = prev
    bf = ml_dtypes.bfloat16
    tk = np.arange(WIN)
    etab = (tk[None, :] // 256 == np.arange(32)[:, None]).astype(np.float32).astype(bf)
    slopes_b = 2.0 ** (-8.0 * np.arange(1, 9, dtype=np.float64) / 8)
    akt = np.zeros((8, 4, WIN), np.float32)
    aqt = np.zeros((8, 4, NOWN), np.float32)
    tq = OWN0 + np.arange(NOWN)
    for h in range(8):
        s = slopes_b[h]
        akt[h, 0] = s * 128 * (tk // 128)
        akt[h, 1] = s * (tk % 128)
        akt[h, 2] = 1.0
        akt[h, 3] = 1.0
        aqt[h, 0] = 1.0
        aqt[h, 1] = 1.0
        aqt[h, 2] = -s * 128 * (tq // 128)
        aqt[h, 3] = -s * (tq % 128)
    trib = (p[:, None] <= np.arange(128)[None, :]).astype(np.float32).astype(bf)
    return cst, etab, akt.astype(bf), aqt.astype(bf), trib


STOP = [99]
RUN = [set(range(6))]


def _phase(n):
    if n in RUN[0]:
        with ExitStack() as ph:
            yield ph
DBG = [False]
LAST = {}
SUB = [""]


class _Stop(Exception):
    pass


def _chk(n):
    if STOP[0] == n:
        raise _Stop()


def build():
    nc = bass.Bass("TRN2", target_bir_lowering=False)
    xw = nc.dram_tensor("xw", [8, 128, WIN], F32, kind="ExternalInput").ap()
    wall = nc.dram_tensor("wall", [128, TOTF], F32, kind="ExternalInput").ap()
    cstd = nc.dram_tensor("cst", [128, CF], F32, kind="ExternalInput").ap()
    etabd = nc.dram_tensor("etab", [32, WIN], BF16, kind="ExternalInput").ap()
    aktd = nc.dram_tensor("akt", [8, 4, WIN], BF16, kind="ExternalInput").ap()
    aqtd = nc.dram_tensor("aqt", [8, 4, NOWN], BF16, kind="ExternalInput").ap()
    tribd = nc.dram_tensor("trib", [128, 128], BF16, kind="ExternalInput").ap()
    outT = nc.dram_tensor("outT", [8, 128, NOWN], F32, kind="ExternalOutput").ap()
    WB = nc.dram_tensor("wb_s", [128, TOTF], BF16, kind=("Internal" if 0 in RUN[0] else "ExternalInput")).ap()
    sk = "ExternalOutput" if DBG[0] else "Internal"
    R_ = RUN[0]
    kin = lambda prod: sk if prod in R_ else "ExternalInput"
    HS = nc.dram_tensor("h_s", [8, 128, WIN], BF16, kind=kin(1)).ap()
    X1S = nc.dram_tensor("x1_s", [8, 128, NOWN], F32, kind=kin(1)).ap()
    MS = nc.dram_tensor("m_s", [8, 128, NOWN], BF16, kind=kin(4)).ap()
    YAD = nc.dram_tensor("ya_d", [64, 4, NOWN], BF16, kind=kin(2)).ap()
    YBD = nc.dram_tensor("yb_d", [64, 8, NOWN], BF16, kind=kin(3)).ap()
    HSr = HS.rearrange("k p t -> p k t")
    X1r = X1S.rearrange("k p t -> p k t")
    MSr = MS.rearrange("k p t -> p k t")
    xwr = xw.rearrange("k p t -> p k t")
    outr = outT.rearrange("k p t -> p k t")

    try:
        _build_body(nc, xwr, wall, cstd, etabd, aktd, aqtd, tribd, outr, WB, HSr, X1r, MSr, YAD, YBD)
    except _Stop:
        pass
    return nc


def _build_body(nc, xwr, wall, cstd, etabd, aktd, aqtd, tribd, outr, WB, HSr, X1r, MSr, YAD, YBD):
    with ExitStack() as top:
        P = Prog(nc, top)

        def SB(stk, name, shape, dt):
            return stk.enter_context(nc.sbuf_tensor("s_" + name, shape, dt))

        bank = [top.enter_context(nc.psum_tensor(f"bank{i}", [128, 512], F32)) for i in range(8)]
        cst = SB(top, "cst", [128, CF], F32)
        ones = SB(top, "ones", [128, 128], BF16)
        onesf = SB(top, "onesf", [128, 64], F32)
        trib = SB(top, "trib", [128, 128], BF16)

        def DMA(out, in_, reads, writes, q="sp"):
            P.dma(lambda e: e.dma_start(out=out, in_=in_), reads, writes, q=q)

        def MMS(groups, reads, writes):
            def fn(e):
                ins = None
                for out, pairs in groups:
                    n = len(pairs)
                    for i, (l, r) in enumerate(pairs):
                        ins = e.matmul(out, lhsT=l, rhs=r, start=(i == 0), stop=(i == n - 1))
                return ins
            P.op("pe", fn, reads, writes)

        def ACT(out, in_, func, reads, writes, scale=None):
            if scale is None:
                P.op("act", lambda e: e.activation(out=out, in_=in_, func=func), reads, writes)
            else:
                P.op("act", lambda e: e.activation(out=out, in_=in_, func=func, scale=scale), reads, writes)

        def ACP(out, in_, reads, writes, mul=None):
            if mul is None:
                P.op("act", lambda e: e.copy(out=out, in_=in_), reads, writes)
            else:
                P.op("act", lambda e: e.mul(out=out, in_=in_, mul=mul), reads, writes)

        def TT(out, in0, in1, op, reads, writes, eng="dve"):
            P.op(eng, lambda e: e.tensor_tensor(out=out, in0=in0, in1=in1, op=op), reads, writes)

        def TS(out, in0, s1, s2, op0, op1, reads, writes, eng="dve"):
            if op1 is None:
                P.op(eng, lambda e: e.tensor_scalar(out=out, in0=in0, scalar1=s1, scalar2=None, op0=op0), reads, writes)
            else:
                P.op(eng, lambda e: e.tensor_scalar(out=out, in0=in0, scalar1=s1, scalar2=s2, op0=op0, op1=op1), reads, writes)

        def STT(out, in0, scalar, in1, op0, op1, reads, writes):
            P.op("dve", lambda e: e.scalar_tensor_tensor(out=out, in0=in0, scalar=scalar, in1=in1, op0=op0, op1=op1), reads, writes)

        def CP(out, in_, reads, writes, eng="dve"):
            P.op(eng, lambda e: e.tensor_copy(out=out, in_=in_), reads, writes)

        def RCP(out, in_, reads, writes):
            P.op("dve", lambda e: e.reciprocal(out=out, in_=in_), reads, writes)

        def MSET(ap, val, writes, eng="pool"):
            P.op(eng, lambda e: e.memset(ap, val), (), writes)

        def wres(name):
            o, w = WLAY[name]
            return [f"WB{b}" for b in range(o // CB, (o + w - 1) // CB + 1)]

        def WLOAD(dst, name, writes, q="sp"):
            o, w = WLAY[name]
            DMA(dst, WB[:, o:o + w], wres(name), writes, q=q)

        DMA(cst[:], cstd, (), ["cst"])
        DMA(trib[:], tribd, (), ["trib"])
        MSET(ones[:], 1.0, ["ones"])
        MSET(onesf[:], 1.0, ["onesf"])
        for ph in _phase(0):
            stg = [SB(ph, f"stg{i}", [128, CB], F32) for i in range(3)]
            cvt = [SB(ph, f"cvt{i}", [128, CB], BF16) for i in range(3)]
            nblk = (TOTF + CB - 1) // CB
            engs = ("dve", "pool", "act")
            for b in range(nblk):
                s = b % 3
                c0 = b * CB
                w = min(CB, TOTF - c0)
                DMA(stg[s][:, 0:w], wall[:, c0:c0 + w], (), [f"stg{s}"])
                en = engs[b % 3]
                if en == "act":
                    ACP(cvt[s][:, 0:w], stg[s][:, 0:w], [f"stg{s}"], [f"cvt{s}"])
                else:
                    CP(cvt[s][:, 0:w], stg[s][:, 0:w], [f"stg{s}"], [f"cvt{s}"], eng=en)
                DMA(WB[:, c0:c0 + w], cvt[s][:, 0:w], [f"cvt{s}"], [f"WB{b}"], q="pool" if b % 2 else "sp")
            P.flush()
            _chk(0)

        def rmsnorm(tag, X, xr, gcol, out, outr, bufs, n=ST):
            sq, t1, t2 = bufs["sq"], bufs["t1"], bufs["t2"]
            nh = n // 512
            for c in range(8):
                s = c % 2
                ACT(sq[s][:, 0:n], X[:, c, :], AF.Square, [xr(c)], [f"{tag}sq{s}"])
                for hf in range(nh):
                    P.op("pe", (lambda hf=hf, s=s, c=c: (lambda e: e.matmul(bank[hf][:, :], lhsT=ones[:], rhs=sq[s][:, hf * 512:(hf + 1) * 512],
                                                                        start=(c == 0), stop=(c == 7))))(),
                         ["ones", f"{tag}sq{s}"], [f"ps{hf}"])
            for hf in range(nh):
                TS(t1[:, hf * 512:(hf + 1) * 512], bank[hf][:, :], 1.0 / DM, EPS, ALU.mult, ALU.add, [f"ps{hf}"], [f"{tag}t1"])
            ACT(t2[:, 0:n], t1[:, 0:n], AF.Sqrt, [f"{tag}t1"], [f"{tag}t2"])
            RCP(t1[:, 0:n], t2[:, 0:n], [f"{tag}t2"], [f"{tag}t1"])
            for c in range(8):
                STT(out[:, c, :], X[:, c, :], cst[:, gcol + c:gcol + c + 1], t1[:, 0:n], ALU.mult, ALU.mult,
                    [xr(c), "cst", f"{tag}t1"], [outr(c)])

        def ffn(tag, X, xr, gcol, wt, bufs):
            h0, act, sg, wgu, wd = bufs["h0"], bufs["act"], bufs["sg"], bufs["wgu"], bufs["wd"]
            h0r = lambda c: f"{tag}h0_{c}"
            rmsnorm(tag, X, xr, gcol, h0, h0r, bufs)
            h0all = [h0r(c) for c in range(8)]
            WLOAD(wgu[0][:], f"{wt}gu0", [f"{tag}wgu0"])
            it = 0
            for f in range(NFF):
                s = f % 2
                if f + 1 < NFF:
                    WLOAD(wgu[1 - s][:], f"{wt}gu{f + 1}", [f"{tag}wgu{1 - s}"])
                elif True:
                    WLOAD(wd[0][:], f"{wt}d0", [f"{tag}wd0"])
                for hf in range(2):
                    pb = 2 + 2 * (it % 2)
                    it += 1
                    cols = slice(hf * 512, (hf + 1) * 512)
                    w4 = wgu[s][:].rearrange("p (g k c) -> p g k c", g=2, k=8)
                    MMS([(bank[pb][:, :], [(w4[:, 0, k, :], h0[:, k, cols]) for k in range(8)]),
                         (bank[pb + 1][:, :], [(w4[:, 1, k, :], h0[:, k, cols]) for k in range(8)])],
                        [f"{tag}wgu{s}"] + h0all, [f"ps{pb}", f"ps{pb + 1}"])
                    ss_ = it % 2
                    ACT(sg[ss_][:, :], bank[pb][:, :], AF.Silu, [f"ps{pb}"], [f"{tag}sg{ss_}"])
                    TT(act[:, f, cols], sg[ss_][:, :], bank[pb + 1][:, :], ALU.mult, [f"{tag}sg{ss_}", f"ps{pb + 1}"], [f"{tag}act{f}_{hf}"])
            actall = [f"{tag}act{f}_{hf}" for f in range(NFF) for hf in range(2)]
            it = 0
            for o in range(8):
                s = o % 2
                if o + 1 < 8:
                    WLOAD(wd[1 - s][:], f"{wt}d{o + 1}", [f"{tag}wd{1 - s}"])
                w3 = wd[s][:].rearrange("p (f c) -> p f c", f=NFF)
                for hf in range(2):
                    pb = 6 + (it % 2)
                    it += 1
                    cols = slice(hf * 512, (hf + 1) * 512)
                    MMS([(bank[pb][:, :], [(w3[:, f, :], act[:, f, cols]) for f in range(NFF)])],
                        [f"{tag}wd{s}"] + actall, [f"ps{pb}"])
                    STT(X[:, o, cols], bank[pb][:, :], 0.5, X[:, o, cols], ALU.mult, ALU.add, [f"ps{pb}", xr(o)], [xr(o)])

        def ffn_bufs(stk, tag):
            return dict(
                h0=SB(stk, tag + "h0", [128, 8, ST], BF16),
                act=SB(stk, tag + "act", [128, NFF, ST], BF16),
                sq=[SB(stk, f"{tag}sq{i}", [128, ST], BF16) for i in range(2)],
                t1=SB(stk, tag + "t1", [128, ST], F32),
                t2=SB(stk, tag + "t2", [128, ST], F32),
                sg=[SB(stk, f"{tag}sg{i}", [128, 512], F32) for i in range(2)],
                wgu=[SB(stk, f"{tag}wgu{i}", [128, 2048], BF16) for i in range(2)],
                wd=[SB(stk, f"{tag}wd{i}", [128, NFF * 128], BF16) for i in range(2)],
            )

        for ph in _phase(1):
            bufs = ffn_bufs(ph, "a")
            Xb = [SB(ph, f"aX{i}", [128, 8, ST], F32) for i in range(2)]
            nst = WIN // ST
            DMA(Xb[0][:], xwr[:, :, 0:ST], (), [f"aX0_{c}" for c in range(8)])
            for st in range(nst):
                s = st % 2
                X = Xb[s]
                xr = (lambda s: (lambda c: f"aX{s}_{c}"))(s)
                if st + 1 < nst:
                    DMA(Xb[1 - s][:], xwr[:, :, (st + 1) * ST:(st + 2) * ST], (), [f"aX{1 - s}_{c}" for c in range(8)])
                ffn("a", X, xr, C_G1, "f1", bufs)
                if st * ST >= OWN0:
                    DMA(X1r[:, :, st * ST - OWN0:(st + 1) * ST - OWN0], X[:], [xr(c) for c in range(8)], [f"X1S{st}"])
                h0r = lambda c: f"ah0_{c}"
                rmsnorm("a", X, xr, C_GM, bufs["h0"], h0r, bufs)
                DMA(HSr[:, :, st * ST:(st + 1) * ST], bufs["h0"][:], [h0r(c) for c in range(8)], [f"HS{st}"])
            P.flush()
            _chk(1)

        with ExitStack() as mid:
            YA = SB(mid, "YA", [64, 4, NOWN], BF16)
            for ph in _phase(2):
                hb = SB(ph, "d_hb", [128, 8, 2048], BF16)
                ACC = SB(ph, "d_acc", [128, 4, NOWN], F32)
                QT = [SB(ph, f"d_qt{i}", [128, 2048], BF16) for i in range(2)]
                KT = [SB(ph, f"d_kt{i}", [128, 2048 + 128 * 16], BF16) for i in range(2)]
                VA = SB(ph, "d_va", [128, 32, 4, 65], BF16)
                wq = [SB(ph, f"d_wq{i}", [128, 8, 128], BF16) for i in range(2)]
                wk = [SB(ph, f"d_wk{i}", [128, 8, 128], BF16) for i in range(2)]
                wv = SB(ph, "d_wv", [128, 8, 256], BF16)
                et = [SB(ph, f"d_et{i}", [128, 2, 2, 128], F32) for i in range(2)]
                PT = [SB(ph, f"d_pt{i}", [128, 2, 2, 128], BF16) for i in range(4)]
                rd = SB(ph, "d_rd", [128, 512], F32)
                dec = cst[:, C_DEC:C_DEC + 3072].rearrange("p (h a b) -> p h a b", h=12, a=2)
                pcnt = [0]

                def nbank():
                    pcnt[0] += 1
                    return pcnt[0] % 4

                for g, D in enumerate(DIL):
                    npr = NOWN // D
                    nq = 16 // D
                    KW = npr + 128
                    ntile = D * (nq + 1)
                    for pr in range(2):
                        WLOAD(wq[pr][:].rearrange("p k c -> p (k c)"), f"dq{g}{pr}", [f"d_wq{pr}"])
                        WLOAD(wk[pr][:].rearrange("p k c -> p (k c)"), f"dk{g}{pr}", [f"d_wk{pr}"])
                    WLOAD(wv[:].rearrange("p k c -> p (k c)"), f"dv{g}", ["d_wv"])
                    for hd in range(4):
                        CP(VA[:, 0:ntile, hd, 64], cst[:, C_VALD + VALD_OFF[g]:C_VALD + VALD_OFF[g] + ntile], ["cst"], ["d_va"], eng="pool")
                    for half in range(2):
                        hs_res = ["HS4", "HS5"] if half == 0 else ["HS6", "HS7"]
                        DMA(hb[:], HSr[:, :, 4096 + 2048 * half:4096 + 2048 * (half + 1)], hs_res, ["d_hb"])
                        if half == 0:
                            col0 = lambda r: 2048 - 128 * D + r
                            ncol = 128
                            kdst0 = 0
                        else:
                            col0 = lambda r: r
                            ncol = npr
                            kdst0 = 128
                        for pr in range(2):
                            rpb = max(1, 512 // ncol)
                            for r0 in range(0, D, rpb):
                                rs = list(range(r0, min(D, r0 + rpb)))
                                for c0 in range(0, ncol, 512):
                                    cn = min(512, ncol - c0)
                                    targets = [("k", wk[pr], KT[pr])] + ([("q", wq[pr], QT[pr])] if half == 1 else [])
                                    for kind, wt_, dst in targets:
                                        b = nbank()
                                        groups = []
                                        for ri, r in enumerate(rs):
                                            groups.append((bank[b][:, ri * cn:(ri + 1) * cn],
                                                           [(wt_[:, k, :], hb[:, k, ss(col0(r) + D * c0, cn, D)]) for k in range(8)]))
                                        MMS(groups, ["d_hb", f"d_w{kind}{pr}"], [f"ps{b}"])
                                        for ri, r in enumerate(rs):
                                            src = bank[b][:, ri * cn:(ri + 1) * cn]
                                            if kind == "k":
                                                CP(KT[pr][:, r * KW + kdst0 + c0:r * KW + kdst0 + c0 + cn], src, [f"ps{b}"], [f"d_kt{pr}"])
                                            else:
                                                ACP(QT[pr][:, r * npr + c0:r * npr + c0 + cn], src, [f"ps{b}"], [f"d_qt{pr}"], mul=0.125)
                        jts = [0] if half == 0 else list(range(1, nq + 1))
                        for r in range(D):
                            for jt in jts:
                                c_first = (2048 - 128 * D + r) if half == 0 else (r + D * 128 * (jt - 1))
                                b = nbank()
                                MMS([(bank[b][:, 0:256], [(hb[:, k, ss(c_first, 128, D)], wv[:, k, :]) for k in range(8)])],
                                    ["d_hb", "d_wv"], [f"ps{b}"])
                                t_ = r * (nq + 1) + jt
                                src = bank[b][:, 0:256].rearrange("p (h d) -> p h d", h=4)
                                if (r + jt) % 2:
                                    ACP(VA[:, t_, :, 0:64], src, [f"ps{b}"], ["d_va"])
                                else:
                                    CP(VA[:, t_, :, 0:64], src, [f"ps{b}"], ["d_va"])
                    gcount = 0
                    for pr in range(2):
                        ob = [4 + 2 * pr, 5 + 2 * pr]
                        qslot = 0
                        ptc = 0
                        prev_pt = None
                        for r in range(D):
                            for jt in range(nq + 1):
                                halves = [1] if jt == 0 else ([0] if jt == nq else [0, 1])
                                h0_, h1_ = halves[0], halves[-1] + 1
                                qc0 = r * npr + 128 * (jt - 1 + h0_)
                                nqc = 128 * len(halves)
                                sb0 = 2 * (gcount % 2)
                                sv = [bank[sb0 + e_][:, 0:256].rearrange("p (a b) -> p a b", a=2) for e_ in range(2)]
                                kc = r * KW + 128 * jt
                                MMS([(sv[e_][:, h0_:h1_, :], [(KT[pr][64 * e_:64 * e_ + 64, kc:kc + 128], QT[pr][64 * e_:64 * e_ + 64, qc0:qc0 + nqc])])
                                     for e_ in range(2)], [f"d_kt{pr}", f"d_qt{pr}"], [f"ps{sb0}", f"ps{sb0 + 1}"])
                                es = gcount % 2
                                ps_ = ptc % 4
                                ptc += 1
                                gcount += 1
                                for e_ in range(2):
                                    ACT(et[es][:, e_, h0_:h1_, :], sv[e_][:, h0_:h1_, :], AF.Exp, [f"ps{sb0 + e_}"], [f"d_et{es}_{e_}"])
                                hh0 = 4 * g + 2 * pr
                                TT(PT[ps_][:, :, h0_:h1_, :], et[es][:, :, h0_:h1_, :], dec[:, hh0:hh0 + 2, h0_:h1_, :], ALU.mult,
                                   [f"d_et{es}_0", f"d_et{es}_1", "cst"], [f"d_pt{ps_}"])
                                if jt >= 1:
                                    qt_ = jt - 1
                                    t_prev = r * (nq + 1) + jt - 1
                                    t_cur = r * (nq + 1) + jt
                                    pp = prev_pt
                                    groups = []
                                    for e_ in range(2):
                                        hd = 2 * pr + e_
                                        groups.append((bank[ob[e_]][0:65, qslot * 128:(qslot + 1) * 128],
                                                       [(VA[:, t_prev, hd, :], PT[pp][:, e_, 1, :]), (VA[:, t_cur, hd, :], PT[ps_][:, e_, 0, :])]))
                                    MMS(groups, ["d_va", f"d_pt{pp}", f"d_pt{ps_}"], [f"ps{ob[0]}", f"ps{ob[1]}"])
                                    qslot += 1
                                    if qslot == 4:
                                        qslot = 0
                                        for e_ in range(2):
                                            hd = 2 * pr + e_
                                            if D == 1:
                                                m = qt_ // 4
                                                dst = ACC[0:65, hd, m * 512:(m + 1) * 512]
                                                src = bank[ob[e_]][0:65, :]
                                            elif D == 4:
                                                dst = ACC[0:65, hd, ss(r, 512, 4)]
                                                src = bank[ob[e_]][0:65, :]
                                            else:
                                                r0 = r - 3
                                                dst = ACC[0:65, hd, :].rearrange("p (i r) -> p r i", r=16)[:, r0:r0 + 4, :]
                                                src = bank[ob[e_]][0:65, :].rearrange("p (r i) -> p r i", r=4)
                                            if g == 0:
                                                CP(dst, src, [f"ps{ob[e_]}"], [f"d_acc{hd}"])
                                            else:
                                                TT(dst, src, dst, ALU.add, [f"ps{ob[e_]}", f"d_acc{hd}"], [f"d_acc{hd}"])
                                prev_pt = ps_
                for hd in range(4):
                    for m in range(4):
                        cols = slice(m * 512, (m + 1) * 512)
                        RCP(rd[64:65, :], ACC[64:65, hd, cols], [f"d_acc{hd}"], ["d_rd"])
                        b = nbank()
                        MMS([(bank[b][0:64, :], [(onesf[64:65, 0:64], rd[64:65, :])])], ["onesf", "d_rd"], [f"ps{b}"])
                        TT(YA[0:64, hd, cols], ACC[0:64, hd, cols], bank[b][0:64, :], ALU.mult, [f"d_acc{hd}", f"ps{b}"], [f"YA{hd}"])
                if DBG[0]:
                    DMA(YAD, YA[:], [f"YA{hd}" for hd in range(4)], ["YAD"])
                P.flush()
                _chk(2)

            YB = SB(mid, "YB", [64, 8, NOWN], BF16)
            for ph in _phase(3):
                KAB = [SB(ph, f"m_k{i}", [128, WIN], BF16) for i in range(2)]
                QAB = [SB(ph, f"m_q{i}", [128, NOWN], BF16) for i in range(2)]
                VAB = SB(ph, "m_v", [128, 64, 2, 65], BF16)
                hbuf = [SB(ph, f"m_hb{i}", [128, 8, 512], BF16) for i in range(2)]
                wq = SB(ph, "m_wq", [128, 8, 128], BF16)
                wk = SB(ph, "m_wk", [128, 8, 128], BF16)
                wv = SB(ph, "m_wv", [128, 8, 128], BF16)
                QF = SB(ph, "m_qf", [128, 512], F32)
                KM = SB(ph, "m_km", [128, 32], F32)
                g2 = SB(ph, "m_g2", [128, 2, 32], F32)
                mx = SB(ph, "m_mx", [128, 2, 8], F32)
                thr = SB(ph, "m_thr", [128, 2, 1], F32)
                sel = SB(ph, "m_sel", [128, 2, 32], F32)
                ZA = SB(ph, "m_za", [128, 96], F32)
                ZB = SB(ph, "m_zb", [128, 32], F32)
                PTm = [SB(ph, f"m_pt{i}", [128, 512], BF16) for i in range(6)]
                osb = SB(ph, "m_osb", [128, 512], F32)
                rdm = SB(ph, "m_rd", [128, 512], F32)
                erow = (slice(64, 96), slice(0, 32))
                arow = (slice(96, 100), slice(32, 36))
                drow = (slice(0, 64), slice(64, 128))
                krows = (slice(0, 100), slice(0, 128))
                if "S1" not in SUB[0]:
                    MSET(KAB[1][32:64, :], 0.0, ["m_k1x"])
                    MSET(QAB[1][32:64, :], 0.0, ["m_q1x"])
                MSET(ZA[:], 0.0, ["m_za"])
                MSET(KM[:], 0.0, ["m_km"])
                if "S2" not in SUB[0]:
                    DMA(KAB[0][64:96, :], etabd, (), ["m_k0e"])
                    DMA(KAB[1][0:32, :], etabd, (), ["m_k1e"])
                for e_ in (range(2) if "S3" not in SUB[0] else ()):
                    CP(VAB[:, :, e_, 64], cst[:, C_VALM:C_VALM + 64], ["cst"], ["m_vval"], eng="pool")
                sring = [0]
                for i in (range(4) if "P1" not in SUB[0] else range(1)):
                    WLOAD(wq[:].rearrange("p k c -> p (k c)"), f"mq{i}", ["m_wq"])
                    WLOAD(wk[:].rearrange("p k c -> p (k c)"), f"mk{i}", ["m_wk"])
                    WLOAD(wv[:].rearrange("p k c -> p (k c)"), f"mv{i}", ["m_wv"])
                    for e_ in (range(2) if "L" not in SUB[0] else ()):
                        DMA(KAB[e_][arow[e_], :], aktd[2 * i + e_], ["m_k1x"], [f"m_k{e_}a"])
                        DMA(QAB[e_][arow[e_], :], aqtd[2 * i + e_], ["m_q1x"], [f"m_q{e_}a"])
                    DMA(hbuf[0][:], HSr[:, :, 0:512], ["HS0"], ["m_hb0"])
                    for s2 in range(16):
                        hs = s2 % 2
                        H_ = hbuf[hs]
                        if s2 + 1 < 16:
                            DMA(hbuf[1 - hs][:], HSr[:, :, (s2 + 1) * 512:(s2 + 2) * 512], [f"HS{(s2 + 1) // 2}"], [f"m_hb{1 - hs}"])
                        cols = slice(s2 * 512, (s2 + 1) * 512)
                        if "K" not in SUB[0]:
                          MMS([(bank[6][:, :], [(wk[:, k, :], H_[:, k, :]) for k in range(8)])], [f"m_hb{hs}", "m_wk"], ["ps6"])
                        ACP(KAB[0][0:64, cols], bank[6][0:64, :], ["ps6"], ["m_k0d"])
                        CP(KAB[1][64:128, cols], bank[6][64:128, :], ["ps6"], ["m_k1d"])
                        P.op("dve", (lambda s2=s2: (lambda e: e.tensor_reduce(out=KM[:, 2 * s2:2 * s2 + 2],
                                                                              in_=bank[6][:, :].rearrange("p (a b) -> p a b", a=2),
                                                                              axis=AX.X, op=ALU.add)))(), ["ps6"], ["m_km"])
                        MMS([(bank[7][:, tt * 128:(tt + 1) * 128], [(H_[:, k, tt * 128:(tt + 1) * 128], wv[:, k, :]) for k in range(8)])
                             for tt in range(4)], [f"m_hb{hs}", "m_wv"], ["ps7"])
                        v4 = bank[7][:, :].rearrange("p (t e d) -> p t e d", t=4, e=2)
                        ACP(VAB[:, 4 * s2:4 * s2 + 4, 0, 0:64], v4[:, :, 0, :], ["ps7"], ["m_v0"])
                        if "T0" not in SUB[0]:
                            ACP(VAB[:, 4 * s2:4 * s2 + 4, 1, 0:64], v4[:, :, 1, :], ["ps7"], ["m_v1"])
                        elif "T2" in SUB[0]:
                            TS(VAB[:, 4 * s2:4 * s2 + 4, 1, 0:64], v4[:, :, 1, :], 1.0, None, ALU.mult, None, ["ps7"], ["m_v1"])
                        elif "T3" in SUB[0]:
                            for tt_ in range(4):
                                CP(VAB[:, 4 * s2 + tt_, 1, 0:64], v4[:, tt_, 1, :], ["ps7"], ["m_v1"])
                        else:
                            CP(VAB[:, 4 * s2:4 * s2 + 4, 1, 0:64], v4[:, :, 1, :], ["ps7"], ["m_v1"])
                        if s2 >= 12:
                            qi = s2 - 12
                            qcols = slice(qi * 512, (qi + 1) * 512)
                            MMS([(bank[5][:, :], [(wq[:, k, :], H_[:, k, :]) for k in range(8)])], [f"m_hb{hs}", "m_wq"], ["ps5"])
                            ACP(QAB[0][0:64, qcols], bank[5][0:64, :], ["ps5"], ["m_q0d"], mul=0.125)
                            TS(QAB[1][64:128, qcols], bank[5][64:128, :], 0.125, None, ALU.mult, None, ["ps5"], ["m_q1d"])
                            ACP(QF[:, :], bank[5][:, :], ["ps5"], ["m_qf"], mul=0.125)
                            for sub in (range(4) if "G" not in SUB[0] else ()):
                                qt16 = 4 * qi + sub
                                sc = slice(sub * 128, (sub + 1) * 128)
                                MMS([(bank[4 + e_][:, 0:32], [(QF[drow[e_], sc], KM[drow[e_], :])]) for e_ in range(2)],
                                    ["m_qf", "m_km"], ["ps4", "ps5"])
                                for e_ in range(2):
                                    TT(g2[:, e_, :], bank[4 + e_][:, 0:32], cst[:, C_GMASK + 32 * qt16:C_GMASK + 32 * qt16 + 32],
                                       ALU.add, [f"ps{4 + e_}", "cst"], ["m_g2"])
                                for e_ in range(2):
                                    P.op("dve", (lambda e_=e_: (lambda e: e.max(out=mx[:, e_, :], in_=g2[:, e_, :])))(), ["m_g2"], ["m_mx"])
                                TS(thr[:, :, :], mx[:, :, 2:3], -1.0e29, None, ALU.max, None, ["m_mx"], ["m_thr"])
                                for e_ in range(2):
                                    TS(sel[:, e_, :], g2[:, e_, :], thr[:, e_, :], None, ALU.is_ge, None, ["m_g2", "m_thr"], ["m_sel"])
                                    TT(sel[:, e_, :], sel[:, e_, :], cst[:, C_OWNHOT + 32 * qt16:C_OWNHOT + 32 * qt16 + 32], ALU.add,
                                       ["m_sel", "cst"], ["m_sel"])
                                TS(ZA[:, 64:96], sel[:, 0, :], BIG, -BIG, ALU.mult, ALU.add, ["m_sel"], ["m_za"])
                                TS(ZB[:, :], sel[:, 1, :], BIG, -BIG, ALU.mult, ALU.add, ["m_sel"], ["m_zb"])
                                P.op("pe", (lambda: (lambda e: e.transpose(bank[3][0:96, 0:128], ZA[:], cst[:, C_ID:C_ID + 128])))(),
                                     ["m_za", "cst"], ["ps3"])
                                P.op("pe", (lambda: (lambda e: e.transpose(bank[3][0:32, 128:256], ZB[:], cst[:, C_ID:C_ID + 128])))(),
                                     ["m_zb", "cst"], ["ps3"])
                                qc = slice(qi * 512 + sub * 128, qi * 512 + (sub + 1) * 128)
                                ACP(QAB[0][64:96, qc], bank[3][64:96, 0:128], ["ps3"], ["m_q0n"])
                                ACP(QAB[1][0:32, qc], bank[3][0:32, 128:256], ["ps3"], ["m_q1n"])
                    for e_ in (range(2) if "A" not in SUB[0] else ()):
                        h = 2 * i + e_
                        kres = [f"m_k{e_}d", f"m_k{e_}a", f"m_k{e_}e", "m_k1x"]
                        qres = [f"m_q{e_}d", f"m_q{e_}a", f"m_q{e_}n", "m_q1x"]
                        rows = krows[e_]
                        for qi in range(4):
                            ob_ = 3 + 0
                            ob_ = 4 if (qi % 2 == 0) else 5
                            nfull = 48 + 4 * qi
                            nk = nfull + 4
                            slots = {}

                            def emit_qk(kt, e_=e_, qi=qi, nfull=nfull, rows=rows, kres=kres, qres=qres, slots=slots):
                                di = kt - nfull
                                c0 = 128 * di if di >= 0 else 0
                                sb_ = sring[0] % 4
                                pt_ = sring[0] % 6
                                sring[0] += 1
                                slots[kt] = (c0, pt_)
                                MMS([(bank[sb_][:, c0:512], [(KAB[e_][rows, kt * 128:(kt + 1) * 128], QAB[e_][rows, qi * 512 + c0:(qi + 1) * 512])])],
                                    kres + qres, [f"ps{sb_}"])
                                ACT(PTm[pt_][:, c0:512], bank[sb_][:, c0:512], AF.Exp, [f"ps{sb_}"], [f"m_pt{pt_}"])
                                if di >= 0:
                                    TT(PTm[pt_][:, c0:c0 + 128], PTm[pt_][:, c0:c0 + 128], trib[:, :], ALU.mult, [f"m_pt{pt_}", "trib"], [f"m_pt{pt_}"],
                                       eng="pool")

                            def emit_pv(kt, e_=e_, ob_=ob_, nk=nk, slots=slots):
                                c0, pt_ = slots[kt]
                                P.op("pe", (lambda kt=kt, c0=c0, pt_=pt_, ob_=ob_, nk=nk, e_=e_:
                                            (lambda e: e.matmul(bank[ob_][0:65, c0:512], lhsT=VAB[:, kt, e_, :], rhs=PTm[pt_][:, c0:512],
                                                                start=(kt == 0), stop=(kt == nk - 1))))(),
                                     [f"m_v{e_}", "m_vval", f"m_pt{pt_}"], [f"ps{ob_}"])

                            LA = 3
                            for kt in range(min(LA, nk)):
                                emit_qk(kt)
                            for kt in range(nk):
                                if kt + LA < nk:
                                    emit_qk(kt + LA)
                                emit_pv(kt)
                            ACP(osb[0:65, :], bank[ob_][0:65, :], [f"ps{ob_}"], ["m_osb"])
                            RCP(rdm[64:65, :], osb[64:65, :], ["m_osb"], ["m_rd"])
                            MMS([(bank[7][0:64, :], [(onesf[64:65, 0:64], rdm[64:65, :])])], ["onesf", "m_rd"], ["ps7"])
                            TT(YB[0:64, h, qi * 512:(qi + 1) * 512], osb[0:64, :], bank[7][0:64, :], ALU.mult, ["m_osb", "ps7"], [f"YB{h}"])
                if DBG[0]:
                    DMA(YBD, YB[:], [f"YB{h}" for h in range(8)], ["YBD"])
                P.flush()
                _chk(3)

            for ph in _phase(4):
                hown = [SB(ph, f"g_h{i}", [128, 8, ST], BF16) for i in range(2)]
                mg = SB(ph, "g_mg", [128, 8, ST], BF16)
                wga = [SB(ph, f"g_wga{i}", [128, 8, 128], BF16) for i in range(2)]
                wgb = [SB(ph, f"g_wgb{i}", [128, 8, 128], BF16) for i in range(2)]
                wua = [SB(ph, f"g_wua{i}", [128, 4, 128], BF16) for i in range(2)]
                wub = [SB(ph, f"g_wub{i}", [128, 8, 128], BF16) for i in range(2)]
                sga = [SB(ph, f"g_sa{i}", [128, 512], F32) for i in range(2)]
                sgb = [SB(ph, f"g_sb{i}", [128, 512], F32) for i in range(2)]
                yall = [f"YA{hd}" for hd in range(4)] + [f"YB{h}" for h in range(8)]
                if 2 not in RUN[0]:
                    DMA(YA[:], YAD, (), [f"YA{hd}" for hd in range(4)])
                if 3 not in RUN[0]:
                    DMA(YB[:], YBD, (), [f"YB{h}" for h in range(8)])
                it = 0
                for stl in range(2):
                    H_ = hown[stl]
                    DMA(H_[:], HSr[:, :, OWN0 + stl * ST:OWN0 + (stl + 1) * ST], [f"HS{6 + stl}"], [f"g_h{stl}"])
                    for o in range(8):
                        s = o % 2
                        WLOAD(wga[s][:].rearrange("p k c -> p (k c)"), f"ga{o}", [f"g_wga{s}"])
                        WLOAD(wgb[s][:].rearrange("p k c -> p (k c)"), f"gb{o}", [f"g_wgb{s}"])
                        WLOAD(wua[s][:].rearrange("p k c -> p (k c)"), f"ua{o}", [f"g_wua{s}"])
                        WLOAD(wub[s][:].rearrange("p k c -> p (k c)"), f"ub{o}", [f"g_wub{s}"])
                        for hf in range(2):
                            b0 = 4 * (it % 2)
                            ts_ = it % 2
                            it += 1
                            lc = slice(hf * 512, (hf + 1) * 512)
                            gc = slice(stl * ST + hf * 512, stl * ST + (hf + 1) * 512)
                            MMS([(bank[b0][:, :], [(wua[s][0:64, hd, :], YA[0:64, hd, gc]) for hd in range(4)]),
                                 (bank[b0 + 1][:, :], [(wub[s][0:64, h, :], YB[0:64, h, gc]) for h in range(8)]),
                                 (bank[b0 + 2][:, :], [(wga[s][:, k, :], H_[:, k, lc]) for k in range(8)]),
                                 (bank[b0 + 3][:, :], [(wgb[s][:, k, :], H_[:, k, lc]) for k in range(8)])],
                                yall + [f"g_h{stl}", f"g_wga{s}", f"g_wgb{s}", f"g_wua{s}", f"g_wub{s}"],
                                [f"ps{b0 + q_}" for q_ in range(4)])
                            ACT(sga[ts_][:, :], bank[b0 + 2][:, :], AF.Sigmoid, [f"ps{b0 + 2}"], [f"g_sa{ts_}"])
                            ACT(sgb[ts_][:, :], bank[b0 + 3][:, :], AF.Sigmoid, [f"ps{b0 + 3}"], [f"g_sb{ts_}"])
                            TT(sga[ts_][:, :], sga[ts_][:, :], bank[b0][:, :], ALU.mult, [f"g_sa{ts_}", f"ps{b0}"], [f"g_sa{ts_}"])
                            TT(sgb[ts_][:, :], sgb[ts_][:, :], bank[b0 + 1][:, :], ALU.mult, [f"g_sb{ts_}", f"ps{b0 + 1}"], [f"g_sb{ts_}"])
                            TT(mg[:, o, lc], sga[ts_][:, :], sgb[ts_][:, :], ALU.add, [f"g_sa{ts_}", f"g_sb{ts_}"], [f"g_mg{o}"], eng="pool")
                    DMA(MSr[:, :, stl * ST:(stl + 1) * ST], mg[:], [f"g_mg{o}" for o in range(8)], [f"MS{stl}"])
                P.flush()
                _chk(4)

        for ph in _phase(5):
            bufs = ffn_bufs(ph, "b")
            X = SB(ph, "bX", [128, 8, ST], F32)
            mgt = SB(ph, "b_mg", [128, 8, ST], BF16)
            wo = [SB(ph, f"b_wo{i}", [128, 8, 128], BF16) for i in range(2)]
            xr = lambda c: f"bX_{c}"
            for stl in range(2):
                DMA(X[:], X1r[:, :, stl * ST:(stl + 1) * ST], [f"X1S{6 + stl}"], [xr(c) for c in range(8)])
                DMA(mgt[:], MSr[:, :, stl * ST:(stl + 1) * ST], [f"MS{stl}"], ["b_mg"])
                it = 0
                for o2 in range(8):
                    s = o2 % 2
                    WLOAD(wo[s][:].rearrange("p k c -> p (k c)"), f"wo{o2}", [f"b_wo{s}"])
                    for hf in range(2):
                        pb = 6 + (it % 2)
                        it += 1
                        cols = slice(hf * 512, (hf + 1) * 512)
                        MMS([(bank[pb][:, :], [(wo[s][:, o, :], mgt[:, o, cols]) for o in range(8)])], ["b_mg", f"b_wo{s}"], [f"ps{pb}"])
                        TT(X[:, o2, cols], bank[pb][:, :], X[:, o2, cols], ALU.add, [f"ps{pb}", xr(o2)], [xr(o2)])
                ffn("b", X, xr, C_G2, "f2", bufs)
                rmsnorm("b", X, xr, C_GF, X, xr, bufs)
                DMA(outr[:, :, stl * ST:(stl + 1) * ST], X[:], [xr(c) for c in range(8)], [f"OUT{stl}"])
            P.flush()
            _chk(5)
    return nc


_NC_CACHE = {}


def kernel(x, norm_ffn1, ffn1_gate, ffn1_up, ffn1_down, norm_mix, w_in, w_up_a, w_up_b, w_out,
           norm_ffn2, ffn2_gate, ffn2_up, ffn2_down, norm_final):
    inp = dict(x=x, norm_ffn1=norm_ffn1, ffn1_gate=ffn1_gate, ffn1_up=ffn1_up, ffn1_down=ffn1_down,
               norm_mix=norm_mix, w_in=w_in, w_up_a=w_up_a, w_up_b=w_up_b, w_out=w_out, norm_ffn2=norm_ffn2,
               ffn2_gate=ffn2_gate, ffn2_up=ffn2_up, ffn2_down=ffn2_down, norm_final=norm_final)
    inp = {k: np.asarray(v, dtype=np.float32) for k, v in inp.items()}
    wall = _pack_weights(inp)
    in_maps = []
    for c in range(8):
        b, j = c // 4, c % 4
        end = 2048 * (j + 1)
        xwin = np.zeros((WIN, DM), np.float32)
        xwin[WIN - end:] = inp["x"][b, 0:end]
        xw = np.ascontiguousarray(xwin.T).reshape(8, 128, WIN)
        cst, etab, akt, aqt, trib = _const_tables(inp, j)
        in_maps.append(dict(xw=xw, wall=wall, cst=cst, etab=etab, akt=akt, aqt=aqt, trib=trib))
    if "nc" not in _NC_CACHE:
        _NC_CACHE["nc"] = build()
    res = run_bass_kernel_spmd(_NC_CACHE["nc"], in_maps, core_ids=list(range(8)))
    if DBG[0]:
        LAST["res"] = res.results
    out = np.zeros((2, SEQ, DM), np.float32)
    for c in range(8):
        b, j = c // 4, c % 4
        o = np.asarray(res.results[c]["outT"], np.float32).reshape(DM, NOWN)
        out[b, 2048 * j:2048 * (j + 1), :] = o.T
    return out
```

```python
from contextlib import ExitStack
import numpy as np
import ml_dtypes
import concourse.bass as bass
import concourse.mybir as mybir
from concourse.bass_utils import run_bass_kernel_spmd

F32 = mybir.dt.float32
BF16 = mybir.dt.bfloat16
ALU = mybir.AluOpType
AF = mybir.ActivationFunctionType
AX = mybir.AxisListType

DM = 1024
SEQ = 8192
DFF = 2816
NFF = 22
WIN = 8192
OWN0 = 6144
NOWN = 2048
ST = 1024
EPS = 1e-6
BIG = 30000.0
NEGG = -1.0e30
DIL = (1, 4, 16)
CB = 4096

ENGS = ("pe", "act", "dve", "pool", "sp")
BUDGET = [None]


class Prog:
    def __init__(self, nc, stack, n_dma_slots=8):
        self.nc = nc
        self.sem = {e: stack.enter_context(nc.semaphore("sem_" + e)) for e in ("pe", "act", "dve", "pool")}
        self.base = {e: 0 for e in self.sem}
        self.dsem = {}
        self.dcnt = {}
        for q in ("sp", "pool", "act"):
            self.dsem[q] = [stack.enter_context(nc.semaphore(f"dma_{q}_{i}")) for i in range(n_dma_slots if q == "sp" else 4)]
            self.dcnt[q] = [0] * len(self.dsem[q])
        self.drr = {q: 0 for q in self.dsem}
        self._reset()

    def _reset(self):
        self.idx = {e: 0 for e in self.sem}
        self.ops = {e: [] for e in ENGS}
        self.last_w = {}
        self.readers = {}
        self.waited = {e: {} for e in ENGS}
        self.targets = {e: set() for e in self.sem}

    def _need(self, eng, tok, waits):
        if tok is None:
            return
        key, val = tok[0], tok[-1]
        if key == eng and eng == "pe":
            return
        if self.waited[eng].get(key, -1) >= val:
            return
        self.waited[eng][key] = val
        waits.append(tok)
        if key in self.targets:
            self.targets[key].add(val)

    def _deps(self, eng, reads, writes):
        waits = []
        for r in reads:
            self._need(eng, self.last_w.get(r), waits)
            if r.startswith("ps"):
                for t in self.readers.get(r, ()):
                    if t[0] != eng:
                        self._need(eng, t, waits)
        for w in writes:
            self._need(eng, self.last_w.get(w), waits)
            for t in self.readers.get(w, ()):
                self._need(eng, t, waits)
        return waits

    def _commit(self, tok, reads, writes):
        for r in reads:
            self.readers.setdefault(r, []).append(tok)
        for w in writes:
            self.last_w[w] = tok
            self.readers[w] = []

    def op(self, eng, fn, reads=(), writes=()):
        if BUDGET[0] is not None:
            BUDGET[0] -= 1
            if BUDGET[0] < 0:
                return
        waits = self._deps(eng, reads, writes)
        self.idx[eng] += 1
        tok = (eng, self.idx[eng])
        self.ops[eng].append((waits, fn, tok))
        self._commit(tok, reads, writes)

    def dma(self, fn, reads=(), writes=(), q="sp"):
        if BUDGET[0] is not None:
            BUDGET[0] -= 1
            if BUDGET[0] < 0:
                return
        waits = self._deps(q, reads, writes)
        i = self.drr[q]
        self.drr[q] = (i + 1) % len(self.dsem[q])
        key = f"d{q}{i}"
        prev = self.dcnt[q][i]
        if prev > 0 and self.waited[q].get(key, -1) < prev:
            self.waited[q][key] = prev
            waits.append((key, q, i, prev))
        self.dcnt[q][i] = prev + 16
        tok = (key, q, i, prev + 16)
        self.ops[q].append((waits, fn, tok))
        self._commit(tok, reads, writes)

    def flush(self):
        nc = self.nc
        for q in self.dsem:
            waits = []
            for i in range(len(self.dsem[q])):
                v = self.dcnt[q][i]
                key = f"d{q}{i}"
                if v > 0 and self.waited[q].get(key, -1) < v:
                    self.waited[q][key] = v
                    waits.append((key, q, i, v))
            if waits:
                self.ops[q].append((waits, None, None))
        ops = self.ops
        rank = {}
        for e in self.sem:
            for r_, ix in enumerate(sorted(self.targets[e])):
                rank[(e, ix)] = self.base[e] + r_ + 1
        sem, dsem = self.sem, self.dsem

        def resolve(tok):
            if len(tok) == 2:
                return sem[tok[0]], rank[tok]
            return dsem[tok[1]][tok[2]], tok[3]

        def replay(h, lst):
            for waits, fn, tok in lst:
                for w in waits:
                    s_, v_ = resolve(w)
                    h.wait_ge(s_, v_)
                if fn is not None:
                    ins = fn(h)
                    if len(tok) == 4:
                        ins.then_inc(dsem[tok[1]][tok[2]], 16)
                    elif tok in rank:
                        ins.then_inc(sem[tok[0]], 1)

        with nc.Block() as block:
            if ops["sp"]:
                @block.sync
                def _(e):
                    replay(e, ops["sp"])
            if ops["pe"]:
                @block.tensor
                def _(e):
                    replay(e, ops["pe"])
            if ops["act"]:
                @block.scalar
                def _(e):
                    replay(e, ops["act"])
            if ops["dve"]:
                @block.vector
                def _(e):
                    replay(e, ops["dve"])
            if ops["pool"]:
                @block.gpsimd
                def _(e):
                    replay(e, ops["pool"])
        for e in self.sem:
            self.base[e] += len(self.targets[e])
        self._reset()


def ss(start, n, step):
    return slice(start, start + (n - 1) * step + 1, step)


def _weight_layout():
    lay = {}
    off = 0

    def add(name, w):
        nonlocal off
        lay[name] = (off, w)
        off += w

    for t in ("f1", "f2"):
        for f in range(NFF):
            add(f"{t}gu{f}", 2 * 8 * 128)
        for o in range(8):
            add(f"{t}d{o}", NFF * 128)
    for g in range(3):
        for pr in range(2):
            add(f"dq{g}{pr}", 1024)
            add(f"dk{g}{pr}", 1024)
        add(f"dv{g}", 2048)
    for i in range(4):
        add(f"mq{i}", 1024)
        add(f"mk{i}", 1024)
        add(f"mv{i}", 1024)
    for o in range(8):
        add(f"ga{o}", 1024)
        add(f"gb{o}", 1024)
    for o in range(8):
        add(f"ua{o}", 512)
        add(f"ub{o}", 1024)
    for o in range(8):
        add(f"wo{o}", 1024)
    return lay, off


WLAY, TOTF = _weight_layout()

C_G1, C_GM, C_G2, C_GF = 0, 8, 16, 24
C_ID = 32
C_GMASK = C_ID + 128
C_OWNHOT = C_GMASK + 512
C_VALM = C_OWNHOT + 512
C_VALD = C_VALM + 64
C_DEC = C_VALD + 69
CF = C_DEC + 12 * 256
VALD_OFF = (0, 17, 37)


def _kchunks(w, c0, width):
    return np.ascontiguousarray(w[:, c0:c0 + width].reshape(8, 128, width).transpose(1, 0, 2)).reshape(128, 8 * width)


def _pack_weights(inp):
    wall = np.zeros((128, TOTF), np.float32)

    def put(name, arr):
        o, w = WLAY[name]
        assert arr.shape == (128, w), (name, arr.shape, w)
        wall[:, o:o + w] = arr

    for t, gk, uk, dk in (("f1", "ffn1_gate", "ffn1_up", "ffn1_down"), ("f2", "ffn2_gate", "ffn2_up", "ffn2_down")):
        wg, wu, wd = inp[gk][0], inp[uk][0], inp[dk][0]
        for f in range(NFF):
            a = np.stack([wg[:, f * 128:(f + 1) * 128].reshape(8, 128, 128), wu[:, f * 128:(f + 1) * 128].reshape(8, 128, 128)], 0)
            put(f"{t}gu{f}", a.transpose(2, 0, 1, 3).reshape(128, 2048))
        for o in range(8):
            put(f"{t}d{o}", wd[:, o * 128:(o + 1) * 128].reshape(NFF, 128, 128).transpose(1, 0, 2).reshape(128, NFF * 128))
    win = inp["w_in"][0]
    QA, KA, VA, QB, KB, VB, GA, GB = 0, 768, 1536, 2304, 2816, 3328, 3840, 4864
    for g in range(3):
        for pr in range(2):
            h0 = 4 * g + 2 * pr
            put(f"dq{g}{pr}", _kchunks(win, QA + h0 * 64, 128))
            put(f"dk{g}{pr}", _kchunks(win, KA + h0 * 64, 128))
        put(f"dv{g}", _kchunks(win, VA + 4 * g * 64, 256))
    for i in range(4):
        put(f"mq{i}", _kchunks(win, QB + i * 128, 128))
        put(f"mk{i}", _kchunks(win, KB + i * 128, 128))
        put(f"mv{i}", _kchunks(win, VB + i * 128, 128))
    for o in range(8):
        put(f"ga{o}", _kchunks(win, GA + o * 128, 128))
        put(f"gb{o}", _kchunks(win, GB + o * 128, 128))
    wua, wub, wo = inp["w_up_a"][0], inp["w_up_b"][0], inp["w_out"][0]
    for o in range(8):
        a = np.zeros((128, 4, 128), np.float32)
        a[:64] = wua[:, o * 128:(o + 1) * 128].reshape(4, 64, 128).transpose(1, 0, 2)
        put(f"ua{o}", a.reshape(128, 512))
        b = np.zeros((128, 8, 128), np.float32)
        b[:64] = wub[:, o * 128:(o + 1) * 128].reshape(8, 64, 128).transpose(1, 0, 2)
        put(f"ub{o}", b.reshape(128, 1024))
        put(f"wo{o}", _kchunks(wo, o * 128, 128))
    return wall


def _const_tables(inp, j):
    cst = np.zeros((128, CF), np.float32)
    for col, key in ((C_G1, "norm_ffn1"), (C_GM, "norm_mix"), (C_G2, "norm_ffn2"), (C_GF, "norm_final")):
        g = np.asarray(inp[key], np.float32).reshape(-1)
        cst[:, col:col + 8] = g.reshape(8, 128).T
    cst[:, C_ID:C_ID + 128] = np.eye(128, dtype=np.float32)
    first_valid_tok = 2048 * (3 - j)
    first_valid_blk = first_valid_tok // 256
    p = np.arange(128)
    blk = np.arange(32)
    for qt in range(16):
        q = qt * 128 + p
        ob = (OWN0 + q) // 256
        ok = (blk[None, :] < ob[:, None]) & (blk[None, :] >= first_valid_blk)
        cst[:, C_GMASK + qt * 32:C_GMASK + (qt + 1) * 32] = np.where(ok, 0.0, NEGG)
        cst[:, C_OWNHOT + qt * 32:C_OWNHOT + (qt + 1) * 32] = (blk[None, :] == ob[:, None]).astype(np.float32)
    t = np.arange(64)[None, :] * 128 + p[:, None]
    cst[:, C_VALM:C_VALM + 64] = (t >= first_valid_tok).astype(np.float32)
    for g, D in enumerate(DIL):
        nq = 16 // D
        for r in range(D):
            for jt in range(nq + 1):
                i = OWN0 // D - 128 + 128 * jt + p
                pos = r + D * i
                cst[:, C_VALD + VALD_OFF[g] + r * (nq + 1) + jt] = (pos >= first_valid_tok).astype(np.float32)
    slopes_a = 2.0 ** (-8.0 * np.arange(1, 13, dtype=np.float64) / 12)
    b = p[:, None].astype(np.float64)
    a = np.arange(128)[None, :].astype(np.float64)
    for h in range(12):
        D = DIL[h // 4]
        s = slopes_a[h]
        cur = np.where(a >= b, np.exp(-s * D * (a - b)), 0.0)
        prev = np.where(a <= b, np.exp(-s * D * (a - b + 128)), 0.0)
        cst[:, C_DEC + h * 256:C_DEC + h * 256 + 128] = cur
        cst[:, C_DEC + h * 256 + 128:C_DEC + (h + 1) * 256] = prev
    bf = ml_dtypes.bfloat16
    tk = np.arange(WIN)
    etab = (tk[None, :] // 256 == np.arange(32)[:, None]).astype(np.float32).astype(bf)
    slopes_b = 2.0 ** (-8.0 * np.arange(1, 9, dtype=np.float64) / 8)
    akt = np.zeros((8, 4, WIN), np.float32)
    aqt = np.zeros((8, 4, NOWN), np.float32)
    tq = OWN0 + np.arange(NOWN)
    for h in range(8):
        s = slopes_b[h]
        akt[h, 0] = s * 128 * (tk // 128)
        akt[h, 1] = s * (tk % 128)
        akt[h, 2] = 1.0
        akt[h, 3] = 1.0
        aqt[h, 0] = 1.0
        aqt[h, 1] = 1.0
        aqt[h, 2] = -s * 128 * (tq // 128)
        aqt[h, 3] = -s * (tq % 128)
    trib = (p[:, None] <= np.arange(128)[None, :]).astype(np.float32).astype(bf)
    return cst, etab, akt.astype(bf), aqt.astype(bf), trib


STOP = [99]
RUN = [set(range(6))]


def _phase(n):
    if n in RUN[0]:
        with ExitStack() as ph:
            yield ph
DBG = [False]
LAST = {}
SUB = [""]


class _Stop(Exception):
    pass


def _chk(n):
    if STOP[0] == n:
        raise _Stop()


def build():
    nc = bass.Bass("TRN2", target_bir_lowering=False)
    xw = nc.dram_tensor("xw", [8, 128, WIN], F32, kind="ExternalInput").ap()
    wall = nc.dram_tensor("wall", [128, TOTF], F32, kind="ExternalInput").ap()
    cstd = nc.dram_tensor("cst", [128, CF], F32, kind="ExternalInput").ap()
    etabd = nc.dram_tensor("etab", [32, WIN], BF16, kind="ExternalInput").ap()
    aktd = nc.dram_tensor("akt", [8, 4, WIN], BF16, kind="ExternalInput").ap()
    aqtd = nc.dram_tensor("aqt", [8, 4, NOWN], BF16, kind="ExternalInput").ap()
    tribd = nc.dram_tensor("trib", [128, 128], BF16, kind="ExternalInput").ap()
    outT = nc.dram_tensor("outT", [8, 128, NOWN], F32, kind="ExternalOutput").ap()
    WB = nc.dram_tensor("wb_s", [128, TOTF], BF16, kind=("Internal" if 0 in RUN[0] else "ExternalInput")).ap()
    sk = "ExternalOutput" if DBG[0] else "Internal"
    R_ = RUN[0]
    kin = lambda prod: sk if prod in R_ else "ExternalInput"
    HS = nc.dram_tensor("h_s", [8, 128, WIN], BF16, kind=kin(1)).ap()
    X1S = nc.dram_tensor("x1_s", [8, 128, NOWN], F32, kind=kin(1)).ap()
    MS = nc.dram_tensor("m_s", [8, 128, NOWN], BF16, kind=kin(4)).ap()
    YAD = nc.dram_tensor("ya_d", [64, 4, NOWN], BF16, kind=kin(2)).ap()
    YBD = nc.dram_tensor("yb_d", [64, 8, NOWN], BF16, kind=kin(3)).ap()
    HSr = HS.rearrange("k p t -> p k t")
    X1r = X1S.rearrange("k p t -> p k t")
    MSr = MS.rearrange("k p t -> p k t")
    xwr = xw.rearrange("k p t -> p k t")
    outr = outT.rearrange("k p t -> p k t")

    try:
        _build_body(nc, xwr, wall, cstd, etabd, aktd, aqtd, tribd, outr, WB, HSr, X1r, MSr, YAD, YBD)
    except _Stop:
        pass
    return nc


def _build_body(nc, xwr, wall, cstd, etabd, aktd, aqtd, tribd, outr, WB, HSr, X1r, MSr, YAD, YBD):
    with ExitStack() as top:
        P = Prog(nc, top)

        def SB(stk, name, shape, dt):
            return stk.enter_context(nc.sbuf_tensor("s_" + name, shape, dt))

        bank = [top.enter_context(nc.psum_tensor(f"bank{i}", [128, 512], F32)) for i in range(8)]
        cst = SB(top, "cst", [128, CF], F32)
        ones = SB(top, "ones", [128, 128], BF16)
        onesf = SB(top, "onesf", [128, 64], F32)
        trib = SB(top, "trib", [128, 128], BF16)

        def DMA(out, in_, reads, writes, q="sp"):
            P.dma(lambda e: e.dma_start(out=out, in_=in_), reads, writes, q=q)

        def MMS(groups, reads, writes):
            def fn(e):
                ins = None
                for out, pairs in groups:
                    n = len(pairs)
                    for i, (l, r) in enumerate(pairs):
                        ins = e.matmul(out, lhsT=l, rhs=r, start=(i == 0), stop=(i == n - 1))
                return ins
            P.op("pe", fn, reads, writes)

        def ACT(out, in_, func, reads, writes, scale=None):
            if scale is None:
                P.op("act", lambda e: e.activation(out=out, in_=in_, func=func), reads, writes)
            else:
                P.op("act", lambda e: e.activation(out=out, in_=in_, func=func, scale=scale), reads, writes)

        def ACP(out, in_, reads, writes, mul=None):
            if mul is None:
                P.op("act", lambda e: e.copy(out=out, in_=in_), reads, writes)
            else:
                P.op("act", lambda e: e.mul(out=out, in_=in_, mul=mul), reads, writes)

        def TT(out, in0, in1, op, reads, writes, eng="dve"):
            P.op(eng, lambda e: e.tensor_tensor(out=out, in0=in0, in1=in1, op=op), reads, writes)

        def TS(out, in0, s1, s2, op0, op1, reads, writes, eng="dve"):
            if op1 is None:
                P.op(eng, lambda e: e.tensor_scalar(out=out, in0=in0, scalar1=s1, scalar2=None, op0=op0), reads, writes)
            else:
                P.op(eng, lambda e: e.tensor_scalar(out=out, in0=in0, scalar1=s1, scalar2=s2, op0=op0, op1=op1), reads, writes)

        def STT(out, in0, scalar, in1, op0, op1, reads, writes):
            P.op("dve", lambda e: e.scalar_tensor_tensor(out=out, in0=in0, scalar=scalar, in1=in1, op0=op0, op1=op1), reads, writes)

        def CP(out, in_, reads, writes, eng="dve"):
            P.op(eng, lambda e: e.tensor_copy(out=out, in_=in_), reads, writes)

        def RCP(out, in_, reads, writes):
            P.op("dve", lambda e: e.reciprocal(out=out, in_=in_), reads, writes)

        def MSET(ap, val, writes, eng="pool"):
            P.op(eng, lambda e: e.memset(ap, val), (), writes)

        def wres(name):
            o, w = WLAY[name]
            return [f"WB{b}" for b in range(o // CB, (o + w - 1) // CB + 1)]

        def WLOAD(dst, name, writes, q="sp"):
            o, w = WLAY[name]
            DMA(dst, WB[:, o:o + w], wres(name), writes, q=q)

        DMA(cst[:], cstd, (), ["cst"])
        DMA(trib[:], tribd, (), ["trib"])
        MSET(ones[:], 1.0, ["ones"])
        MSET(onesf[:], 1.0, ["onesf"])
        for ph in _phase(0):
            stg = [SB(ph, f"stg{i}", [128, CB], F32) for i in range(3)]
            cvt = [SB(ph, f"cvt{i}", [128, CB], BF16) for i in range(3)]
            nblk = (TOTF + CB - 1) // CB
            engs = ("dve", "act")
            for b in range(nblk):
                s = b % 3
                c0 = b * CB
                w = min(CB, TOTF - c0)
                DMA(stg[s][:, 0:w], wall[:, c0:c0 + w], (), [f"stg{s}"])
                en = engs[b % 2]
                if en == "act":
                    ACP(cvt[s][:, 0:w], stg[s][:, 0:w], [f"stg{s}"], [f"cvt{s}"])
                else:
                    CP(cvt[s][:, 0:w], stg[s][:, 0:w], [f"stg{s}"], [f"cvt{s}"], eng=en)
                DMA(WB[:, c0:c0 + w], cvt[s][:, 0:w], [f"cvt{s}"], [f"WB{b}"], q="pool" if b % 2 else "sp")
            P.flush()
            _chk(0)

        def rmsnorm(tag, X, xr, gcol, out, outr, bufs, n=ST):
            sq, t1, t2 = bufs["sq"], bufs["t1"], bufs["t2"]
            nh = n // 512
            for c in range(8):
                s = c % 2
                ACT(sq[s][:, 0:n], X[:, c, :], AF.Square, [xr(c)], [f"{tag}sq{s}"])
                for hf in range(nh):
                    P.op("pe", (lambda hf=hf, s=s, c=c: (lambda e: e.matmul(bank[hf][:, :], lhsT=ones[:], rhs=sq[s][:, hf * 512:(hf + 1) * 512],
                                                                        start=(c == 0), stop=(c == 7))))(),
                         ["ones", f"{tag}sq{s}"], [f"ps{hf}"])
            for hf in range(nh):
                TS(t1[:, hf * 512:(hf + 1) * 512], bank[hf][:, :], 1.0 / DM, EPS, ALU.mult, ALU.add, [f"ps{hf}"], [f"{tag}t1"])
            ACT(t2[:, 0:n], t1[:, 0:n], AF.Sqrt, [f"{tag}t1"], [f"{tag}t2"])
            RCP(t1[:, 0:n], t2[:, 0:n], [f"{tag}t2"], [f"{tag}t1"])
            for c in range(8):
                STT(out[:, c, :], X[:, c, :], cst[:, gcol + c:gcol + c + 1], t1[:, 0:n], ALU.mult, ALU.mult,
                    [xr(c), "cst", f"{tag}t1"], [outr(c)])

        def ffn(tag, X, xr, gcol, wt, bufs):
            h0, act, sg, wgu, wd = bufs["h0"], bufs["act"], bufs["sg"], bufs["wgu"], bufs["wd"]
            h0r = lambda c: f"{tag}h0_{c}"
            rmsnorm(tag, X, xr, gcol, h0, h0r, bufs)
            h0all = [h0r(c) for c in range(8)]
            WLOAD(wgu[0][:], f"{wt}gu0", [f"{tag}wgu0"])
            it = 0
            for f in range(NFF):
                s = f % 2
                if f + 1 < NFF:
                    WLOAD(wgu[1 - s][:], f"{wt}gu{f + 1}", [f"{tag}wgu{1 - s}"])
                elif True:
                    WLOAD(wd[0][:], f"{wt}d0", [f"{tag}wd0"])
                for hf in range(2):
                    pb = 2 + 2 * (it % 2)
                    it += 1
                    cols = slice(hf * 512, (hf + 1) * 512)
                    w4 = wgu[s][:].rearrange("p (g k c) -> p g k c", g=2, k=8)
                    MMS([(bank[pb][:, :], [(w4[:, 0, k, :], h0[:, k, cols]) for k in range(8)]),
                         (bank[pb + 1][:, :], [(w4[:, 1, k, :], h0[:, k, cols]) for k in range(8)])],
                        [f"{tag}wgu{s}"] + h0all, [f"ps{pb}", f"ps{pb + 1}"])
                    ss_ = it % 2
                    ACT(sg[ss_][:, :], bank[pb][:, :], AF.Silu, [f"ps{pb}"], [f"{tag}sg{ss_}"])
                    TT(act[:, f, cols], sg[ss_][:, :], bank[pb + 1][:, :], ALU.mult, [f"{tag}sg{ss_}", f"ps{pb + 1}"], [f"{tag}act{f}_{hf}"])
            actall = [f"{tag}act{f}_{hf}" for f in range(NFF) for hf in range(2)]
            it = 0
            for o in range(8):
                s = o % 2
                if o + 1 < 8:
                    WLOAD(wd[1 - s][:], f"{wt}d{o + 1}", [f"{tag}wd{1 - s}"])
                w3 = wd[s][:].rearrange("p (f c) -> p f c", f=NFF)
                for hf in range(2):
                    pb = 6 + (it % 2)
                    it += 1
                    cols = slice(hf * 512, (hf + 1) * 512)
                    MMS([(bank[pb][:, :], [(w3[:, f, :], act[:, f, cols]) for f in range(NFF)])],
                        [f"{tag}wd{s}"] + actall, [f"ps{pb}"])
                    STT(X[:, o, cols], bank[pb][:, :], 0.5, X[:, o, cols], ALU.mult, ALU.add, [f"ps{pb}", xr(o)], [xr(o)])

        def ffn_bufs(stk, tag):
            return dict(
                h0=SB(stk, tag + "h0", [128, 8, ST], BF16),
                act=SB(stk, tag + "act", [128, NFF, ST], BF16),
                sq=[SB(stk, f"{tag}sq{i}", [128, ST], BF16) for i in range(2)],
                t1=SB(stk, tag + "t1", [128, ST], F32),
                t2=SB(stk, tag + "t2", [128, ST], F32),
                sg=[SB(stk, f"{tag}sg{i}", [128, 512], F32) for i in range(2)],
                wgu=[SB(stk, f"{tag}wgu{i}", [128, 2048], BF16) for i in range(2)],
                wd=[SB(stk, f"{tag}wd{i}", [128, NFF * 128], BF16) for i in range(2)],
            )

        for ph in _phase(1):
            bufs = ffn_bufs(ph, "a")
            Xb = [SB(ph, f"aX{i}", [128, 8, ST], F32) for i in range(2)]
            nst = WIN // ST
            DMA(Xb[0][:], xwr[:, :, 0:ST], (), [f"aX0_{c}" for c in range(8)])
            for st in range(nst):
                s = st % 2
                X = Xb[s]
                xr = (lambda s: (lambda c: f"aX{s}_{c}"))(s)
                if st + 1 < nst:
                    DMA(Xb[1 - s][:], xwr[:, :, (st + 1) * ST:(st + 2) * ST], (), [f"aX{1 - s}_{c}" for c in range(8)])
                ffn("a", X, xr, C_G1, "f1", bufs)
                if st * ST >= OWN0:
                    DMA(X1r[:, :, st * ST - OWN0:(st + 1) * ST - OWN0], X[:], [xr(c) for c in range(8)], [f"X1S{st}"])
                h0r = lambda c: f"ah0_{c}"
                rmsnorm("a", X, xr, C_GM, bufs["h0"], h0r, bufs)
                DMA(HSr[:, :, st * ST:(st + 1) * ST], bufs["h0"][:], [h0r(c) for c in range(8)], [f"HS{st}"])
            P.flush()
            _chk(1)

        with ExitStack() as mid:
            YA = SB(mid, "YA", [64, 4, NOWN], BF16)
            for ph in _phase(2):
                hb = SB(ph, "d_hb", [128, 8, 2048], BF16)
                ACC = SB(ph, "d_acc", [128, 4, NOWN], F32)
                QT = [SB(ph, f"d_qt{i}", [128, 2048], BF16) for i in range(2)]
                KT = [SB(ph, f"d_kt{i}", [128, 2048 + 128 * 16], BF16) for i in range(2)]
                VA = SB(ph, "d_va", [128, 32, 4, 65], BF16)
                wq = [SB(ph, f"d_wq{i}", [128, 8, 128], BF16) for i in range(2)]
                wk = [SB(ph, f"d_wk{i}", [128, 8, 128], BF16) for i in range(2)]
                wv = SB(ph, "d_wv", [128, 8, 256], BF16)
                et = [SB(ph, f"d_et{i}", [128, 2, 2, 128], F32) for i in range(2)]
                PT = [SB(ph, f"d_pt{i}", [128, 2, 2, 128], BF16) for i in range(4)]
                rd = SB(ph, "d_rd", [128, 512], F32)
                dec = cst[:, C_DEC:C_DEC + 3072].rearrange("p (h a b) -> p h a b", h=12, a=2)
                pcnt = [0]

                def nbank():
                    pcnt[0] += 1
                    return pcnt[0] % 4

                for g, D in enumerate(DIL):
                    npr = NOWN // D
                    nq = 16 // D
                    KW = npr + 128
                    ntile = D * (nq + 1)
                    for pr in range(2):
                        WLOAD(wq[pr][:].rearrange("p k c -> p (k c)"), f"dq{g}{pr}", [f"d_wq{pr}"])
                        WLOAD(wk[pr][:].rearrange("p k c -> p (k c)"), f"dk{g}{pr}", [f"d_wk{pr}"])
                    WLOAD(wv[:].rearrange("p k c -> p (k c)"), f"dv{g}", ["d_wv"])
                    for hd in range(4):
                        CP(VA[:, 0:ntile, hd, 64], cst[:, C_VALD + VALD_OFF[g]:C_VALD + VALD_OFF[g] + ntile], ["cst"], ["d_va"], eng="pool")
                    for half in range(2):
                        hs_res = ["HS4", "HS5"] if half == 0 else ["HS6", "HS7"]
                        DMA(hb[:], HSr[:, :, 4096 + 2048 * half:4096 + 2048 * (half + 1)], hs_res, ["d_hb"])
                        if half == 0:
                            col0 = lambda r: 2048 - 128 * D + r
                            ncol = 128
                            kdst0 = 0
                        else:
                            col0 = lambda r: r
                            ncol = npr
                            kdst0 = 128
                        for pr in range(2):
                            rpb = max(1, 512 // ncol)
                            for r0 in range(0, D, rpb):
                                rs = list(range(r0, min(D, r0 + rpb)))
                                for c0 in range(0, ncol, 512):
                                    cn = min(512, ncol - c0)
                                    targets = [("k", wk[pr], KT[pr])] + ([("q", wq[pr], QT[pr])] if half == 1 else [])
                                    for kind, wt_, dst in targets:
                                        b = nbank()
                                        groups = []
                                        for ri, r in enumerate(rs):
                                            groups.append((bank[b][:, ri * cn:(ri + 1) * cn],
                                                           [(wt_[:, k, :], hb[:, k, ss(col0(r) + D * c0, cn, D)]) for k in range(8)]))
                                        MMS(groups, ["d_hb", f"d_w{kind}{pr}"], [f"ps{b}"])
                                        for ri, r in enumerate(rs):
                                            src = bank[b][:, ri * cn:(ri + 1) * cn]
                                            if kind == "k":
                                                CP(KT[pr][:, r * KW + kdst0 + c0:r * KW + kdst0 + c0 + cn], src, [f"ps{b}"], [f"d_kt{pr}"])
                                            else:
                                                ACP(QT[pr][:, r * npr + c0:r * npr + c0 + cn], src, [f"ps{b}"], [f"d_qt{pr}"], mul=0.125)
                        jts = [0] if half == 0 else list(range(1, nq + 1))
                        for r in range(D):
                            for jt in jts:
                                c_first = (2048 - 128 * D + r) if half == 0 else (r + D * 128 * (jt - 1))
                                b = nbank()
                                MMS([(bank[b][:, 0:256], [(hb[:, k, ss(c_first, 128, D)], wv[:, k, :]) for k in range(8)])],
                                    ["d_hb", "d_wv"], [f"ps{b}"])
                                t_ = r * (nq + 1) + jt
                                src = bank[b][:, 0:256].rearrange("p (h d) -> p h d", h=4)
                                if (r + jt) % 2:
                                    ACP(VA[:, t_, :, 0:64], src, [f"ps{b}"], ["d_va"])
                                else:
                                    CP(VA[:, t_, :, 0:64], src, [f"ps{b}"], ["d_va"])
                    gcount = 0
                    for pr in range(2):
                        ob = [4 + 2 * pr, 5 + 2 * pr]
                        qslot = 0
                        ptc = 0
                        prev_pt = None
                        for r in range(D):
                            for jt in range(nq + 1):
                                halves = [1] if jt == 0 else ([0] if jt == nq else [0, 1])
                                h0_, h1_ = halves[0], halves[-1] + 1
                                qc0 = r * npr + 128 * (jt - 1 + h0_)
                                nqc = 128 * len(halves)
                                sb0 = 2 * (gcount % 2)
                                sv = [bank[sb0 + e_][:, 0:256].rearrange("p (a b) -> p a b", a=2) for e_ in range(2)]
                                kc = r * KW + 128 * jt
                                MMS([(sv[e_][:, h0_:h1_, :], [(KT[pr][64 * e_:64 * e_ + 64, kc:kc + 128], QT[pr][64 * e_:64 * e_ + 64, qc0:qc0 + nqc])])
                                     for e_ in range(2)], [f"d_kt{pr}", f"d_qt{pr}"], [f"ps{sb0}", f"ps{sb0 + 1}"])
                                es = gcount % 2
                                ps_ = ptc % 4
                                ptc += 1
                                gcount += 1
                                for e_ in range(2):
                                    ACT(et[es][:, e_, h0_:h1_, :], sv[e_][:, h0_:h1_, :], AF.Exp, [f"ps{sb0 + e_}"], [f"d_et{es}_{e_}"])
                                hh0 = 4 * g + 2 * pr
                                TT(PT[ps_][:, :, h0_:h1_, :], et[es][:, :, h0_:h1_, :], dec[:, hh0:hh0 + 2, h0_:h1_, :], ALU.mult,
                                   [f"d_et{es}_0", f"d_et{es}_1", "cst"], [f"d_pt{ps_}"])
                                if jt >= 1:
                                    qt_ = jt - 1
                                    t_prev = r * (nq + 1) + jt - 1
                                    t_cur = r * (nq + 1) + jt
                                    pp = prev_pt
                                    groups = []
                                    for e_ in range(2):
                                        hd = 2 * pr + e_
                                        groups.append((bank[ob[e_]][0:65, qslot * 128:(qslot + 1) * 128],
                                                       [(VA[:, t_prev, hd, :], PT[pp][:, e_, 1, :]), (VA[:, t_cur, hd, :], PT[ps_][:, e_, 0, :])]))
                                    MMS(groups, ["d_va", f"d_pt{pp}", f"d_pt{ps_}"], [f"ps{ob[0]}", f"ps{ob[1]}"])
                                    qslot += 1
                                    if qslot == 4:
                                        qslot = 0
                                        for e_ in range(2):
                                            hd = 2 * pr + e_
                                            if D == 1:
                                                m = qt_ // 4
                                                dst = ACC[0:65, hd, m * 512:(m + 1) * 512]
                                                src = bank[ob[e_]][0:65, :]
                                            elif D == 4:
                                                dst = ACC[0:65, hd, ss(r, 512, 4)]
                                                src = bank[ob[e_]][0:65, :]
                                            else:
                                                r0 = r - 3
                                                dst = ACC[0:65, hd, :].rearrange("p (i r) -> p r i", r=16)[:, r0:r0 + 4, :]
                                                src = bank[ob[e_]][0:65, :].rearrange("p (r i) -> p r i", r=4)
                                            if g == 0:
                                                CP(dst, src, [f"ps{ob[e_]}"], [f"d_acc{hd}"])
                                            else:
                                                TT(dst, src, dst, ALU.add, [f"ps{ob[e_]}", f"d_acc{hd}"], [f"d_acc{hd}"])
                                prev_pt = ps_
                for hd in range(4):
                    for m in range(4):
                        cols = slice(m * 512, (m + 1) * 512)
                        RCP(rd[64:65, :], ACC[64:65, hd, cols], [f"d_acc{hd}"], ["d_rd"])
                        b = nbank()
                        MMS([(bank[b][0:64, :], [(onesf[64:65, 0:64], rd[64:65, :])])], ["onesf", "d_rd"], [f"ps{b}"])
                        TT(YA[0:64, hd, cols], ACC[0:64, hd, cols], bank[b][0:64, :], ALU.mult, [f"d_acc{hd}", f"ps{b}"], [f"YA{hd}"])
                if DBG[0]:
                    DMA(YAD, YA[:], [f"YA{hd}" for hd in range(4)], ["YAD"])
                P.flush()
                _chk(2)

            YB = SB(mid, "YB", [64, 8, NOWN], BF16)
            for ph in _phase(3):
                KAB = [SB(ph, f"m_k{i}", [128, WIN], BF16) for i in range(2)]
                QAB = [SB(ph, f"m_q{i}", [128, NOWN], BF16) for i in range(2)]
                VAB = SB(ph, "m_v", [128, 64, 2, 65], BF16)
                hbuf = [SB(ph, f"m_hb{i}", [128, 8, 512], BF16) for i in range(2)]
                wq = SB(ph, "m_wq", [128, 8, 128], BF16)
                wk = SB(ph, "m_wk", [128, 8, 128], BF16)
                wv = SB(ph, "m_wv", [128, 8, 128], BF16)
                QF = SB(ph, "m_qf", [128, 512], F32)
                KM = SB(ph, "m_km", [128, 32], F32)
                g2 = SB(ph, "m_g2", [128, 2, 32], F32)
                mx = SB(ph, "m_mx", [128, 2, 8], F32)
                thr = SB(ph, "m_thr", [128, 2, 1], F32)
                sel = SB(ph, "m_sel", [128, 2, 32], F32)
                ZA = SB(ph, "m_za", [128, 96], F32)
                ZB = SB(ph, "m_zb", [128, 32], F32)
                PTm = [SB(ph, f"m_pt{i}", [128, 512], BF16) for i in range(6)]
                osb = SB(ph, "m_osb", [128, 512], F32)
                rdm = SB(ph, "m_rd", [128, 512], F32)
                erow = (slice(64, 96), slice(0, 32))
                arow = (slice(96, 100), slice(32, 36))
                drow = (slice(0, 64), slice(64, 128))
                krows = (slice(0, 100), slice(0, 128))
                if "S1" not in SUB[0]:
                    MSET(KAB[1][32:64, :], 0.0, ["m_k1x"])
                    MSET(QAB[1][32:64, :], 0.0, ["m_q1x"])
                MSET(ZA[:], 0.0, ["m_za"])
                MSET(KM[:], 0.0, ["m_km"])
                if "S2" not in SUB[0]:
                    DMA(KAB[0][64:96, :], etabd, (), ["m_k0e"])
                    DMA(KAB[1][0:32, :], etabd, (), ["m_k1e"])
                for e_ in (range(2) if "S3" not in SUB[0] else ()):
                    CP(VAB[:, :, e_, 64], cst[:, C_VALM:C_VALM + 64], ["cst"], ["m_vval"], eng="pool")
                sring = [0]
                for i in (range(4) if "P1" not in SUB[0] else range(1)):
                    WLOAD(wq[:].rearrange("p k c -> p (k c)"), f"mq{i}", ["m_wq"])
                    WLOAD(wk[:].rearrange("p k c -> p (k c)"), f"mk{i}", ["m_wk"])
                    WLOAD(wv[:].rearrange("p k c -> p (k c)"), f"mv{i}", ["m_wv"])
                    for e_ in (range(2) if "L" not in SUB[0] else ()):
                        DMA(KAB[e_][arow[e_], :], aktd[2 * i + e_], ["m_k1x"], [f"m_k{e_}a"])
                        DMA(QAB[e_][arow[e_], :], aqtd[2 * i + e_], ["m_q1x"], [f"m_q{e_}a"])
                    DMA(hbuf[0][:], HSr[:, :, 0:512], ["HS0"], ["m_hb0"])
                    for s2 in range(16):
                        hs = s2 % 2
                        H_ = hbuf[hs]
                        if s2 + 1 < 16:
                            DMA(hbuf[1 - hs][:], HSr[:, :, (s2 + 1) * 512:(s2 + 2) * 512], [f"HS{(s2 + 1) // 2}"], [f"m_hb{1 - hs}"])
                        cols = slice(s2 * 512, (s2 + 1) * 512)
                        if "K" not in SUB[0]:
                          MMS([(bank[6][:, :], [(wk[:, k, :], H_[:, k, :]) for k in range(8)])], [f"m_hb{hs}", "m_wk"], ["ps6"])
                        ACP(KAB[0][0:64, cols], bank[6][0:64, :], ["ps6"], ["m_k0d"])
                        CP(KAB[1][64:128, cols], bank[6][64:128, :], ["ps6"], ["m_k1d"])
                        P.op("dve", (lambda s2=s2: (lambda e: e.tensor_reduce(out=KM[:, 2 * s2:2 * s2 + 2],
                                                                              in_=bank[6][:, :].rearrange("p (a b) -> p a b", a=2),
                                                                              axis=AX.X, op=ALU.add)))(), ["ps6"], ["m_km"])
                        MMS([(bank[7][:, tt * 128:(tt + 1) * 128], [(H_[:, k, tt * 128:(tt + 1) * 128], wv[:, k, :]) for k in range(8)])
                             for tt in range(4)], [f"m_hb{hs}", "m_wv"], ["ps7"])
                        v4 = bank[7][:, :].rearrange("p (t e d) -> p t e d", t=4, e=2)
                        ACP(VAB[:, 4 * s2:4 * s2 + 4, 0, 0:64], v4[:, :, 0, :], ["ps7"], ["m_v0"])
                        if "T0" not in SUB[0]:
                            ACP(VAB[:, 4 * s2:4 * s2 + 4, 1, 0:64], v4[:, :, 1, :], ["ps7"], ["m_v1"])
                        elif "T2" in SUB[0]:
                            TS(VAB[:, 4 * s2:4 * s2 + 4, 1, 0:64], v4[:, :, 1, :], 1.0, None, ALU.mult, None, ["ps7"], ["m_v1"])
                        elif "T3" in SUB[0]:
                            for tt_ in range(4):
                                CP(VAB[:, 4 * s2 + tt_, 1, 0:64], v4[:, tt_, 1, :], ["ps7"], ["m_v1"])
                        else:
                            CP(VAB[:, 4 * s2:4 * s2 + 4, 1, 0:64], v4[:, :, 1, :], ["ps7"], ["m_v1"])
                        if s2 >= 12:
                            qi = s2 - 12
                            qcols = slice(qi * 512, (qi + 1) * 512)
                            MMS([(bank[5][:, :], [(wq[:, k, :], H_[:, k, :]) for k in range(8)])], [f"m_hb{hs}", "m_wq"], ["ps5"])
                            ACP(QAB[0][0:64, qcols], bank[5][0:64, :], ["ps5"], ["m_q0d"], mul=0.125)
                            TS(QAB[1][64:128, qcols], bank[5][64:128, :], 0.125, None, ALU.mult, None, ["ps5"], ["m_q1d"])
                            ACP(QF[:, :], bank[5][:, :], ["ps5"], ["m_qf"], mul=0.125)
                            for sub in (range(4) if "G" not in SUB[0] else ()):
                                qt16 = 4 * qi + sub
                                sc = slice(sub * 128, (sub + 1) * 128)
                                MMS([(bank[4 + e_][:, 0:32], [(QF[drow[e_], sc], KM[drow[e_], :])]) for e_ in range(2)],
                                    ["m_qf", "m_km"], ["ps4", "ps5"])
                                for e_ in range(2):
                                    TT(g2[:, e_, :], bank[4 + e_][:, 0:32], cst[:, C_GMASK + 32 * qt16:C_GMASK + 32 * qt16 + 32],
                                       ALU.add, [f"ps{4 + e_}", "cst"], ["m_g2"])
                                for e_ in range(2):
                                    P.op("dve", (lambda e_=e_: (lambda e: e.max(out=mx[:, e_, :], in_=g2[:, e_, :])))(), ["m_g2"], ["m_mx"])
                                TS(thr[:, :, :], mx[:, :, 2:3], -1.0e29, None, ALU.max, None, ["m_mx"], ["m_thr"])
                                for e_ in range(2):
                                    TS(sel[:, e_, :], g2[:, e_, :], thr[:, e_, :], None, ALU.is_ge, None, ["m_g2", "m_thr"], ["m_sel"])
                                    TT(sel[:, e_, :], sel[:, e_, :], cst[:, C_OWNHOT + 32 * qt16:C_OWNHOT + 32 * qt16 + 32], ALU.add,
                                       ["m_sel", "cst"], ["m_sel"])
                                TS(ZA[:, 64:96], sel[:, 0, :], BIG, -BIG, ALU.mult, ALU.add, ["m_sel"], ["m_za"])
                                TS(ZB[:, :], sel[:, 1, :], BIG, -BIG, ALU.mult, ALU.add, ["m_sel"], ["m_zb"])
                                P.op("pe", (lambda: (lambda e: e.transpose(bank[3][0:96, 0:128], ZA[:], cst[:, C_ID:C_ID + 128])))(),
                                     ["m_za", "cst"], ["ps3"])
                                P.op("pe", (lambda: (lambda e: e.transpose(bank[3][0:32, 128:256], ZB[:], cst[:, C_ID:C_ID + 128])))(),
                                     ["m_zb", "cst"], ["ps3"])
                                qc = slice(qi * 512 + sub * 128, qi * 512 + (sub + 1) * 128)
                                ACP(QAB[0][64:96, qc], bank[3][64:96, 0:128], ["ps3"], ["m_q0n"])
                                ACP(QAB[1][0:32, qc], bank[3][0:32, 128:256], ["ps3"], ["m_q1n"])
                    for e_ in (range(2) if "A" not in SUB[0] else ()):
                        h = 2 * i + e_
                        kres = [f"m_k{e_}d", f"m_k{e_}a", f"m_k{e_}e", "m_k1x"]
                        qres = [f"m_q{e_}d", f"m_q{e_}a", f"m_q{e_}n", "m_q1x"]
                        rows = krows[e_]
                        for qi in range(4):
                            ob_ = 3 + 0
                            ob_ = 4 if (qi % 2 == 0) else 5
                            nfull = 48 + 4 * qi
                            nk = nfull + 4
                            slots = {}

                            def emit_qk(kt, e_=e_, qi=qi, nfull=nfull, rows=rows, kres=kres, qres=qres, slots=slots):
                                di = kt - nfull
                                c0 = 128 * di if di >= 0 else 0
                                sb_ = sring[0] % 4
                                pt_ = sring[0] % 6
                                sring[0] += 1
                                slots[kt] = (c0, pt_)
                                MMS([(bank[sb_][:, c0:512], [(KAB[e_][rows, kt * 128:(kt + 1) * 128], QAB[e_][rows, qi * 512 + c0:(qi + 1) * 512])])],
                                    kres + qres, [f"ps{sb_}"])
                                ACT(PTm[pt_][:, c0:512], bank[sb_][:, c0:512], AF.Exp, [f"ps{sb_}"], [f"m_pt{pt_}"])
                                if di >= 0:
                                    TT(PTm[pt_][:, c0:c0 + 128], PTm[pt_][:, c0:c0 + 128], trib[:, :], ALU.mult, [f"m_pt{pt_}", "trib"], [f"m_pt{pt_}"],
                                       eng="pool")

                            def emit_pv(kt, e_=e_, ob_=ob_, nk=nk, slots=slots):
                                c0, pt_ = slots[kt]
                                P.op("pe", (lambda kt=kt, c0=c0, pt_=pt_, ob_=ob_, nk=nk, e_=e_:
                                            (lambda e: e.matmul(bank[ob_][0:65, c0:512], lhsT=VAB[:, kt, e_, :], rhs=PTm[pt_][:, c0:512],
                                                                start=(kt == 0), stop=(kt == nk - 1))))(),
                                     [f"m_v{e_}", "m_vval", f"m_pt{pt_}"], [f"ps{ob_}"])

                            LA = 3
                            for kt in range(min(LA, nk)):
                                emit_qk(kt)
                            for kt in range(nk):
                                if kt + LA < nk:
                                    emit_qk(kt + LA)
                                emit_pv(kt)
                            ACP(osb[0:65, :], bank[ob_][0:65, :], [f"ps{ob_}"], ["m_osb"])
                            RCP(rdm[64:65, :], osb[64:65, :], ["m_osb"], ["m_rd"])
                            MMS([(bank[7][0:64, :], [(onesf[64:65, 0:64], rdm[64:65, :])])], ["onesf", "m_rd"], ["ps7"])
                            TT(YB[0:64, h, qi * 512:(qi + 1) * 512], osb[0:64, :], bank[7][0:64, :], ALU.mult, ["m_osb", "ps7"], [f"YB{h}"])
                if DBG[0]:
                    DMA(YBD, YB[:], [f"YB{h}" for h in range(8)], ["YBD"])
                P.flush()
                _chk(3)

            for ph in _phase(4):
                hown = [SB(ph, f"g_h{i}", [128, 8, ST], BF16) for i in range(2)]
                mg = SB(ph, "g_mg", [128, 8, ST], BF16)
                wga = [SB(ph, f"g_wga{i}", [128, 8, 128], BF16) for i in range(2)]
                wgb = [SB(ph, f"g_wgb{i}", [128, 8, 128], BF16) for i in range(2)]
                wua = [SB(ph, f"g_wua{i}", [128, 4, 128], BF16) for i in range(2)]
                wub = [SB(ph, f"g_wub{i}", [128, 8, 128], BF16) for i in range(2)]
                sga = [SB(ph, f"g_sa{i}", [128, 512], F32) for i in range(2)]
                sgb = [SB(ph, f"g_sb{i}", [128, 512], F32) for i in range(2)]
                yall = [f"YA{hd}" for hd in range(4)] + [f"YB{h}" for h in range(8)]
                if 2 not in RUN[0]:
                    DMA(YA[:], YAD, (), [f"YA{hd}" for hd in range(4)])
                if 3 not in RUN[0]:
                    DMA(YB[:], YBD, (), [f"YB{h}" for h in range(8)])
                it = 0
                for stl in range(2):
                    H_ = hown[stl]
                    DMA(H_[:], HSr[:, :, OWN0 + stl * ST:OWN0 + (stl + 1) * ST], [f"HS{6 + stl}"], [f"g_h{stl}"])
                    for o in range(8):
                        s = o % 2
                        WLOAD(wga[s][:].rearrange("p k c -> p (k c)"), f"ga{o}", [f"g_wga{s}"])
                        WLOAD(wgb[s][:].rearrange("p k c -> p (k c)"), f"gb{o}", [f"g_wgb{s}"])
                        WLOAD(wua[s][:].rearrange("p k c -> p (k c)"), f"ua{o}", [f"g_wua{s}"])
                        WLOAD(wub[s][:].rearrange("p k c -> p (k c)"), f"ub{o}", [f"g_wub{s}"])
                        for hf in range(2):
                            b0 = 4 * (it % 2)
                            ts_ = it % 2
                            it += 1
                            lc = slice(hf * 512, (hf + 1) * 512)
                            gc = slice(stl * ST + hf * 512, stl * ST + (hf + 1) * 512)
                            MMS([(bank[b0][:, :], [(wua[s][0:64, hd, :], YA[0:64, hd, gc]) for hd in range(4)]),
                                 (bank[b0 + 1][:, :], [(wub[s][0:64, h, :], YB[0:64, h, gc]) for h in range(8)]),
                                 (bank[b0 + 2][:, :], [(wga[s][:, k, :], H_[:, k, lc]) for k in range(8)]),
                                 (bank[b0 + 3][:, :], [(wgb[s][:, k, :], H_[:, k, lc]) for k in range(8)])],
                                yall + [f"g_h{stl}", f"g_wga{s}", f"g_wgb{s}", f"g_wua{s}", f"g_wub{s}"],
                                [f"ps{b0 + q_}" for q_ in range(4)])
                            ACT(sga[ts_][:, :], bank[b0 + 2][:, :], AF.Sigmoid, [f"ps{b0 + 2}"], [f"g_sa{ts_}"])
                            ACT(sgb[ts_][:, :], bank[b0 + 3][:, :], AF.Sigmoid, [f"ps{b0 + 3}"], [f"g_sb{ts_}"])
                            TT(sga[ts_][:, :], sga[ts_][:, :], bank[b0][:, :], ALU.mult, [f"g_sa{ts_}", f"ps{b0}"], [f"g_sa{ts_}"])
                            TT(sgb[ts_][:, :], sgb[ts_][:, :], bank[b0 + 1][:, :], ALU.mult, [f"g_sb{ts_}", f"ps{b0 + 1}"], [f"g_sb{ts_}"])
                            TT(mg[:, o, lc], sga[ts_][:, :], sgb[ts_][:, :], ALU.add, [f"g_sa{ts_}", f"g_sb{ts_}"], [f"g_mg{o}"], eng="pool")
                    DMA(MSr[:, :, stl * ST:(stl + 1) * ST], mg[:], [f"g_mg{o}" for o in range(8)], [f"MS{stl}"])
                P.flush()
                _chk(4)

        for ph in _phase(5):
            bufs = ffn_bufs(ph, "b")
            X = SB(ph, "bX", [128, 8, ST], F32)
            mgt = SB(ph, "b_mg", [128, 8, ST], BF16)
            wo = [SB(ph, f"b_wo{i}", [128, 8, 128], BF16) for i in range(2)]
            xr = lambda c: f"bX_{c}"
            for stl in range(2):
                DMA(X[:], X1r[:, :, stl * ST:(stl + 1) * ST], [f"X1S{6 + stl}"], [xr(c) for c in range(8)])
                DMA(mgt[:], MSr[:, :, stl * ST:(stl + 1) * ST], [f"MS{stl}"], ["b_mg"])
                it = 0
                for o2 in range(8):
                    s = o2 % 2
                    WLOAD(wo[s][:].rearrange("p k c -> p (k c)"), f"wo{o2}", [f"b_wo{s}"])
                    for hf in range(2):
                        pb = 6 + (it % 2)
                        it += 1
                        cols = slice(hf * 512, (hf + 1) * 512)
                        MMS([(bank[pb][:, :], [(wo[s][:, o, :], mgt[:, o, cols]) for o in range(8)])], ["b_mg", f"b_wo{s}"], [f"ps{pb}"])
                        TT(X[:, o2, cols], bank[pb][:, :], X[:, o2, cols], ALU.add, [f"ps{pb}", xr(o2)], [xr(o2)])
                ffn("b", X, xr, C_G2, "f2", bufs)
                rmsnorm("b", X, xr, C_GF, X, xr, bufs)
                DMA(outr[:, :, stl * ST:(stl + 1) * ST], X[:], [xr(c) for c in range(8)], [f"OUT{stl}"])
            P.flush()
            _chk(5)
    return nc


_NC_CACHE = {}


def kernel(x, norm_ffn1, ffn1_gate, ffn1_up, ffn1_down, norm_mix, w_in, w_up_a, w_up_b, w_out,
           norm_ffn2, ffn2_gate, ffn2_up, ffn2_down, norm_final):
    inp = dict(x=x, norm_ffn1=norm_ffn1, ffn1_gate=ffn1_gate, ffn1_up=ffn1_up, ffn1_down=ffn1_down,
               norm_mix=norm_mix, w_in=w_in, w_up_a=w_up_a, w_up_b=w_up_b, w_out=w_out, norm_ffn2=norm_ffn2,
               ffn2_gate=ffn2_gate, ffn2_up=ffn2_up, ffn2_down=ffn2_down, norm_final=norm_final)
    inp = {k: np.asarray(v, dtype=np.float32) for k, v in inp.items()}
    wall = _pack_weights(inp)
    in_maps = []
    for c in range(8):
        b, j = c // 4, c % 4
        end = 2048 * (j + 1)
        xwin = np.zeros((WIN, DM), np.float32)
        xwin[WIN - end:] = inp["x"][b, 0:end]
        xw = np.ascontiguousarray(xwin.T).reshape(8, 128, WIN)
        cst, etab, akt, aqt, trib = _const_tables(inp, j)
        in_maps.append(dict(xw=xw, wall=wall, cst=cst, etab=etab, akt=akt, aqt=aqt, trib=trib))
    if "nc" not in _NC_CACHE:
        _NC_CACHE["nc"] = build()
    res = run_bass_kernel_spmd(_NC_CACHE["nc"], in_maps, core_ids=list(range(8)))
    if DBG[0]:
        LAST["res"] = res.results
    out = np.zeros((2, SEQ, DM), np.float32)
    for c in range(8):
        b, j = c // 4, c % 4
        o = np.asarray(res.results[c]["outT"], np.float32).reshape(DM, NOWN)
        out[b, 2048 * j:2048 * (j + 1), :] = o.T
    return out
```

```python
from contextlib import ExitStack
import numpy as np
import ml_dtypes
import concourse.bass as bass
import concourse.mybir as mybir
from concourse.bass_utils import run_bass_kernel_spmd

F32 = mybir.dt.float32
BF16 = mybir.dt.bfloat16
ALU = mybir.AluOpType
AF = mybir.ActivationFunctionType
AX = mybir.AxisListType

DM = 1024
SEQ = 8192
DFF = 2816
NFF = 22
WIN = 8192
OWN0 = 6144
NOWN = 2048
ST = 1024
EPS = 1e-6
BIG = 30000.0
NEGG = -1.0e30
DIL = (1, 4, 16)
CB = 4096

ENGS = ("pe", "act", "dve", "pool", "sp")
BUDGET = [None]


class Prog:
    def __init__(self, nc, stack, n_dma_slots=8):
        self.nc = nc
        self.sem = {e: stack.enter_context(nc.semaphore("sem_" + e)) for e in ("pe", "act", "dve", "pool")}
        self.base = {e: 0 for e in self.sem}
        self.dsem = {}
        self.dcnt = {}
        for q in ("sp", "pool", "act"):
            self.dsem[q] = [stack.enter_context(nc.semaphore(f"dma_{q}_{i}")) for i in range(n_dma_slots if q == "sp" else 4)]
            self.dcnt[q] = [0] * len(self.dsem[q])
        self.drr = {q: 0 for q in self.dsem}
        self._reset()

    def _reset(self):
        self.idx = {e: 0 for e in self.sem}
        self.ops = {e: [] for e in ENGS}
        self.last_w = {}
        self.readers = {}
        self.waited = {e: {} for e in ENGS}
        self.targets = {e: set() for e in self.sem}

    def _need(self, eng, tok, waits):
        if tok is None:
            return
        key, val = tok[0], tok[-1]
        if key == eng and eng == "pe":
            return
        if self.waited[eng].get(key, -1) >= val:
            return
        self.waited[eng][key] = val
        waits.append(tok)
        if key in self.targets:
            self.targets[key].add(val)

    def _deps(self, eng, reads, writes):
        waits = []
        for r in reads:
            self._need(eng, self.last_w.get(r), waits)
            if r.startswith("ps"):
                for t in self.readers.get(r, ()):
                    if t[0] != eng:
                        self._need(eng, t, waits)
        for w in writes:
            self._need(eng, self.last_w.get(w), waits)
            for t in self.readers.get(w, ()):
                self._need(eng, t, waits)
        return waits

    def _commit(self, tok, reads, writes):
        for r in reads:
            self.readers.setdefault(r, []).append(tok)
        for w in writes:
            self.last_w[w] = tok
            self.readers[w] = []

    def op(self, eng, fn, reads=(), writes=()):
        if BUDGET[0] is not None:
            BUDGET[0] -= 1
            if BUDGET[0] < 0:
                return
        waits = self._deps(eng, reads, writes)
        self.idx[eng] += 1
        tok = (eng, self.idx[eng])
        self.ops[eng].append((waits, fn, tok))
        self._commit(tok, reads, writes)

    def dma(self, fn, reads=(), writes=(), q="sp"):
        if BUDGET[0] is not None:
            BUDGET[0] -= 1
            if BUDGET[0] < 0:
                return
        waits = self._deps(q, reads, writes)
        i = self.drr[q]
        self.drr[q] = (i + 1) % len(self.dsem[q])
        key = f"d{q}{i}"
        prev = self.dcnt[q][i]
        if prev > 0 and self.waited[q].get(key, -1) < prev:
            self.waited[q][key] = prev
            waits.append((key, q, i, prev))
        self.dcnt[q][i] = prev + 16
        tok = (key, q, i, prev + 16)
        self.ops[q].append((waits, fn, tok))
        self._commit(tok, reads, writes)

    def flush(self):
        nc = self.nc
        for q in self.dsem:
            waits = []
            for i in range(len(self.dsem[q])):
                v = self.dcnt[q][i]
                key = f"d{q}{i}"
                if v > 0 and self.waited[q].get(key, -1) < v:
                    self.waited[q][key] = v
                    waits.append((key, q, i, v))
            if waits:
                self.ops[q].append((waits, None, None))
        ops = self.ops
        rank = {}
        for e in self.sem:
            for r_, ix in enumerate(sorted(self.targets[e])):
                rank[(e, ix)] = self.base[e] + r_ + 1
        sem, dsem = self.sem, self.dsem

        def resolve(tok):
            if len(tok) == 2:
                return sem[tok[0]], rank[tok]
            return dsem[tok[1]][tok[2]], tok[3]

        def replay(h, lst):
            for waits, fn, tok in lst:
                for w in waits:
                    s_, v_ = resolve(w)
                    h.wait_ge(s_, v_)
                if fn is not None:
                    ins = fn(h)
                    if len(tok) == 4:
                        ins.then_inc(dsem[tok[1]][tok[2]], 16)
                    elif tok in rank:
                        ins.then_inc(sem[tok[0]], 1)

        with nc.Block() as block:
            if ops["sp"]:
                @block.sync
                def _(e):
                    replay(e, ops["sp"])
            if ops["pe"]:
                @block.tensor
                def _(e):
                    replay(e, ops["pe"])
            if ops["act"]:
                @block.scalar
                def _(e):
                    replay(e, ops["act"])
            if ops["dve"]:
                @block.vector
                def _(e):
                    replay(e, ops["dve"])
            if ops["pool"]:
                @block.gpsimd
                def _(e):
                    replay(e, ops["pool"])
        for e in self.sem:
            self.base[e] += len(self.targets[e])
        self._reset()


def ss(start, n, step):
    return slice(start, start + (n - 1) * step + 1, step)


def _weight_layout():
    lay = {}
    off = 0

    def add(name, w):
        nonlocal off
        lay[name] = (off, w)
        off += w

    for t in ("f1", "f2"):
        for f in range(NFF):
            add(f"{t}gu{f}", 2 * 8 * 128)
        for o in range(8):
            add(f"{t}d{o}", NFF * 128)
    for g in range(3):
        for pr in range(2):
            add(f"dq{g}{pr}", 1024)
            add(f"dk{g}{pr}", 1024)
        add(f"dv{g}", 2048)
    for i in range(4):
        add(f"mq{i}", 1024)
        add(f"mk{i}", 1024)
        add(f"mv{i}", 1024)
    for o in range(8):
        add(f"ga{o}", 1024)
        add(f"gb{o}", 1024)
    for o in range(8):
        add(f"ua{o}", 512)
        add(f"ub{o}", 1024)
    for o in range(8):
        add(f"wo{o}", 1024)
    return lay, off


WLAY, TOTF = _weight_layout()

C_G1, C_GM, C_G2, C_GF = 0, 8, 16, 24
C_ID = 32
C_GMASK = C_ID + 128
C_OWNHOT = C_GMASK + 512
C_VALM = C_OWNHOT + 512
C_VALD = C_VALM + 64
C_DEC = C_VALD + 69
CF = C_DEC + 12 * 256
VALD_OFF = (0, 17, 37)


def _kchunks(w, c0, width):
    return np.ascontiguousarray(w[:, c0:c0 + width].reshape(8, 128, width).transpose(1, 0, 2)).reshape(128, 8 * width)


def _pack_weights(inp):
    wall = np.zeros((128, TOTF), np.float32)

    def put(name, arr):
        o, w = WLAY[name]
        assert arr.shape == (128, w), (name, arr.shape, w)
        wall[:, o:o + w] = arr

    for t, gk, uk, dk in (("f1", "ffn1_gate", "ffn1_up", "ffn1_down"), ("f2", "ffn2_gate", "ffn2_up", "ffn2_down")):
        wg, wu, wd = inp[gk][0], inp[uk][0], inp[dk][0]
        for f in range(NFF):
            a = np.stack([wg[:, f * 128:(f + 1) * 128].reshape(8, 128, 128), wu[:, f * 128:(f + 1) * 128].reshape(8, 128, 128)], 0)
            put(f"{t}gu{f}", a.transpose(2, 0, 1, 3).reshape(128, 2048))
        for o in range(8):
            put(f"{t}d{o}", wd[:, o * 128:(o + 1) * 128].reshape(NFF, 128, 128).transpose(1, 0, 2).reshape(128, NFF * 128))
    win = inp["w_in"][0]
    QA, KA, VA, QB, KB, VB, GA, GB = 0, 768, 1536, 2304, 2816, 3328, 3840, 4864
    for g in range(3):
        for pr in range(2):
            h0 = 4 * g + 2 * pr
            put(f"dq{g}{pr}", _kchunks(win, QA + h0 * 64, 128))
            put(f"dk{g}{pr}", _kchunks(win, KA + h0 * 64, 128))
        put(f"dv{g}", _kchunks(win, VA + 4 * g * 64, 256))
    for i in range(4):
        put(f"mq{i}", _kchunks(win, QB + i * 128, 128))
        put(f"mk{i}", _kchunks(win, KB + i * 128, 128))
        put(f"mv{i}", _kchunks(win, VB + i * 128, 128))
    for o in range(8):
        put(f"ga{o}", _kchunks(win, GA + o * 128, 128))
        put(f"gb{o}", _kchunks(win, GB + o * 128, 128))
    wua, wub, wo = inp["w_up_a"][0], inp["w_up_b"][0], inp["w_out"][0]
    for o in range(8):
        a = np.zeros((128, 4, 128), np.float32)
        a[:64] = wua[:, o * 128:(o + 1) * 128].reshape(4, 64, 128).transpose(1, 0, 2)
        put(f"ua{o}", a.reshape(128, 512))
        b = np.zeros((128, 8, 128), np.float32)
        b[:64] = wub[:, o * 128:(o + 1) * 128].reshape(8, 64, 128).transpose(1, 0, 2)
        put(f"ub{o}", b.reshape(128, 1024))
        put(f"wo{o}", _kchunks(wo, o * 128, 128))
    return wall


def _const_tables(inp, j):
    cst = np.zeros((128, CF), np.float32)
    for col, key in ((C_G1, "norm_ffn1"), (C_GM, "norm_mix"), (C_G2, "norm_ffn2"), (C_GF, "norm_final")):
        g = np.asarray(inp[key], np.float32).reshape(-1)
        cst[:, col:col + 8] = g.reshape(8, 128).T
    cst[:, C_ID:C_ID + 128] = np.eye(128, dtype=np.float32)
    first_valid_tok = 2048 * (3 - j)
    first_valid_blk = first_valid_tok // 256
    p = np.arange(128)
    blk = np.arange(32)
    for qt in range(16):
        q = qt * 128 + p
        ob = (OWN0 + q) // 256
        ok = (blk[None, :] < ob[:, None]) & (blk[None, :] >= first_valid_blk)
        cst[:, C_GMASK + qt * 32:C_GMASK + (qt + 1) * 32] = np.where(ok, 0.0, NEGG)
        cst[:, C_OWNHOT + qt * 32:C_OWNHOT + (qt + 1) * 32] = (blk[None, :] == ob[:, None]).astype(np.float32)
    t = np.arange(64)[None, :] * 128 + p[:, None]
    cst[:, C_VALM:C_VALM + 64] = (t >= first_valid_tok).astype(np.float32)
    for g, D in enumerate(DIL):
        nq = 16 // D
        for r in range(D):
            for jt in range(nq + 1):
                i = OWN0 // D - 128 + 128 * jt + p
                pos = r + D * i
                cst[:, C_VALD + VALD_OFF[g] + r * (nq + 1) + jt] = (pos >= first_valid_tok).astype(np.float32)
    slopes_a = 2.0 ** (-8.0 * np.arange(1, 13, dtype=np.float64) / 12)
    b = p[:, None].astype(np.float64)
    a = np.arange(128)[None, :].astype(np.float64)
    for h in range(12):
        D = DIL[h // 4]
        s = slopes_a[h]
        cur = np.where(a >= b, np.exp(-s * D * (a - b)), 0.0)
        prev = np.where(a <= b, np.exp(-s * D * (a - b + 128)), 0.0)
        cst[:, C_DEC + h * 256:C_DEC + h * 256 + 128] = cur
        cst[:, C_DEC + h * 256 + 128:C_DEC + (h + 1) * 256] = prev
    bf = ml_dtypes.bfloat16
    tk = np.arange(WIN)
    etab = (tk[None, :] // 256 == np.arange(32)[:, None]).astype(np.float32).astype(bf)
    slopes_b = 2.0 ** (-8.0 * np.arange(1, 9, dtype=np.float64) / 8)
    akt = np.zeros((8, 4, WIN), np.float32)
    aqt = np.zeros((8, 4, NOWN), np.float32)
    tq = OWN0 + np.arange(NOWN)
    for h in range(8):
        s = slopes_b[h]
        akt[h, 0] = s * 128 * (tk // 128)
        akt[h, 1] = s * (tk % 128)
        akt[h, 2] = 1.0
        akt[h, 3] = 1.0
        aqt[h, 0] = 1.0
        aqt[h, 1] = 1.0
        aqt[h, 2] = -s * 128 * (tq // 128)
        aqt[h, 3] = -s * (tq % 128)
    trib = (p[:, None] <= np.arange(128)[None, :]).astype(np.float32).astype(bf)
    return cst, etab, akt.astype(bf), aqt.astype(bf), trib


STOP = [99]
RUN = [set(range(6))]


def _phase(n):
    if n in RUN[0]:
        with ExitStack() as ph:
            yield ph
DBG = [False]
LAST = {}
SUB = [""]


class _Stop(Exception):
    pass


def _chk(n):
    if STOP[0] == n:
        raise _Stop()


def build():
    nc = bass.Bass("TRN2", target_bir_lowering=False)
    xw = nc.dram_tensor("xw", [8, 128, WIN], F32, kind="ExternalInput").ap()
    wall = nc.dram_tensor("wall", [128, TOTF], F32, kind="ExternalInput").ap()
    cstd = nc.dram_tensor("cst", [128, CF], F32, kind="ExternalInput").ap()
    etabd = nc.dram_tensor("etab", [32, WIN], BF16, kind="ExternalInput").ap()
    aktd = nc.dram_tensor("akt", [8, 4, WIN], BF16, kind="ExternalInput").ap()
    aqtd = nc.dram_tensor("aqt", [8, 4, NOWN], BF16, kind="ExternalInput").ap()
    tribd = nc.dram_tensor("trib", [128, 128], BF16, kind="ExternalInput").ap()
    outT = nc.dram_tensor("outT", [8, 128, NOWN], F32, kind="ExternalOutput").ap()
    WB = nc.dram_tensor("wb_s", [128, TOTF], BF16, kind=("Internal" if 0 in RUN[0] else "ExternalInput")).ap()
    sk = "ExternalOutput" if DBG[0] else "Internal"
    R_ = RUN[0]
    kin = lambda prod: sk if prod in R_ else "ExternalInput"
    HS = nc.dram_tensor("h_s", [8, 128, WIN], BF16, kind=kin(1)).ap()
    X1S = nc.dram_tensor("x1_s", [8, 128, NOWN], F32, kind=kin(1)).ap()
    MS = nc.dram_tensor("m_s", [8, 128, NOWN], BF16, kind=kin(4)).ap()
    YAD = nc.dram_tensor("ya_d", [64, 4, NOWN], BF16, kind=kin(2)).ap()
    YBD = nc.dram_tensor("yb_d", [64, 8, NOWN], BF16, kind=kin(3)).ap()
    HSr = HS.rearrange("k p t -> p k t")
    X1r = X1S.rearrange("k p t -> p k t")
    MSr = MS.rearrange("k p t -> p k t")
    xwr = xw.rearrange("k p t -> p k t")
    outr = outT.rearrange("k p t -> p k t")

    try:
        _build_body(nc, xwr, wall, cstd, etabd, aktd, aqtd, tribd, outr, WB, HSr, X1r, MSr, YAD, YBD)
    except _Stop:
        pass
    return nc


def _build_body(nc, xwr, wall, cstd, etabd, aktd, aqtd, tribd, outr, WB, HSr, X1r, MSr, YAD, YBD):
    with ExitStack() as top:
        P = Prog(nc, top)

        def SB(stk, name, shape, dt):
            return stk.enter_context(nc.sbuf_tensor("s_" + name, shape, dt))

        bank = [top.enter_context(nc.psum_tensor(f"bank{i}", [128, 512], F32)) for i in range(8)]
        cst = SB(top, "cst", [128, CF], F32)
        ones = SB(top, "ones", [128, 128], BF16)
        onesf = SB(top, "onesf", [128, 64], F32)
        trib = SB(top, "trib", [128, 128], BF16)

        def DMA(out, in_, reads, writes, q="sp"):
            P.dma(lambda e: e.dma_start(out=out, in_=in_), reads, writes, q=q)

        def MMS(groups, reads, writes):
            def fn(e):
                ins = None
                for out, pairs in groups:
                    n = len(pairs)
                    for i, (l, r) in enumerate(pairs):
                        ins = e.matmul(out, lhsT=l, rhs=r, start=(i == 0), stop=(i == n - 1))
                return ins
            P.op("pe", fn, reads, writes)

        def ACT(out, in_, func, reads, writes, scale=None):
            if scale is None:
                P.op("act", lambda e: e.activation(out=out, in_=in_, func=func), reads, writes)
            else:
                P.op("act", lambda e: e.activation(out=out, in_=in_, func=func, scale=scale), reads, writes)

        def ACP(out, in_, reads, writes, mul=None):
            if mul is None:
                P.op("act", lambda e: e.copy(out=out, in_=in_), reads, writes)
            else:
                P.op("act", lambda e: e.mul(out=out, in_=in_, mul=mul), reads, writes)

        def TT(out, in0, in1, op, reads, writes, eng="dve"):
            P.op(eng, lambda e: e.tensor_tensor(out=out, in0=in0, in1=in1, op=op), reads, writes)

        def TS(out, in0, s1, s2, op0, op1, reads, writes, eng="dve"):
            if op1 is None:
                P.op(eng, lambda e: e.tensor_scalar(out=out, in0=in0, scalar1=s1, scalar2=None, op0=op0), reads, writes)
            else:
                P.op(eng, lambda e: e.tensor_scalar(out=out, in0=in0, scalar1=s1, scalar2=s2, op0=op0, op1=op1), reads, writes)

        def STT(out, in0, scalar, in1, op0, op1, reads, writes):
            P.op("dve", lambda e: e.scalar_tensor_tensor(out=out, in0=in0, scalar=scalar, in1=in1, op0=op0, op1=op1), reads, writes)

        def CP(out, in_, reads, writes, eng="dve"):
            P.op(eng, lambda e: e.tensor_copy(out=out, in_=in_), reads, writes)

        def RCP(out, in_, reads, writes):
            P.op("dve", lambda e: e.reciprocal(out=out, in_=in_), reads, writes)

        def MSET(ap, val, writes, eng="pool"):
            P.op(eng, lambda e: e.memset(ap, val), (), writes)

        def wres(name):
            o, w = WLAY[name]
            return [f"WB{b}" for b in range(o // CB, (o + w - 1) // CB + 1)]

        def WLOAD(dst, name, writes, q="sp"):
            o, w = WLAY[name]
            DMA(dst, WB[:, o:o + w], wres(name), writes, q=q)

        DMA(cst[:], cstd, (), ["cst"])
        DMA(trib[:], tribd, (), ["trib"])
        MSET(ones[:], 1.0, ["ones"])
        MSET(onesf[:], 1.0, ["onesf"])
        for ph in _phase(0):
            stg = [SB(ph, f"stg{i}", [128, CB], F32) for i in range(3)]
            cvt = [SB(ph, f"cvt{i}", [128, CB], BF16) for i in range(3)]
            nblk = (TOTF + CB - 1) // CB
            engs = ("dve", "act")
            for b in range(nblk):
                s = b % 3
                c0 = b * CB
                w = min(CB, TOTF - c0)
                DMA(stg[s][:, 0:w], wall[:, c0:c0 + w], (), [f"stg{s}"])
                en = engs[b % 2]
                if en == "act":
                    ACP(cvt[s][:, 0:w], stg[s][:, 0:w], [f"stg{s}"], [f"cvt{s}"])
                else:
                    CP(cvt[s][:, 0:w], stg[s][:, 0:w], [f"stg{s}"], [f"cvt{s}"], eng=en)
                DMA(WB[:, c0:c0 + w], cvt[s][:, 0:w], [f"cvt{s}"], [f"WB{b}"], q="pool" if b % 2 else "sp")
            P.flush()
            _chk(0)

        def rmsnorm(tag, X, xr, gcol, out, outr, bufs, n=ST):
            sq, t1, t2 = bufs["sq"], bufs["t1"], bufs["t2"]
            nh = n // 512
            for c in range(8):
                s = c % 2
                ACT(sq[s][:, 0:n], X[:, c, :], AF.Square, [xr(c)], [f"{tag}sq{s}"])
                for hf in range(nh):
                    P.op("pe", (lambda hf=hf, s=s, c=c: (lambda e: e.matmul(bank[hf][:, :], lhsT=ones[:], rhs=sq[s][:, hf * 512:(hf + 1) * 512],
                                                                        start=(c == 0), stop=(c == 7))))(),
                         ["ones", f"{tag}sq{s}"], [f"ps{hf}"])
            for hf in range(nh):
                TS(t1[:, hf * 512:(hf + 1) * 512], bank[hf][:, :], 1.0 / DM, EPS, ALU.mult, ALU.add, [f"ps{hf}"], [f"{tag}t1"])
            ACT(t2[:, 0:n], t1[:, 0:n], AF.Sqrt, [f"{tag}t1"], [f"{tag}t2"])
            RCP(t1[:, 0:n], t2[:, 0:n], [f"{tag}t2"], [f"{tag}t1"])
            for c in range(8):
                STT(out[:, c, :], X[:, c, :], cst[:, gcol + c:gcol + c + 1], t1[:, 0:n], ALU.mult, ALU.mult,
                    [xr(c), "cst", f"{tag}t1"], [outr(c)])

        def ffn(tag, X, xr, gcol, wt, bufs):
            h0, act, sg, wgu, wd = bufs["h0"], bufs["act"], bufs["sg"], bufs["wgu"], bufs["wd"]
            h0r = lambda c: f"{tag}h0_{c}"
            rmsnorm(tag, X, xr, gcol, h0, h0r, bufs)
            h0all = [h0r(c) for c in range(8)]
            WLOAD(wgu[0][:], f"{wt}gu0", [f"{tag}wgu0"])
            it = 0
            for f in range(NFF):
                s = f % 2
                if f + 1 < NFF:
                    WLOAD(wgu[1 - s][:], f"{wt}gu{f + 1}", [f"{tag}wgu{1 - s}"])
                elif True:
                    WLOAD(wd[0][:], f"{wt}d0", [f"{tag}wd0"])
                for hf in range(2):
                    pb = 2 + 2 * (it % 2)
                    it += 1
                    cols = slice(hf * 512, (hf + 1) * 512)
                    w4 = wgu[s][:].rearrange("p (g k c) -> p g k c", g=2, k=8)
                    MMS([(bank[pb][:, :], [(w4[:, 0, k, :], h0[:, k, cols]) for k in range(8)]),
                         (bank[pb + 1][:, :], [(w4[:, 1, k, :], h0[:, k, cols]) for k in range(8)])],
                        [f"{tag}wgu{s}"] + h0all, [f"ps{pb}", f"ps{pb + 1}"])
                    ss_ = it % 2
                    ACT(sg[ss_][:, :], bank[pb][:, :], AF.Silu, [f"ps{pb}"], [f"{tag}sg{ss_}"])
                    TT(act[:, f, cols], sg[ss_][:, :], bank[pb + 1][:, :], ALU.mult, [f"{tag}sg{ss_}", f"ps{pb + 1}"], [f"{tag}act{f}_{hf}"])
            actall = [f"{tag}act{f}_{hf}" for f in range(NFF) for hf in range(2)]
            it = 0
            for o in range(8):
                s = o % 2
                if o + 1 < 8:
                    WLOAD(wd[1 - s][:], f"{wt}d{o + 1}", [f"{tag}wd{1 - s}"])
                w3 = wd[s][:].rearrange("p (f c) -> p f c", f=NFF)
                for hf in range(2):
                    pb = 6 + (it % 2)
                    it += 1
                    cols = slice(hf * 512, (hf + 1) * 512)
                    MMS([(bank[pb][:, :], [(w3[:, f, :], act[:, f, cols]) for f in range(NFF)])],
                        [f"{tag}wd{s}"] + actall, [f"ps{pb}"])
                    STT(X[:, o, cols], bank[pb][:, :], 0.5, X[:, o, cols], ALU.mult, ALU.add, [f"ps{pb}", xr(o)], [xr(o)])

        def ffn_bufs(stk, tag):
            return dict(
                h0=SB(stk, tag + "h0", [128, 8, ST], BF16),
                act=SB(stk, tag + "act", [128, NFF, ST], BF16),
                sq=[SB(stk, f"{tag}sq{i}", [128, ST], BF16) for i in range(2)],
                t1=SB(stk, tag + "t1", [128, ST], F32),
                t2=SB(stk, tag + "t2", [128, ST], F32),
                sg=[SB(stk, f"{tag}sg{i}", [128, 512], F32) for i in range(2)],
                wgu=[SB(stk, f"{tag}wgu{i}", [128, 2048], BF16) for i in range(2)],
                wd=[SB(stk, f"{tag}wd{i}", [128, NFF * 128], BF16) for i in range(2)],
            )

        for ph in _phase(1):
            bufs = ffn_bufs(ph, "a")
            Xb = [SB(ph, f"aX{i}", [128, 8, ST], F32) for i in range(2)]
            nst = WIN // ST
            DMA(Xb[0][:], xwr[:, :, 0:ST], (), [f"aX0_{c}" for c in range(8)])
            for st in range(nst):
                s = st % 2
                X = Xb[s]
                xr = (lambda s: (lambda c: f"aX{s}_{c}"))(s)
                if st + 1 < nst:
                    DMA(Xb[1 - s][:], xwr[:, :, (st + 1) * ST:(st + 2) * ST], (), [f"aX{1 - s}_{c}" for c in range(8)])
                ffn("a", X, xr, C_G1, "f1", bufs)
                if st * ST >= OWN0:
                    DMA(X1r[:, :, st * ST - OWN0:(st + 1) * ST - OWN0], X[:], [xr(c) for c in range(8)], [f"X1S{st}"])
                h0r = lambda c: f"ah0_{c}"
                rmsnorm("a", X, xr, C_GM, bufs["h0"], h0r, bufs)
                DMA(HSr[:, :, st * ST:(st + 1) * ST], bufs["h0"][:], [h0r(c) for c in range(8)], [f"HS{st}"])
            P.flush()
            _chk(1)

        with ExitStack() as mid:
            YA = SB(mid, "YA", [64, 4, NOWN], BF16)
            for ph in _phase(2):
                hb = SB(ph, "d_hb", [128, 8, 2048], BF16)
                ACC = SB(ph, "d_acc", [128, 4, NOWN], F32)
                QT = [SB(ph, f"d_qt{i}", [128, 2048], BF16) for i in range(2)]
                KT = [SB(ph, f"d_kt{i}", [128, 2048 + 128 * 16], BF16) for i in range(2)]
                VA = SB(ph, "d_va", [128, 32, 4, 65], BF16)
                wq = [SB(ph, f"d_wq{i}", [128, 8, 128], BF16) for i in range(2)]
                wk = [SB(ph, f"d_wk{i}", [128, 8, 128], BF16) for i in range(2)]
                wv = SB(ph, "d_wv", [128, 8, 256], BF16)
                et = [SB(ph, f"d_et{i}", [128, 2, 2, 128], F32) for i in range(2)]
                PT = [SB(ph, f"d_pt{i}", [128, 2, 2, 128], BF16) for i in range(4)]
                rd = SB(ph, "d_rd", [128, 512], F32)
                dec = cst[:, C_DEC:C_DEC + 3072].rearrange("p (h a b) -> p h a b", h=12, a=2)
                pcnt = [0]

                def nbank():
                    pcnt[0] += 1
                    return pcnt[0] % 4

                for g, D in enumerate(DIL):
                    npr = NOWN // D
                    nq = 16 // D
                    KW = npr + 128
                    ntile = D * (nq + 1)
                    for pr in range(2):
                        WLOAD(wq[pr][:].rearrange("p k c -> p (k c)"), f"dq{g}{pr}", [f"d_wq{pr}"])
                        WLOAD(wk[pr][:].rearrange("p k c -> p (k c)"), f"dk{g}{pr}", [f"d_wk{pr}"])
                    WLOAD(wv[:].rearrange("p k c -> p (k c)"), f"dv{g}", ["d_wv"])
                    for hd in range(4):
                        CP(VA[:, 0:ntile, hd, 64], cst[:, C_VALD + VALD_OFF[g]:C_VALD + VALD_OFF[g] + ntile], ["cst"], ["d_va"], eng="pool")
                    for half in range(2):
                        hs_res = ["HS4", "HS5"] if half == 0 else ["HS6", "HS7"]
                        DMA(hb[:], HSr[:, :, 4096 + 2048 * half:4096 + 2048 * (half + 1)], hs_res, ["d_hb"])
                        if half == 0:
                            col0 = lambda r: 2048 - 128 * D + r
                            ncol = 128
                            kdst0 = 0
                        else:
                            col0 = lambda r: r
                            ncol = npr
                            kdst0 = 128
                        for pr in range(2):
                            rpb = max(1, 512 // ncol)
                            for r0 in range(0, D, rpb):
                                rs = list(range(r0, min(D, r0 + rpb)))
                                for c0 in range(0, ncol, 512):
                                    cn = min(512, ncol - c0)
                                    targets = [("k", wk[pr], KT[pr])] + ([("q", wq[pr], QT[pr])] if half == 1 else [])
                                    for kind, wt_, dst in targets:
                                        b = nbank()
                                        groups = []
                                        for ri, r in enumerate(rs):
                                            groups.append((bank[b][:, ri * cn:(ri + 1) * cn],
                                                           [(wt_[:, k, :], hb[:, k, ss(col0(r) + D * c0, cn, D)]) for k in range(8)]))
                                        MMS(groups, ["d_hb", f"d_w{kind}{pr}"], [f"ps{b}"])
                                        for ri, r in enumerate(rs):
                                            src = bank[b][:, ri * cn:(ri + 1) * cn]
                                            if kind == "k":
                                                CP(KT[pr][:, r * KW + kdst0 + c0:r * KW + kdst0 + c0 + cn], src, [f"ps{b}"], [f"d_kt{pr}"])
                                            else:
                                                ACP(QT[pr][:, r * npr + c0:r * npr + c0 + cn], src, [f"ps{b}"], [f"d_qt{pr}"], mul=0.125)
                        jts = [0] if half == 0 else list(range(1, nq + 1))
                        for r in range(D):
                            for jt in jts:
                                c_first = (2048 - 128 * D + r) if half == 0 else (r + D * 128 * (jt - 1))
                                b = nbank()
                                MMS([(bank[b][:, 0:256], [(hb[:, k, ss(c_first, 128, D)], wv[:, k, :]) for k in range(8)])],
                                    ["d_hb", "d_wv"], [f"ps{b}"])
                                t_ = r * (nq + 1) + jt
                                src = bank[b][:, 0:256].rearrange("p (h d) -> p h d", h=4)
                                if (r + jt) % 2:
                                    ACP(VA[:, t_, :, 0:64], src, [f"ps{b}"], ["d_va"])
                                else:
                                    CP(VA[:, t_, :, 0:64], src, [f"ps{b}"], ["d_va"])
                    gcount = 0
                    for pr in range(2):
                        ob = [4 + 2 * pr, 5 + 2 * pr]
                        qslot = 0
                        ptc = 0
                        prev_pt = None
                        for r in range(D):
                            for jt in range(nq + 1):
                                halves = [1] if jt == 0 else ([0] if jt == nq else [0, 1])
                                h0_, h1_ = halves[0], halves[-1] + 1
                                qc0 = r * npr + 128 * (jt - 1 + h0_)
                                nqc = 128 * len(halves)
                                sb0 = 2 * (gcount % 2)
                                sv = [bank[sb0 + e_][:, 0:256].rearrange("p (a b) -> p a b", a=2) for e_ in range(2)]
                                kc = r * KW + 128 * jt
                                MMS([(sv[e_][:, h0_:h1_, :], [(KT[pr][64 * e_:64 * e_ + 64, kc:kc + 128], QT[pr][64 * e_:64 * e_ + 64, qc0:qc0 + nqc])])
                                     for e_ in range(2)], [f"d_kt{pr}", f"d_qt{pr}"], [f"ps{sb0}", f"ps{sb0 + 1}"])
                                es = gcount % 2
                                ps_ = ptc % 4
                                ptc += 1
                                gcount += 1
                                for e_ in range(2):
                                    ACT(et[es][:, e_, h0_:h1_, :], sv[e_][:, h0_:h1_, :], AF.Exp, [f"ps{sb0 + e_}"], [f"d_et{es}_{e_}"])
                                hh0 = 4 * g + 2 * pr
                                TT(PT[ps_][:, :, h0_:h1_, :], et[es][:, :, h0_:h1_, :], dec[:, hh0:hh0 + 2, h0_:h1_, :], ALU.mult,
                                   [f"d_et{es}_0", f"d_et{es}_1", "cst"], [f"d_pt{ps_}"])
                                if jt >= 1:
                                    qt_ = jt - 1
                                    t_prev = r * (nq + 1) + jt - 1
                                    t_cur = r * (nq + 1) + jt
                                    pp = prev_pt
                                    groups = []
                                    for e_ in range(2):
                                        hd = 2 * pr + e_
                                        groups.append((bank[ob[e_]][0:65, qslot * 128:(qslot + 1) * 128],
                                                       [(VA[:, t_prev, hd, :], PT[pp][:, e_, 1, :]), (VA[:, t_cur, hd, :], PT[ps_][:, e_, 0, :])]))
                                    MMS(groups, ["d_va", f"d_pt{pp}", f"d_pt{ps_}"], [f"ps{ob[0]}", f"ps{ob[1]}"])
                                    qslot += 1
                                    if qslot == 4:
                                        qslot = 0
                                        for e_ in range(2):
                                            hd = 2 * pr + e_
                                            if D == 1:
                                                m = qt_ // 4
                                                dst = ACC[0:65, hd, m * 512:(m + 1) * 512]
                                                src = bank[ob[e_]][0:65, :]
                                            elif D == 4:
                                                dst = ACC[0:65, hd, ss(r, 512, 4)]
                                                src = bank[ob[e_]][0:65, :]
                                            else:
                                                r0 = r - 3
                                                dst = ACC[0:65, hd, :].rearrange("p (i r) -> p r i", r=16)[:, r0:r0 + 4, :]
                                                src = bank[ob[e_]][0:65, :].rearrange("p (r i) -> p r i", r=4)
                                            if g == 0:
                                                CP(dst, src, [f"ps{ob[e_]}"], [f"d_acc{hd}"])
                                            else:
                                                TT(dst, src, dst, ALU.add, [f"ps{ob[e_]}", f"d_acc{hd}"], [f"d_acc{hd}"])
                                prev_pt = ps_
                for hd in range(4):
                    for m in range(4):
                        cols = slice(m * 512, (m + 1) * 512)
                        RCP(rd[64:65, :], ACC[64:65, hd, cols], [f"d_acc{hd}"], ["d_rd"])
                        b = nbank()
                        MMS([(bank[b][0:64, :], [(onesf[64:65, 0:64], rd[64:65, :])])], ["onesf", "d_rd"], [f"ps{b}"])
                        TT(YA[0:64, hd, cols], ACC[0:64, hd, cols], bank[b][0:64, :], ALU.mult, [f"d_acc{hd}", f"ps{b}"], [f"YA{hd}"])
                if DBG[0]:
                    DMA(YAD, YA[:], [f"YA{hd}" for hd in range(4)], ["YAD"])
                P.flush()
                _chk(2)

            YB = SB(mid, "YB", [64, 8, NOWN], BF16)
            for ph in _phase(3):
                KAB = [SB(ph, f"m_k{i}", [128, WIN], BF16) for i in range(2)]
                QAB = [SB(ph, f"m_q{i}", [128, NOWN], BF16) for i in range(2)]
                VAB = SB(ph, "m_v", [128, 64, 2, 65], BF16)
                hbuf = [SB(ph, f"m_hb{i}", [128, 8, 512], BF16) for i in range(2)]
                wq = SB(ph, "m_wq", [128, 8, 128], BF16)
                wk = SB(ph, "m_wk", [128, 8, 128], BF16)
                wv = SB(ph, "m_wv", [128, 8, 128], BF16)
                QF = SB(ph, "m_qf", [128, 512], F32)
                KM = SB(ph, "m_km", [128, 32], F32)
                g2 = SB(ph, "m_g2", [128, 2, 32], F32)
                mx = SB(ph, "m_mx", [128, 2, 8], F32)
                thr = SB(ph, "m_thr", [128, 2, 1], F32)
                sel = SB(ph, "m_sel", [128, 2, 32], F32)
                ZA = SB(ph, "m_za", [128, 96], F32)
                ZB = SB(ph, "m_zb", [128, 32], F32)
                PTm = [SB(ph, f"m_pt{i}", [128, 512], BF16) for i in range(6)]
                osb = SB(ph, "m_osb", [128, 512], F32)
                rdm = SB(ph, "m_rd", [128, 512], F32)
                erow = (slice(64, 96), slice(0, 32))
                arow = (slice(96, 100), slice(32, 36))
                drow = (slice(0, 64), slice(64, 128))
                krows = (slice(0, 100), slice(0, 128))
                if "S1" not in SUB[0]:
                    MSET(KAB[1][32:64, :], 0.0, ["m_k1x"])
                    MSET(QAB[1][32:64, :], 0.0, ["m_q1x"])
                MSET(ZA[:], 0.0, ["m_za"])
                MSET(KM[:], 0.0, ["m_km"])
                if "S2" not in SUB[0]:
                    DMA(KAB[0][64:96, :], etabd, (), ["m_k0e"])
                    DMA(KAB[1][0:32, :], etabd, (), ["m_k1e"])
                for e_ in (range(2) if "S3" not in SUB[0] else ()):
                    CP(VAB[:, :, e_, 64], cst[:, C_VALM:C_VALM + 64], ["cst"], ["m_vval"], eng="pool")
                sring = [0]
                pending = []
                for i in (range(4) if "P1" not in SUB[0] else range(1)):
                    WLOAD(wq[:].rearrange("p k c -> p (k c)"), f"mq{i}", ["m_wq"])
                    WLOAD(wk[:].rearrange("p k c -> p (k c)"), f"mk{i}", ["m_wk"])
                    WLOAD(wv[:].rearrange("p k c -> p (k c)"), f"mv{i}", ["m_wv"])
                    for e_ in (range(2) if "L" not in SUB[0] else ()):
                        DMA(KAB[e_][arow[e_], :], aktd[2 * i + e_], ["m_k1x"], [f"m_k{e_}a"])
                        DMA(QAB[e_][arow[e_], :], aqtd[2 * i + e_], ["m_q1x"], [f"m_q{e_}a"])
                    DMA(hbuf[0][:], HSr[:, :, 0:512], ["HS0"], ["m_hb0"])
                    for s2 in range(16):
                        hs = s2 % 2
                        H_ = hbuf[hs]
                        if s2 + 1 < 16:
                            DMA(hbuf[1 - hs][:], HSr[:, :, (s2 + 1) * 512:(s2 + 2) * 512], [f"HS{(s2 + 1) // 2}"], [f"m_hb{1 - hs}"])
                        cols = slice(s2 * 512, (s2 + 1) * 512)
                        if "K" not in SUB[0]:
                          MMS([(bank[6][:, :], [(wk[:, k, :], H_[:, k, :]) for k in range(8)])], [f"m_hb{hs}", "m_wk"], ["ps6"])
                        ACP(KAB[0][0:64, cols], bank[6][0:64, :], ["ps6"], ["m_k0d"])
                        CP(KAB[1][64:128, cols], bank[6][64:128, :], ["ps6"], ["m_k1d"])
                        P.op("dve", (lambda s2=s2: (lambda e: e.tensor_reduce(out=KM[:, 2 * s2:2 * s2 + 2],
                                                                              in_=bank[6][:, :].rearrange("p (a b) -> p a b", a=2),
                                                                              axis=AX.X, op=ALU.add)))(), ["ps6"], ["m_km"])
                        MMS([(bank[7][:, tt * 128:(tt + 1) * 128], [(H_[:, k, tt * 128:(tt + 1) * 128], wv[:, k, :]) for k in range(8)])
                             for tt in range(4)], [f"m_hb{hs}", "m_wv"], ["ps7"])
                        v4 = bank[7][:, :].rearrange("p (t e d) -> p t e d", t=4, e=2)
                        ACP(VAB[:, 4 * s2:4 * s2 + 4, 0, 0:64], v4[:, :, 0, :], ["ps7"], ["m_v0"])
                        if "T0" not in SUB[0]:
                            ACP(VAB[:, 4 * s2:4 * s2 + 4, 1, 0:64], v4[:, :, 1, :], ["ps7"], ["m_v1"])
                        elif "T2" in SUB[0]:
                            TS(VAB[:, 4 * s2:4 * s2 + 4, 1, 0:64], v4[:, :, 1, :], 1.0, None, ALU.mult, None, ["ps7"], ["m_v1"])
                        elif "T3" in SUB[0]:
                            for tt_ in range(4):
                                CP(VAB[:, 4 * s2 + tt_, 1, 0:64], v4[:, tt_, 1, :], ["ps7"], ["m_v1"])
                        else:
                            CP(VAB[:, 4 * s2:4 * s2 + 4, 1, 0:64], v4[:, :, 1, :], ["ps7"], ["m_v1"])
                        if s2 >= 12:
                            qi = s2 - 12
                            qcols = slice(qi * 512, (qi + 1) * 512)
                            MMS([(bank[5][:, :], [(wq[:, k, :], H_[:, k, :]) for k in range(8)])], [f"m_hb{hs}", "m_wq"], ["ps5"])
                            ACP(QAB[0][0:64, qcols], bank[5][0:64, :], ["ps5"], ["m_q0d"], mul=0.125)
                            TS(QAB[1][64:128, qcols], bank[5][64:128, :], 0.125, None, ALU.mult, None, ["ps5"], ["m_q1d"])
                            ACP(QF[:, :], bank[5][:, :], ["ps5"], ["m_qf"], mul=0.125)
                            for sub in (range(4) if "G" not in SUB[0] else ()):
                                qt16 = 4 * qi + sub
                                sc = slice(sub * 128, (sub + 1) * 128)
                                MMS([(bank[4 + e_][:, 0:32], [(QF[drow[e_], sc], KM[drow[e_], :])]) for e_ in range(2)],
                                    ["m_qf", "m_km"], ["ps4", "ps5"])
                                for e_ in range(2):
                                    TT(g2[:, e_, :], bank[4 + e_][:, 0:32], cst[:, C_GMASK + 32 * qt16:C_GMASK + 32 * qt16 + 32],
                                       ALU.add, [f"ps{4 + e_}", "cst"], ["m_g2"])
                                for e_ in range(2):
                                    P.op("dve", (lambda e_=e_: (lambda e: e.max(out=mx[:, e_, :], in_=g2[:, e_, :])))(), ["m_g2"], ["m_mx"])
                                TS(thr[:, :, :], mx[:, :, 2:3], -1.0e29, None, ALU.max, None, ["m_mx"], ["m_thr"])
                                for e_ in range(2):
                                    TS(sel[:, e_, :], g2[:, e_, :], thr[:, e_, :], None, ALU.is_ge, None, ["m_g2", "m_thr"], ["m_sel"])
                                    TT(sel[:, e_, :], sel[:, e_, :], cst[:, C_OWNHOT + 32 * qt16:C_OWNHOT + 32 * qt16 + 32], ALU.add,
                                       ["m_sel", "cst"], ["m_sel"])
                                TS(ZA[:, 64:96], sel[:, 0, :], BIG, -BIG, ALU.mult, ALU.add, ["m_sel"], ["m_za"])
                                TS(ZB[:, :], sel[:, 1, :], BIG, -BIG, ALU.mult, ALU.add, ["m_sel"], ["m_zb"])
                                P.op("pe", (lambda: (lambda e: e.transpose(bank[3][0:96, 0:128], ZA[:], cst[:, C_ID:C_ID + 128])))(),
                                     ["m_za", "cst"], ["ps3"])
                                P.op("pe", (lambda: (lambda e: e.transpose(bank[3][0:32, 128:256], ZB[:], cst[:, C_ID:C_ID + 128])))(),
                                     ["m_zb", "cst"], ["ps3"])
                                qc = slice(qi * 512 + sub * 128, qi * 512 + (sub + 1) * 128)
                                ACP(QAB[0][64:96, qc], bank[3][64:96, 0:128], ["ps3"], ["m_q0n"])
                                ACP(QAB[1][0:32, qc], bank[3][0:32, 128:256], ["ps3"], ["m_q1n"])
                    for e_ in (range(2) if "A" not in SUB[0] else ()):
                        h = 2 * i + e_
                        kres = [f"m_k{e_}d", f"m_k{e_}a", f"m_k{e_}e", "m_k1x"]
                        qres = [f"m_q{e_}d", f"m_q{e_}a", f"m_q{e_}n", "m_q1x"]
                        rows = krows[e_]
                        for qi in range(4):
                            ob_ = 3 + 0
                            ob_ = 4 if (qi % 2 == 0) else 5
                            nfull = 48 + 4 * qi
                            nk = nfull + 4
                            slots = {}

                            def emit_qk(kt, e_=e_, qi=qi, nfull=nfull, rows=rows, kres=kres, qres=qres, slots=slots):
                                di = kt - nfull
                                c0 = 128 * di if di >= 0 else 0
                                sb_ = sring[0] % 4
                                pt_ = sring[0] % 6
                                sring[0] += 1
                                slots[kt] = (c0, pt_)
                                MMS([(bank[sb_][:, c0:512], [(KAB[e_][rows, kt * 128:(kt + 1) * 128], QAB[e_][rows, qi * 512 + c0:(qi + 1) * 512])])],
                                    kres + qres, [f"ps{sb_}"])
                                ACT(PTm[pt_][:, c0:512], bank[sb_][:, c0:512], AF.Exp, [f"ps{sb_}"], [f"m_pt{pt_}"])
                                if di >= 0:
                                    TT(PTm[pt_][:, c0:c0 + 128], PTm[pt_][:, c0:c0 + 128], trib[:, :], ALU.mult, [f"m_pt{pt_}", "trib"], [f"m_pt{pt_}"],
                                       eng="pool")

                            def emit_pv(kt, e_=e_, ob_=ob_, nk=nk, slots=slots):
                                c0, pt_ = slots[kt]
                                P.op("pe", (lambda kt=kt, c0=c0, pt_=pt_, ob_=ob_, nk=nk, e_=e_:
                                            (lambda e: e.matmul(bank[ob_][0:65, c0:512], lhsT=VAB[:, kt, e_, :], rhs=PTm[pt_][:, c0:512],
                                                                start=(kt == 0), stop=(kt == nk - 1))))(),
                                     [f"m_v{e_}", "m_vval", f"m_pt{pt_}"], [f"ps{ob_}"])

                            LA = 3
                            for kt in range(min(LA, nk)):
                                emit_qk(kt)
                            for fn_ in pending:
                                fn_()
                            del pending[:]
                            for kt in range(nk):
                                if kt + LA < nk:
                                    emit_qk(kt + LA)
                                emit_pv(kt)

                            def evac(ob_=ob_, h=h, qi=qi):
                                ACP(osb[0:65, :], bank[ob_][0:65, :], [f"ps{ob_}"], ["m_osb"])
                                RCP(rdm[64:65, :], osb[64:65, :], ["m_osb"], ["m_rd"])
                                MMS([(bank[7][0:64, :], [(onesf[64:65, 0:64], rdm[64:65, :])])], ["onesf", "m_rd"], ["ps7"])
                                TT(YB[0:64, h, qi * 512:(qi + 1) * 512], osb[0:64, :], bank[7][0:64, :], ALU.mult, ["m_osb", "ps7"], [f"YB{h}"])
                            pending.append(evac)
                    for fn_ in pending:
                        fn_()
                    del pending[:]
                if DBG[0]:
                    DMA(YBD, YB[:], [f"YB{h}" for h in range(8)], ["YBD"])
                P.flush()
                _chk(3)

            for ph in _phase(4):
                hown = [SB(ph, f"g_h{i}", [128, 8, ST], BF16) for i in range(2)]
                mg = SB(ph, "g_mg", [128, 8, ST], BF16)
                wga = [SB(ph, f"g_wga{i}", [128, 8, 128], BF16) for i in range(2)]
                wgb = [SB(ph, f"g_wgb{i}", [128, 8, 128], BF16) for i in range(2)]
                wua = [SB(ph, f"g_wua{i}", [128, 4, 128], BF16) for i in range(2)]
                wub = [SB(ph, f"g_wub{i}", [128, 8, 128], BF16) for i in range(2)]
                sga = [SB(ph, f"g_sa{i}", [128, 512], F32) for i in range(2)]
                sgb = [SB(ph, f"g_sb{i}", [128, 512], F32) for i in range(2)]
                yall = [f"YA{hd}" for hd in range(4)] + [f"YB{h}" for h in range(8)]
                if 2 not in RUN[0]:
                    DMA(YA[:], YAD, (), [f"YA{hd}" for hd in range(4)])
                if 3 not in RUN[0]:
                    DMA(YB[:], YBD, (), [f"YB{h}" for h in range(8)])
                it = 0
                for stl in range(2):
                    H_ = hown[stl]
                    DMA(H_[:], HSr[:, :, OWN0 + stl * ST:OWN0 + (stl + 1) * ST], [f"HS{6 + stl}"], [f"g_h{stl}"])
                    for o in range(8):
                        s = o % 2
                        WLOAD(wga[s][:].rearrange("p k c -> p (k c)"), f"ga{o}", [f"g_wga{s}"])
                        WLOAD(wgb[s][:].rearrange("p k c -> p (k c)"), f"gb{o}", [f"g_wgb{s}"])
                        WLOAD(wua[s][:].rearrange("p k c -> p (k c)"), f"ua{o}", [f"g_wua{s}"])
                        WLOAD(wub[s][:].rearrange("p k c -> p (k c)"), f"ub{o}", [f"g_wub{s}"])
                        for hf in range(2):
                            b0 = 4 * (it % 2)
                            ts_ = it % 2
                            it += 1
                            lc = slice(hf * 512, (hf + 1) * 512)
                            gc = slice(stl * ST + hf * 512, stl * ST + (hf + 1) * 512)
                            MMS([(bank[b0][:, :], [(wua[s][0:64, hd, :], YA[0:64, hd, gc]) for hd in range(4)]),
                                 (bank[b0 + 1][:, :], [(wub[s][0:64, h, :], YB[0:64, h, gc]) for h in range(8)]),
                                 (bank[b0 + 2][:, :], [(wga[s][:, k, :], H_[:, k, lc]) for k in range(8)]),
                                 (bank[b0 + 3][:, :], [(wgb[s][:, k, :], H_[:, k, lc]) for k in range(8)])],
                                yall + [f"g_h{stl}", f"g_wga{s}", f"g_wgb{s}", f"g_wua{s}", f"g_wub{s}"],
                                [f"ps{b0 + q_}" for q_ in range(4)])
                            ACT(sga[ts_][:, :], bank[b0 + 2][:, :], AF.Sigmoid, [f"ps{b0 + 2}"], [f"g_sa{ts_}"])
                            ACT(sgb[ts_][:, :], bank[b0 + 3][:, :], AF.Sigmoid, [f"ps{b0 + 3}"], [f"g_sb{ts_}"])
                            TT(sga[ts_][:, :], sga[ts_][:, :], bank[b0][:, :], ALU.mult, [f"g_sa{ts_}", f"ps{b0}"], [f"g_sa{ts_}"])
                            TT(sgb[ts_][:, :], sgb[ts_][:, :], bank[b0 + 1][:, :], ALU.mult, [f"g_sb{ts_}", f"ps{b0 + 1}"], [f"g_sb{ts_}"])
                            TT(mg[:, o, lc], sga[ts_][:, :], sgb[ts_][:, :], ALU.add, [f"g_sa{ts_}", f"g_sb{ts_}"], [f"g_mg{o}"], eng="pool")
                    DMA(MSr[:, :, stl * ST:(stl + 1) * ST], mg[:], [f"g_mg{o}" for o in range(8)], [f"MS{stl}"])
                P.flush()
                _chk(4)

        for ph in _phase(5):
            bufs = ffn_bufs(ph, "b")
            X = SB(ph, "bX", [128, 8, ST], F32)
            mgt = SB(ph, "b_mg", [128, 8, ST], BF16)
            wo = [SB(ph, f"b_wo{i}", [128, 8, 128], BF16) for i in range(2)]
            xr = lambda c: f"bX_{c}"
            for stl in range(2):
                DMA(X[:], X1r[:, :, stl * ST:(stl + 1) * ST], [f"X1S{6 + stl}"], [xr(c) for c in range(8)])
                DMA(mgt[:], MSr[:, :, stl * ST:(stl + 1) * ST], [f"MS{stl}"], ["b_mg"])
                it = 0
                for o2 in range(8):
                    s = o2 % 2
                    WLOAD(wo[s][:].rearrange("p k c -> p (k c)"), f"wo{o2}", [f"b_wo{s}"])
                    for hf in range(2):
                        pb = 6 + (it % 2)
                        it += 1
                        cols = slice(hf * 512, (hf + 1) * 512)
                        MMS([(bank[pb][:, :], [(wo[s][:, o, :], mgt[:, o, cols]) for o in range(8)])], ["b_mg", f"b_wo{s}"], [f"ps{pb}"])
                        TT(X[:, o2, cols], bank[pb][:, :], X[:, o2, cols], ALU.add, [f"ps{pb}", xr(o2)], [xr(o2)])
                ffn("b", X, xr, C_G2, "f2", bufs)
                rmsnorm("b", X, xr, C_GF, X, xr, bufs)
                DMA(outr[:, :, stl * ST:(stl + 1) * ST], X[:], [xr(c) for c in range(8)], [f"OUT{stl}"])
            P.flush()
            _chk(5)
    return nc


_NC_CACHE = {}


def kernel(x, norm_ffn1, ffn1_gate, ffn1_up, ffn1_down, norm_mix, w_in, w_up_a, w_up_b, w_out,
           norm_ffn2, ffn2_gate, ffn2_up, ffn2_down, norm_final):
    inp = dict(x=x, norm_ffn1=norm_ffn1, ffn1_gate=ffn1_gate, ffn1_up=ffn1_up, ffn1_down=ffn1_down,
               norm_mix=norm_mix, w_in=w_in, w_up_a=w_up_a, w_up_b=w_up_b, w_out=w_out, norm_ffn2=norm_ffn2,
               ffn2_gate=ffn2_gate, ffn2_up=ffn2_up, ffn2_down=ffn2_down, norm_final=norm_final)
    inp = {k: np.asarray(v, dtype=np.float32) for k, v in inp.items()}
    wall = _pack_weights(inp)
    in_maps = []
    for c in range(8):
        b, j = c // 4, c % 4
        end = 2048 * (j + 1)
        xwin = np.zeros((WIN, DM), np.float32)
        xwin[WIN - end:] = inp["x"][b, 0:end]
        xw = np.ascontiguousarray(xwin.T).reshape(8, 128, WIN)
        cst, etab, akt, aqt, trib = _const_tables(inp, j)
        in_maps.append(dict(xw=xw, wall=wall, cst=cst, etab=etab, akt=akt, aqt=aqt, trib=trib))
    if "nc" not in _NC_CACHE:
        _NC_CACHE["nc"] = build()
    res = run_bass_kernel_spmd(_NC_CACHE["nc"], in_maps, core_ids=list(range(8)))
    if DBG[0]:
        LAST["res"] = res.results
    out = np.zeros((2, SEQ, DM), np.float32)
    for c in range(8):
        b, j = c // 4, c % 4
        o = np.asarray(res.results[c]["outT"], np.float32).reshape(DM, NOWN)
        out[b, 2048 * j:2048 * (j + 1), :] = o.T
    return out
```
